# Optimizing a Trainium2 kernel written in Bass

```python
import math
import jax, jax.numpy as jnp
from jax import lax
import numpy as np

D_MODEL = 2048
BATCH = 4
SEQ = 4096
DEPTH = 2

CTX_LEN = 256
GRID_W = 64

ATT_HEADS = 16
ATT_KV_HEADS = 4
ATT_HEAD_DIM = 64
ATT_GROUP = ATT_HEADS // ATT_KV_HEADS
ATT_WINDOW = 128
ATT_BLOCK = 128
ROPE_BASE = 10000.0
ATT_Q_W = ATT_HEADS * ATT_HEAD_DIM
ATT_KV_W = ATT_KV_HEADS * ATT_HEAD_DIM

DN_HEADS = 4
DN_HEAD_DIM = 128
DN_W = DN_HEADS * DN_HEAD_DIM
DN_CHUNK = 64

LRU_WIDTH = 512
LRU_BLOCKS = 8
LRU_BLOCK_DIM = LRU_WIDTH // LRU_BLOCKS
LRU_C = 8.0

CONV_W = 4
CONV_LEFT = CONV_W // 2

N_EXPERTS = 32
TOP_K = 4
D_EXPERT = 1024
SWIGLU_LIMIT = 7.0
SWIGLU_ALPHA = 1.702
MOE_BLOCK = 256

MIX_W = ATT_Q_W + DN_W + LRU_WIDTH
IN_SPLITS = (ATT_Q_W, ATT_KV_W, ATT_KV_W, DN_W, DN_W, DN_W, DN_W,
             2 * DN_HEADS, 2 * DN_HEADS, LRU_WIDTH, LRU_WIDTH)
IN_W = sum(IN_SPLITS)
EPS = 1e-6
NEG_INF = -1e30

kernel_name = 'hybrid_lru_deltanet_swa_moe_prefix_dit'


def _rmsnorm(x, g):
    xf = x.astype(jnp.float32)
    y = xf * lax.rsqrt(jnp.mean(xf * xf, axis=-1, keepdims=True) + EPS)
    return (y * g.astype(jnp.float32)).astype(x.dtype)


def _modulate(x, g, shift, scale):
    return _rmsnorm(x, g) * (1.0 + scale) + shift


def _centred_dwconv(x, w):
    t = x.shape[1]
    xp = jnp.pad(x, ((0, 0), (CONV_LEFT, CONV_W - 1 - CONV_LEFT), (0, 0)))
    return sum(xp[:, j:j + t] * w[j] for j in range(CONV_W))


def _l2norm(x):
    return x * lax.rsqrt(jnp.sum(x * x, axis=-1, keepdims=True) + EPS)


def _rotate(xa, pos):
    r = xa.shape[-1]
    inv = ROPE_BASE ** (-jnp.arange(0, r, 2, dtype=jnp.float32) / r)
    ang = pos.astype(jnp.float32)[:, None] * inv
    cos = jnp.cos(ang)[None, :, None, :].astype(xa.dtype)
    sin = jnp.sin(ang)[None, :, None, :].astype(xa.dtype)
    x1, x2 = xa[..., :r // 2], xa[..., r // 2:]
    return jnp.concatenate([x1 * cos - x2 * sin, x2 * cos + x1 * sin], axis=-1)


def _axial_rope(x, row, col):
    half = x.shape[-1] // 2
    return jnp.concatenate([_rotate(x[..., :half], row), _rotate(x[..., half:], col)], axis=-1)


def _sink_probs(logits, sink):
    m = jnp.maximum(jnp.max(logits, axis=-1, keepdims=True), sink)
    p = jnp.exp(logits - m)
    return p / (jnp.sum(p, axis=-1, keepdims=True) + jnp.exp(sink - m))


def _window_attention(q, k, v, kc, vc, sink_b):
    b, s = q.shape[:2]
    nblk = s // ATT_BLOCK
    scale = ATT_HEAD_DIM ** -0.5
    qb = jnp.moveaxis(q.reshape(b, nblk, ATT_BLOCK, ATT_KV_HEADS, ATT_GROUP, ATT_HEAD_DIM), 1, 0)
    pad = ((0, 0), (ATT_BLOCK, ATT_BLOCK), (0, 0), (0, 0))
    kp = jnp.pad(k, pad)
    vp = jnp.pad(v, pad)
    span = 3 * ATT_BLOCK
    rel = (jnp.arange(span)[None, :] - ATT_BLOCK) - jnp.arange(ATT_BLOCK)[:, None]

    def block(args):
        qi, i = args
        start = i * ATT_BLOCK
        ki = lax.dynamic_slice_in_dim(kp, start, span, axis=1)
        vi = lax.dynamic_slice_in_dim(vp, start, span, axis=1)
        kabs = start - ATT_BLOCK + jnp.arange(span)
        valid = (jnp.abs(rel) <= ATT_WINDOW) & (kabs >= 0)[None, :] & (kabs < s)[None, :]
        lg_l = jnp.einsum('bqkgd,bnkd->bkgqn', qi, ki, preferred_element_type=jnp.float32) * scale
        lg_l = jnp.where(valid, lg_l, NEG_INF)
        lg_c = jnp.einsum('bqkgd,bckd->bkgqc', qi, kc, preferred_element_type=jnp.float32) * scale
        p = _sink_probs(jnp.concatenate([lg_l, lg_c], axis=-1), sink_b).astype(v.dtype)
        return (jnp.einsum('bkgqn,bnkd->bqkgd', p[..., :span], vi)
                + jnp.einsum('bkgqc,bckd->bqkgd', p[..., span:], vc))

    o = lax.map(block, (qb, jnp.arange(nblk)))
    return jnp.moveaxis(o, 0, 1).reshape(b, s, ATT_Q_W)


def _context_attention(qc, kc, vc, sink_b):
    b, l = qc.shape[:2]
    lg = jnp.einsum('bqkgd,bckd->bkgqc', qc, kc, preferred_element_type=jnp.float32) * ATT_HEAD_DIM ** -0.5
    p = _sink_probs(lg, sink_b).astype(vc.dtype)
    return jnp.einsum('bkgqc,bckd->bqkgd', p, vc).reshape(b, l, ATT_Q_W)


def _attention_mixer(lat, ctxp, sink, row, col, need_ctx):
    ql, kl, vl = lat
    qc, kc, vc = ctxp
    b, s = ql.shape[:2]
    l = qc.shape[1]
    ql = _axial_rope(ql.reshape(b, s, ATT_HEADS, ATT_HEAD_DIM), row, col)
    kl = _axial_rope(kl.reshape(b, s, ATT_KV_HEADS, ATT_HEAD_DIM), row, col)
    vl = vl.reshape(b, s, ATT_KV_HEADS, ATT_HEAD_DIM)
    kc = kc.reshape(b, l, ATT_KV_HEADS, ATT_HEAD_DIM)
    vc = vc.reshape(b, l, ATT_KV_HEADS, ATT_HEAD_DIM)
    sink_b = sink.astype(jnp.float32).reshape(ATT_KV_HEADS, ATT_GROUP)[None, :, :, None, None]
    out_l = _window_attention(ql, kl, vl, kc, vc, sink_b)
    out_c = None
    if need_ctx:
        out_c = _context_attention(qc.reshape(b, l, ATT_KV_HEADS, ATT_GROUP, ATT_HEAD_DIM), kc, vc, sink_b)
    return out_l, out_c


def _gated_delta_chunked(q, k, v, beta, g, s0):
    b, t, h, dk = q.shape
    dv = v.shape[-1]
    n = t // DN_CHUNK

    def chunks(a):
        a = a.reshape((b, n, DN_CHUNK) + a.shape[2:])
        return jnp.moveaxis(jnp.moveaxis(a, 3, 2), 1, 0)

    qc, kc, vc, bc, gc = chunks(q), chunks(k), chunks(v), chunks(beta), chunks(g)
    gcum = jnp.cumsum(gc, axis=-1)
    tri_incl = jnp.tril(jnp.ones((DN_CHUNK, DN_CHUNK), bool))
    tri_strict = jnp.tril(jnp.ones((DN_CHUNK, DN_CHUNK), bool), -1)
    decay = jnp.exp(jnp.where(tri_incl, gcum[..., :, None] - gcum[..., None, :], -jnp.inf))
    kb = kc * bc[..., None]
    a_strict = jnp.where(tri_strict, jnp.einsum('nbhrd,nbhjd->nbhrj', kb, kc) * decay, 0.0)
    rhs = jnp.concatenate([vc * bc[..., None], kb * jnp.exp(gcum)[..., None]], axis=-1)
    sol = lax.linalg.triangular_solve(a_strict, rhs, left_side=True, lower=True, unit_diagonal=True)
    u, w = sol[..., :dv], sol[..., dv:]
    qk = jnp.einsum('nbhrd,nbhjd->nbhrj', qc, kc) * decay

    def step(state, xs):
        qk_i, q_i, k_i, u_i, w_i, g_i = xs
        v_new = u_i - jnp.einsum('bhcd,bhde->bhce', w_i, state)
        o = (jnp.einsum('bhcd,bhde->bhce', q_i * jnp.exp(g_i)[..., None], state)
             + jnp.einsum('bhcj,bhje->bhce', qk_i, v_new))
        g_last = g_i[..., -1:]
        state = (state * jnp.exp(g_last)[..., None]
                 + jnp.einsum('bhcd,bhce->bhde', k_i * jnp.exp(g_last - g_i)[..., None], v_new))
        return state, o

    s_fin, o = lax.scan(step, s0, (qk, qc, kc, u, w, gcum))
    o = jnp.moveaxis(jnp.moveaxis(o, 0, 1), 2, 3).reshape(b, t, h, dv)
    return o, s_fin


def _deltanet_prep(pq, pk, pv, pb, pa, conv_w, a_log, dt_bias):
    b, t = pq.shape[:2]
    qkv = jax.nn.silu(_centred_dwconv(jnp.concatenate([pq, pk, pv], axis=-1), conv_w)).astype(jnp.float32)
    q, k, v = jnp.split(qkv, 3, axis=-1)
    q = _l2norm(q.reshape(b, t, DN_HEADS, DN_HEAD_DIM)) * DN_HEAD_DIM ** -0.5
    k = _l2norm(k.reshape(b, t, DN_HEADS, DN_HEAD_DIM))
    v = v.reshape(b, t, DN_HEADS, DN_HEAD_DIM)
    beta = jax.nn.sigmoid(pb.astype(jnp.float32)).reshape(b, t, 2, DN_HEADS)
    g = (-jnp.exp(a_log.astype(jnp.float32))
         * jax.nn.softplus(pa.astype(jnp.float32).reshape(b, t, 2, DN_HEADS) + dt_bias.astype(jnp.float32)))
    return q, k, v, beta, g


def _deltanet_gated_out(o, gate, norm_g, dtype):
    b, t = o.shape[:2]
    y = o * lax.rsqrt(jnp.mean(o * o, axis=-1, keepdims=True) + EPS) * norm_g.astype(jnp.float32)
    y = y * jax.nn.silu(gate.astype(jnp.float32).reshape(b, t, DN_HEADS, DN_HEAD_DIM))
    return y.reshape(b, t, DN_W).astype(dtype)


def _deltanet_mixer(lat, ctxp, conv_w, a_log, dt_bias, norm_g, need_ctx):
    ql, kl, vl, bl, gl = _deltanet_prep(lat[0], lat[1], lat[2], lat[4], lat[5], conv_w, a_log, dt_bias)
    qc, kc, vc, bc, gc = _deltanet_prep(ctxp[0], ctxp[1], ctxp[2], ctxp[4], ctxp[5], conv_w, a_log, dt_bias)
    s0 = jnp.zeros((ql.shape[0], DN_HEADS, DN_HEAD_DIM, DN_HEAD_DIM), jnp.float32)
    fl = lambda a: jnp.flip(a, axis=1)
    oc_f, sc_f = _gated_delta_chunked(qc, kc, vc, bc[:, :, 0], gc[:, :, 0], s0)
    ol_f, _ = _gated_delta_chunked(ql, kl, vl, bl[:, :, 0], gl[:, :, 0], sc_f)
    oc_b, sc_b = _gated_delta_chunked(fl(qc), fl(kc), fl(vc), fl(bc[:, :, 1]), fl(gc[:, :, 1]), s0)
    ol_b, _ = _gated_delta_chunked(fl(ql), fl(kl), fl(vl), fl(bl[:, :, 1]), fl(gl[:, :, 1]), sc_b)
    out_l = _deltanet_gated_out(ol_f + fl(ol_b), lat[3], norm_g, lat[0].dtype)
    out_c = _deltanet_gated_out(oc_f + fl(oc_b), ctxp[3], norm_g, ctxp[0].dtype) if need_ctx else None
    return out_l, out_c


def _rglru_coeffs(xc, w_r, b_r, w_i, b_i, lam):
    b, t = xc.shape[:2]
    xr = xc.reshape(b, t, LRU_BLOCKS, LRU_BLOCK_DIM)
    r = jax.nn.sigmoid(jnp.einsum('btni,nij->btnj', xr, w_r.astype(jnp.float32)).reshape(b, t, LRU_WIDTH) + b_r)
    i = jax.nn.sigmoid(jnp.einsum('btni,nij->btnj', xr, w_i.astype(jnp.float32)).reshape(b, t, LRU_WIDTH) + b_i)
    log_a = LRU_C * r * jax.nn.log_sigmoid(lam.astype(jnp.float32))
    return jnp.exp(log_a), jnp.sqrt(-jnp.expm1(2.0 * log_a)) * (i * xc)


def _linear_scan(a, u, h0):
    def combine(lhs, rhs):
        return lhs[0] * rhs[0], rhs[0] * lhs[1] + rhs[1]
    a_cum, u_cum = lax.associative_scan(combine, (a, u), axis=1)
    return a_cum * h0[:, None, :] + u_cum


def _ctx_then_latent(ac, uc, al, ul, h0):
    hc = _linear_scan(ac, uc, h0)
    return hc, _linear_scan(al, ul, hc[:, -1])


def _lru_mixer(lat, ctxp, conv_w, conv_b, w_r, b_r, w_i, b_i, lam, need_ctx):
    xl, yl = lat
    xc, yc = ctxp
    ul_in = (_centred_dwconv(xl, conv_w) + conv_b).astype(jnp.float32)
    uc_in = (_centred_dwconv(xc, conv_w) + conv_b).astype(jnp.float32)
    h0 = jnp.zeros((xl.shape[0], LRU_WIDTH), jnp.float32)
    fl = lambda a: jnp.flip(a, axis=1)
    acf, ucf = _rglru_coeffs(uc_in, w_r[0], b_r[0], w_i[0], b_i[0], lam[0])
    alf, ulf = _rglru_coeffs(ul_in, w_r[0], b_r[0], w_i[0], b_i[0], lam[0])
    hc_f, hl_f = _ctx_then_latent(acf, ucf, alf, ulf, h0)
    acb, ucb = _rglru_coeffs(uc_in, w_r[1], b_r[1], w_i[1], b_i[1], lam[1])
    alb, ulb = _rglru_coeffs(ul_in, w_r[1], b_r[1], w_i[1], b_i[1], lam[1])
    hc_b, hl_b = _ctx_then_latent(fl(acb), fl(ucb), fl(alb), fl(ulb), h0)
    out_l = ((hl_f + fl(hl_b)) * jax.nn.gelu(yl.astype(jnp.float32))).astype(xl.dtype)
    out_c = ((hc_f + fl(hc_b)) * jax.nn.gelu(yc.astype(jnp.float32))).astype(xc.dtype) if need_ctx else None
    return out_l, out_c


def _moe(h, w_router, b_router, w_gu, b_gu, w_down, b_down):
    n, d = h.shape
    logits = (h @ w_router + b_router).astype(jnp.float32)
    top_val, top_idx = lax.top_k(logits, TOP_K)
    gates = jax.nn.softmax(top_val, axis=-1)
    e_flat = top_idx.reshape(-1)
    tok_flat = jnp.repeat(jnp.arange(n), TOP_K)
    order = jnp.argsort(e_flat)
    e_s, tok_s, g_s = e_flat[order], tok_flat[order], gates.reshape(-1)[order]
    counts = jnp.bincount(e_flat, length=N_EXPERTS)
    padded = (counts + MOE_BLOCK - 1) // MOE_BLOCK * MOE_BLOCK
    start = jnp.cumsum(counts) - counts
    pend = jnp.cumsum(padded)
    dest = (pend - padded)[e_s] + (jnp.arange(n * TOP_K) - start[e_s])
    n_blocks = -(-(n * TOP_K + N_EXPERTS * (MOE_BLOCK - 1)) // MOE_BLOCK)
    rows = n_blocks * MOE_BLOCK
    row_tok = jnp.full((rows,), n, jnp.int32).at[dest].set(tok_s.astype(jnp.int32))
    row_gate = jnp.zeros((rows,), jnp.float32).at[dest].set(g_s)
    blk_exp = jnp.minimum(jnp.searchsorted(pend, jnp.arange(n_blocks) * MOE_BLOCK, side='right'), N_EXPERTS - 1)
    h_pad = jnp.concatenate([h, jnp.zeros((1, d), h.dtype)], axis=0)

    def run_block(acc, args):
        tok, gate, e = args
        xb = h_pad[tok]
        gu = xb @ w_gu[e] + b_gu[e]
        glu = jnp.minimum(gu[:, :D_EXPERT], SWIGLU_LIMIT)
        lin = jnp.clip(gu[:, D_EXPERT:], -SWIGLU_LIMIT, SWIGLU_LIMIT)
        act = (lin + 1.0) * glu * jax.nn.sigmoid(SWIGLU_ALPHA * glu)
        y = (act @ w_down[e] + b_down[e]) * gate[:, None].astype(h.dtype)
        return acc.at[tok].add(y.astype(acc.dtype)), None

    acc, _ = lax.scan(run_block, jnp.zeros((n + 1, d), h.dtype),
                      (row_tok.reshape(n_blocks, MOE_BLOCK), row_gate.reshape(n_blocks, MOE_BLOCK), blk_exp))
    return acc[:n]


def setup_inputs(seed: int = 0) -> dict:
    key = jax.random.key(seed)
    ks = jax.random.split(key, 32)
    f32 = jnp.float32
    L, D = DEPTH, D_MODEL

    def nrm(k, shape, s):
        return jax.random.normal(k, shape, f32) * s

    dt = jnp.exp(jax.random.uniform(ks[13], (L, 2, DN_HEADS), f32, math.log(1e-3), math.log(1e-1)))
    a_base = jax.random.uniform(ks[21], (L, 2, LRU_WIDTH), f32, 0.9, 0.999) ** (1.0 / LRU_C)
    return {
        'x': nrm(ks[0], (BATCH, SEQ, D), 1.0),
        'c': nrm(ks[1], (BATCH, D), 1.0),
        'ctx': nrm(ks[2], (BATCH, CTX_LEN, D), 1.0),
        'c_ctx': nrm(ks[3], (D,), 1.0),
        'w_mod': nrm(ks[4], (L, D, 6 * D), 0.5 * D ** -0.5),
        'b_mod': nrm(ks[5], (L, 6 * D), 0.02),
        'norm_mix_g': 1.0 + nrm(ks[6], (L, D), 0.02),
        'norm_ffn_g': 1.0 + nrm(ks[7], (L, D), 0.02),
        'w_in': nrm(ks[8], (L, D, IN_W), D ** -0.5),
        'w_out': nrm(ks[9], (L, MIX_W, D), MIX_W ** -0.5),
        'attn_sink': nrm(ks[10], (L, ATT_HEADS), 0.5),
        'dn_conv_w': nrm(ks[11], (L, CONV_W, 3 * DN_W), CONV_W ** -0.5),
        'dn_a_log': jnp.log(jax.random.uniform(ks[12], (L, 2, DN_HEADS), f32, 1.0, 16.0)),
        'dn_dt_bias': dt + jnp.log(-jnp.expm1(-dt)),
        'dn_norm_g': 1.0 + nrm(ks[14], (L, DN_HEAD_DIM), 0.02),
        'lru_conv_w': nrm(ks[15], (L, CONV_W, LRU_WIDTH), CONV_W ** -0.5),
        'lru_conv_b': nrm(ks[16], (L, LRU_WIDTH), 0.02),
        'lru_w_rgate': nrm(ks[17], (L, 2, LRU_BLOCKS, LRU_BLOCK_DIM, LRU_BLOCK_DIM), LRU_BLOCK_DIM ** -0.5),
        'lru_b_rgate': nrm(ks[18], (L, 2, LRU_WIDTH), 0.02),
        'lru_w_igate': nrm(ks[19], (L, 2, LRU_BLOCKS, LRU_BLOCK_DIM, LRU_BLOCK_DIM), LRU_BLOCK_DIM ** -0.5),
        'lru_b_igate': nrm(ks[20], (L, 2, LRU_WIDTH), 0.02),
        'lru_lambda': jnp.log(a_base) - jnp.log1p(-a_base),
        'w_router': nrm(ks[22], (L, D, N_EXPERTS), D ** -0.5),
        'b_router': nrm(ks[23], (L, N_EXPERTS), 0.01),
        'w_gu': nrm(ks[24], (L, N_EXPERTS, D, 2 * D_EXPERT), D ** -0.5),
        'b_gu': nrm(ks[25], (L, N_EXPERTS, 2 * D_EXPERT), 0.02),
        'w_down': nrm(ks[26], (L, N_EXPERTS, D_EXPERT, D), D_EXPERT ** -0.5),
        'b_down': nrm(ks[27], (L, N_EXPERTS, D), 0.02),
        'final_norm_g': 1.0 + nrm(ks[28], (D,), 0.02),
    }


def reference(x, c, ctx, c_ctx, w_mod, b_mod, norm_mix_g, norm_ffn_g, w_in, w_out, attn_sink,
              dn_conv_w, dn_a_log, dn_dt_bias, dn_norm_g, lru_conv_w, lru_conv_b,
              lru_w_rgate, lru_b_rgate, lru_w_igate, lru_b_igate, lru_lambda,
              w_router, b_router, w_gu, b_gu, w_down, b_down, final_norm_g):
    b, s, d = x.shape
    rows = s // GRID_W
    row = jnp.repeat(jnp.arange(rows), GRID_W)
    col = jnp.arange(rows * GRID_W) % GRID_W
    split_idx = np.cumsum(IN_SPLITS)[:-1].tolist()
    sc = jax.nn.silu(c)
    scc = jax.nn.silu(c_ctx)
    xl, xc = x, ctx
    for l in range(DEPTH):
        need_ctx = l < DEPTH - 1
        mod_l = jnp.split((sc @ w_mod[l] + b_mod[l])[:, None, :], 6, axis=-1)
        mod_c = jnp.split(scc @ w_mod[l] + b_mod[l], 6, axis=-1)
        hl = _modulate(xl, norm_mix_g[l], mod_l[0], mod_l[1])
        hc = _modulate(xc, norm_mix_g[l], mod_c[0], mod_c[1])
        pl = jnp.split(hl @ w_in[l], split_idx, axis=-1)
        pc = jnp.split(hc @ w_in[l], split_idx, axis=-1)
        att_l, att_c = _attention_mixer(pl[0:3], pc[0:3], attn_sink[l], row, col, need_ctx)
        dn_l, dn_c = _deltanet_mixer(pl[3:9], pc[3:9], dn_conv_w[l], dn_a_log[l], dn_dt_bias[l],
                                     dn_norm_g[l], need_ctx)
        lru_l, lru_c = _lru_mixer(pl[9:11], pc[9:11], lru_conv_w[l], lru_conv_b[l], lru_w_rgate[l],
                                  lru_b_rgate[l], lru_w_igate[l], lru_b_igate[l], lru_lambda[l], need_ctx)
        xl = xl + mod_l[2] * (jnp.concatenate([att_l, dn_l, lru_l], axis=-1) @ w_out[l])
        hl = _modulate(xl, norm_ffn_g[l], mod_l[3], mod_l[4])
        if need_ctx:
            xc = xc + mod_c[2] * (jnp.concatenate([att_c, dn_c, lru_c], axis=-1) @ w_out[l])
            hc = _modulate(xc, norm_ffn_g[l], mod_c[3], mod_c[4])
            y = _moe(jnp.concatenate([hl.reshape(-1, d), hc.reshape(-1, d)], axis=0),
                     w_router[l], b_router[l], w_gu[l], b_gu[l], w_down[l], b_down[l])
            xl = xl + mod_l[5] * y[:b * s].reshape(b, s, d)
            xc = xc + mod_c[5] * y[b * s:].reshape(xc.shape)
        else:
            y = _moe(hl.reshape(-1, d), w_router[l], b_router[l], w_gu[l], b_gu[l], w_down[l], b_down[l])
            xl = xl + mod_l[5] * y.reshape(b, s, d)
    return _rmsnorm(xl, final_norm_g)
```

```python
import numpy as np
from contextlib import ExitStack
import concourse.bass as bass
import concourse.mybir as mybir
from concourse.bass_utils import run_bass_kernel_spmd

F32 = mybir.dt.float32
BF16 = mybir.dt.bfloat16
I32 = mybir.dt.int32
U32 = mybir.dt.uint32
AF = mybir.ActivationFunctionType
ALU = mybir.AluOpType
AX = mybir.AxisListType

SEM_LIMIT = 30000

D = 2048
NCTX = 256
NLAT = 4096
T = NCTX + NLAT
OWN_CTX = 256
OWN_LAT = 4096
NE = 32
DE = 1024
EPS = 1e-6


class Res:
    __slots__ = ("w", "r", "name")

    def __init__(self, name=""):
        self.w = []
        self.r = []
        self.name = name


class Eng:
    def __init__(self, kb, name, h, same_sync):
        self.kb = kb
        self.name = name
        self.h = h
        self.same_sync = same_sync
        self.sem = kb.new_sem(name)
        self.count = 0
        self.seen = {}
        self.dma_sems = []
        self.dma_cnt = []
        self.dma_rr = 0
        self.prev = None

    def _wait(self, dep):
        sem, val = dep
        if self.seen.get(sem.num, 0) >= val:
            return
        self.h.wait_ge(sem, val)
        self.seen[sem.num] = val
        if (sem.num, val) in self.kb.settle:
            self.h.nop(cycle_cnt=2400)

    def _roll(self):
        if self.count >= SEM_LIMIT:
            self.prev = (self.sem, self.count)
            self.sem = self.kb.new_sem(self.name)
            self.count = 0


class KB:
    def __init__(self, nc):
        self.nc = nc
        self.es = ExitStack()
        self.stacks = [self.es]
        self.nsem = 0
        self.pe = Eng(self, "pe", nc.tensor, False)
        self.act = Eng(self, "act", nc.scalar, True)
        self.dve = Eng(self, "dve", nc.vector, True)
        self.pool = Eng(self, "pool", nc.gpsimd, True)
        self.sp = Eng(self, "sp", nc.sync, True)
        self.engs = [self.pe, self.act, self.dve, self.pool, self.sp]
        self.n_ops = 0
        self.uid = 0
        self.banks = []
        for i in range(8):
            t = self.es.enter_context(nc.psum_tensor(f"bank{i}", [128, 512], F32))
            self.banks.append((t, Res(f"bank{i}")))
        self.bank_rr = 0
        self.debug = set()
        self._dram, self._res, self._inp = {}, {}, {}
        self._bregs = {}
        self.settle = set()

    def new_sem(self, name):
        self.nsem += 1
        return self.es.enter_context(self.nc.semaphore(f"s{self.nsem}_{name}"))

    def bank(self):
        b = self.banks[self.bank_rr % 8]
        self.bank_rr += 1
        return b

    def sbuf(self, name, shape, dtype):
        self.uid += 1
        t = self.stacks[-1].enter_context(self.nc.sbuf_tensor(f"{name}_{self.uid}", list(shape), dtype))
        return t, Res(name)

    def dram(self, name, shape, dtype):
        if name in self._dram:
            return self._dram[name]
        kind = "ExternalOutput" if name in self.debug else "Internal"
        ap = self.nc.dram_tensor(name, list(shape), dtype, kind=kind).ap()
        self._dram[name] = ap
        return ap

    def res(self, name):
        if name not in self._res:
            self._res[name] = Res(name)
        return self._res[name]

    def inp(self, name, shape, dtype=F32):
        if name not in self._inp:
            self._inp[name] = self.nc.dram_tensor(name, list(shape), dtype, kind="ExternalInput").ap()
        return self._inp[name]

    def begin_phase(self):
        self.stacks.append(ExitStack())

    def end_phase(self):
        self.barrier()
        self.stacks.pop().close()

    def barrier(self):
        toks = []
        for e in self.engs:
            if e.count > 0:
                toks.append((e.sem, e.count))
            elif e.prev is not None:
                toks.append(e.prev)
            for s, c in zip(e.dma_sems, e.dma_cnt):
                if c > 0:
                    toks.append((s, c * 16))
        for e in self.engs:
            for tk in toks:
                if tk[0] is e.sem and not e.same_sync:
                    continue
                e._wait(tk)

    def _deps(self, eng, reads, writes, mwrites=()):
        def wt(d):
            if not (d[0] is eng.sem and not eng.same_sync):
                eng._wait(d)
        for r in reads:
            for d in r.w:
                wt(d)
        for w in writes:
            for d in w.w:
                wt(d)
            for d in w.r:
                wt(d)
        for w in mwrites:
            for d in w.r:
                wt(d)

    @staticmethod
    def _prune(lst):
        last = {}
        for s_, v in lst:
            if s_.num not in last or last[s_.num][1] < v:
                last[s_.num] = (s_, v)
        return list(last.values())

    def _commit(self, tok, reads, writes, mwrites=()):
        for w in writes:
            w.w = [tok]
            w.r = []
        for w in mwrites:
            w.w.append(tok)
            if len(w.w) > 32:
                w.w = self._prune(w.w)
        for r in reads:
            if r not in writes:
                r.r.append(tok)
                if len(r.r) > 32:
                    r.r = self._prune(r.r)

    def op(self, eng, fn, reads=(), writes=(), mwrites=()):
        self._deps(eng, reads, writes, mwrites)
        eng._roll()
        ins = fn()
        eng.count += 1
        ins.then_inc(eng.sem, 1)
        tok = (eng.sem, eng.count)
        self._commit(tok, reads, writes, mwrites)
        self.n_ops += 1
        return tok

    def _dma_sem(self, eng, nsems):
        if len(eng.dma_sems) < nsems:
            eng.dma_sems.append(self.new_sem(eng.name + "dma"))
            eng.dma_cnt.append(0)
            i = len(eng.dma_sems) - 1
        else:
            i = eng.dma_rr % len(eng.dma_sems)
            if eng.dma_cnt[i] * 16 >= SEM_LIMIT:
                eng._wait((eng.dma_sems[i], eng.dma_cnt[i] * 16))
                eng.dma_sems[i] = self.new_sem(eng.name + "dma")
                eng.dma_cnt[i] = 0
        eng.dma_rr += 1
        if eng.dma_cnt[i] > 0:
            eng._wait((eng.dma_sems[i], eng.dma_cnt[i] * 16))
        return i

    def dma(self, eng, out, in_, reads=(), writes=(), mwrites=(), nsems=8, settle=False, **kw):
        self._deps(eng, reads, writes, mwrites)
        i = self._dma_sem(eng, nsems)
        sem = eng.dma_sems[i]
        ins = eng.h.dma_start(out=out, in_=in_, **kw)
        eng.dma_cnt[i] += 1
        ins.then_inc(sem, 16)
        tok = (sem, eng.dma_cnt[i] * 16)
        try:
            bcast = any(int(st_) == 0 and int(n_) > 1 for st_, n_ in in_.ap)
        except Exception:
            bcast = False
        if eng is self.pool or settle or bcast:
            self.settle.add((sem.num, tok[1]))
        self._commit(tok, reads, writes, mwrites)
        self.n_ops += 1
        return tok

    def idma(self, out=None, out_offset=None, in_=None, in_offset=None, reads=(), writes=(), mwrites=(), nsems=8, **kw):
        eng = self.pool
        self._deps(eng, reads, writes, mwrites)
        i = self._dma_sem(eng, nsems)
        sem = eng.dma_sems[i]
        if "bounds_check" in kw and isinstance(kw["bounds_check"], int):
            bv = kw["bounds_check"]
            if bv not in self._bregs:
                reg = self.nc.gpsimd.alloc_register(f"bnd{bv}")
                self.nc.gpsimd.reg_mov(reg, bv)
                self._bregs[bv] = reg
            kw["bounds_check"] = self._bregs[bv]
        ins = eng.h.indirect_dma_start(out=out, out_offset=out_offset, in_=in_, in_offset=in_offset, **kw)
        eng.dma_cnt[i] += 1
        ins.then_inc(sem, 16)
        tok = (sem, eng.dma_cnt[i] * 16)
        self._commit(tok, reads, writes, mwrites)
        self.n_ops += 1
        return tok

    def finish(self):
        self.barrier()
        while self.stacks:
            self.stacks.pop().close()


GROUPS = [(0, NCTX)] + [(NCTX + 512 * i, 512) for i in range(NLAT // 512)]
C_AQ, C_AK, C_AV, C_DQ, C_DK, C_DV, C_DG, C_DB, C_DA, C_LX, C_LY = (
    0, 1024, 1280, 1536, 2048, 2560, 3072, 3584, 3592, 3600, 4112)
IN_W = 4624
PR_Q, PR_K, PR_DN, PR_LRU, PR_BD, PR_ROWS = 0, 1024, 1280, 3328, 4352, 4368


class Layer:
    SHARED = ("xall", "c2", "ropec", "ropes", "permT", "final_g", "iota32", "iotap")

    def __init__(self, nc, kb, need_ctx, stop_after=None, lidx=0, last=False):
        self.nc = nc
        self.kb = kb
        self.need_ctx = need_ctx
        self.stop_after = stop_after
        self.lidx = lidx
        self.last = last
        self.ev = 0

    def inp(self, name, shape, dtype=F32):
        if name not in self.SHARED:
            name = f"l{self.lidx}_{name}"
        return self.kb.inp(name, shape, dtype)

    def declare_io(self):
        i = self.inp
        self.xall = i("xall", [T, D])
        self.c2 = i("c2", [2, D])
        self.w_mod = i("w_mod", [D, 6 * D])
        self.b_mod = i("b_mod", [6 * D])
        self.gmix = i("gmix", [D])
        self.gffn = i("gffn", [D])
        self.w_in = i("w_in", [D, IN_W])
        self.w_bd = i("w_bd", [D, 16])
        kb = self.kb
        self.modbc = kb.dram("modbc", [12, 128, D], F32)
        self.hT_d = kb.dram("hT_d", [len(GROUPS), 128, 16, 512], BF16)
        self.projT = kb.dram("projT", [PR_ROWS, T], F32)
        self.vtok = kb.dram("vtok", [T, 256], BF16)
        self.r_modbc = kb.res("modbc")
        self.r_hT = [kb.res(f"hT_{g}") for g in range(len(GROUPS))]
        self.r_projT = kb.res("projT")
        self.r_vtok = kb.res("vtok")
        self.final_g = i("final_g", [D])
        self.xbuf = kb.dram("xbuf", [T, D], F32)
        self.r_xbuf = kb.res("xbuf")
        self.out = kb.dram("out", [NLAT, D], F32)
        self.r_out = kb.res("out")
        self.declare_attn()
        self.declare_lru()
        self.declare_dn()
        self.declare_ffn()

    def evac_eng(self):
        self.ev += 1
        return self.kb.act if self.ev % 2 else self.kb.dve

    def copy(self, eng, out, in_):
        nc = self.nc
        if eng is self.kb.act:
            return lambda: nc.scalar.copy(out=out, in_=in_)
        if eng is self.kb.dve:
            return lambda: nc.vector.tensor_copy(out=out, in_=in_)
        return lambda: nc.gpsimd.tensor_copy(out=out, in_=in_)

    def run(self):
        stop = self.stop_after
        if self.lidx == 0:
            xsrc, xsrc_r = self.xall, Res("xall")
        else:
            xsrc, xsrc_r = self.xbuf, self.r_xbuf
        self.phase_mod()
        if stop == "mod":
            return
        all_groups = [(g, GROUPS[g][0], GROUPS[g][1], 1 if g == 0 else 0) for g in range(len(GROUPS))]
        self.phase_norm(xsrc, self.gmix, 0, 1, self.hT_d, self.r_hT, all_groups, src_res=xsrc_r)
        self.phase_inproj()
        if stop == "inproj":
            return
        self.phase_rope()
        self.phase_attn()
        if stop == "attn":
            return
        self.phase_lru()
        if stop == "lru":
            return
        self.phase_dn()
        if stop == "dn":
            return
        self.phase_outproj(xsrc, xsrc_r)
        if stop == "outproj":
            return
        self.phase_route()
        if stop == "route":
            return
        self.phase_experts()
        if self.last:
            self.phase_combine(self.out, self.r_out, final_out=self.out, final_g=self.final_g)
        else:
            self.phase_combine(self.xbuf, self.r_xbuf)

    def phase_mod(self):
        nc, kb = self.nc, self.kb
        kb.begin_phase()
        sc, sc_r = kb.sbuf("sc", [128, 2, 16], F32)
        kb.dma(kb.sp, sc[:], self.c2.rearrange("s (p k) -> p s k", k=16), writes=[sc_r])
        kb.op(kb.act, lambda: nc.scalar.activation(out=sc[:], in_=sc[:], func=AF.Silu), reads=[sc_r], writes=[sc_r])
        rep, rep_r = kb.sbuf("rep", [128, 16, 2, 128], F32)
        kb.op(kb.dve, lambda: nc.vector.tensor_copy(
            out=rep[:], in_=sc[:].rearrange("p s k -> p k s").unsqueeze(3).to_broadcast([128, 16, 2, 128])),
            reads=[sc_r], writes=[rep_r])
        wts = [kb.sbuf("wmod", [128, 16, 512], F32) for _ in range(2)]
        bbs = [kb.sbuf("bmod", [128, 512], F32) for _ in range(2)]
        outs = [kb.sbuf("modo", [128, 512], F32) for _ in range(4)]
        wv = self.w_mod.rearrange("(p k) n -> p k n", k=16)
        oi = 0
        for piece in range(24):
            n0 = piece * 512
            wt, wt_r = wts[piece % 2]
            bb, bb_r = bbs[piece % 2]
            kb.dma(kb.sp if piece % 2 else kb.act, wt[:], wv[:, :, n0:n0 + 512], writes=[wt_r])
            kb.dma(kb.sp, bb[:], self.b_mod[n0:n0 + 512].partition_broadcast(128), writes=[bb_r])
            j, c0 = n0 // D, n0 % D
            for s in range(2):
                ps, ps_r = kb.bank()
                for k in range(16):
                    kb.op(kb.pe, lambda: nc.tensor.matmul(ps[:], lhsT=rep[:, k, s, :], rhs=wt[:, k, :],
                                                          start=(k == 0), stop=(k == 15)),
                          reads=[rep_r, wt_r], writes=[ps_r])
                o, o_r = outs[oi % 4]
                oi += 1
                kb.op(kb.dve, lambda: nc.vector.tensor_tensor(out=o[:], in0=ps[:], in1=bb[:], op=ALU.add),
                      reads=[ps_r, bb_r], writes=[o_r])
                kb.dma(kb.sp, self.modbc[s * 6 + j, :, c0:c0 + 512], o[:], reads=[o_r], mwrites=[self.r_modbc])
        kb.end_phase()

    def phase_norm(self, src, gvec, mod_shift, mod_scale, dst, dst_res, groups, src_res=None):
        nc, kb = self.nc, self.kb
        kb.begin_phase()
        ident, id_r = self.make_ident(BF16)
        gb, gb_r = kb.sbuf("gb", [128, D], F32)
        kb.dma(kb.sp, gb[:], gvec.partition_broadcast(128), writes=[gb_r])
        G, SH = [], []
        for s in range(2):
            g1, g1_r = kb.sbuf("g1", [128, D], F32)
            sh, sh_r = kb.sbuf("sh", [128, D], F32)
            kb.dma(kb.sp, g1[:], self.modbc[s * 6 + mod_scale], reads=[self.r_modbc], writes=[g1_r])
            kb.dma(kb.sp, sh[:], self.modbc[s * 6 + mod_shift], reads=[self.r_modbc], writes=[sh_r])
            kb.op(kb.dve, lambda: nc.vector.scalar_tensor_tensor(out=g1[:], in0=g1[:], scalar=1.0, in1=gb[:],
                                                                 op0=ALU.add, op1=ALU.mult),
                  reads=[g1_r, gb_r], writes=[g1_r])
            G.append((g1, g1_r))
            SH.append((sh, sh_r))
        xts = [kb.sbuf("xt", [128, D], F32) for _ in range(2)]
        junk, junk_r = kb.sbuf("junk", [128, D], BF16)
        hbs = [kb.sbuf("hb", [128, D], BF16) for _ in range(2)]
        hTs = [kb.sbuf("hT", [128, 16, 512], BF16) for _ in range(2)]
        sts = [kb.sbuf("st", [128, 4], F32) for _ in range(2)]
        ti = 0
        for gi, (gidx, t0, ntok, s) in enumerate(groups):
            hT, hT_r = hTs[gi % 2]
            for j in range(ntok // 128):
                xt, xt_r = xts[ti % 2]
                hb, hb_r = hbs[ti % 2]
                st, st_r = sts[ti % 2]
                ti += 1
                rd = [src_res] if src_res is not None else []
                kb.dma(kb.sp, xt[:], src[t0 + j * 128:t0 + (j + 1) * 128, :], reads=rd, writes=[xt_r])
                kb.op(kb.act, lambda: nc.scalar.activation(out=junk[:], in_=xt[:], func=AF.Square,
                                                           accum_out=st[:, 0:1]),
                      reads=[xt_r], writes=[junk_r, st_r])
                kb.op(kb.dve, lambda: nc.vector.tensor_scalar(out=st[:, 1:2], in0=st[:, 0:1], scalar1=1.0 / D,
                                                              scalar2=EPS, op0=ALU.mult, op1=ALU.add),
                      reads=[st_r], writes=[st_r])
                kb.op(kb.act, lambda: nc.scalar.sqrt(out=st[:, 2:3], in_=st[:, 1:2]), reads=[st_r], writes=[st_r])
                kb.op(kb.dve, lambda: nc.vector.reciprocal(out=st[:, 3:4], in_=st[:, 2:3]), reads=[st_r], writes=[st_r])
                g1, g1_r = G[s]
                sh, sh_r = SH[s]
                kb.op(kb.dve, lambda: nc.vector.scalar_tensor_tensor(out=xt[:], in0=xt[:], scalar=st[:, 3:4], in1=g1[:],
                                                                     op0=ALU.mult, op1=ALU.mult),
                      reads=[xt_r, st_r, g1_r], writes=[xt_r])
                kb.op(kb.pool, lambda: nc.gpsimd.tensor_tensor(out=hb[:], in0=xt[:], in1=sh[:], op=ALU.add),
                      reads=[xt_r, sh_r], writes=[hb_r])
                for half in range(2):
                    ps, ps_r = kb.bank()
                    pb = ps[:].bitcast(BF16)
                    for kk in range(8):
                        k = half * 8 + kk
                        kb.op(kb.pe, lambda: nc.tensor.transpose(out=pb[:, kk * 128:(kk + 1) * 128],
                                                                 in_=hb[:, k * 128:(k + 1) * 128], identity=ident[:]),
                              reads=[hb_r, id_r], writes=[ps_r])
                    eng = self.evac_eng()
                    kb.op(eng, self.copy(eng, hT[:, half * 8:half * 8 + 8, j * 128:(j + 1) * 128],
                                         pb.rearrange("p (k t) -> p k t", t=128)),
                          reads=[ps_r], writes=[hT_r])
            kb.dma(kb.sp, dst[gidx, :, :, 0:ntok], hT[:, :, 0:ntok], reads=[hT_r], writes=[dst_res[gidx]])
        kb.end_phase()

    def make_ident(self, dtype):
        nc, kb = self.nc, self.kb
        idf, idf_r = kb.sbuf("idf", [128, 128], F32)
        kb.op(kb.pool, lambda: nc.gpsimd.memset(idf[:], 1.0), writes=[idf_r])
        kb.op(kb.pool, lambda: nc.gpsimd.affine_select(out=idf[:], in_=idf[:], pattern=[[-1, 128]],
                                                       compare_op=ALU.is_equal, fill=0.0, base=0,
                                                       channel_multiplier=1),
              reads=[idf_r], writes=[idf_r])
        if dtype == F32:
            return idf, idf_r
        idb, idb_r = kb.sbuf("idb", [128, 128], dtype)
        kb.op(kb.dve, lambda: nc.vector.tensor_copy(out=idb[:], in_=idf[:]), reads=[idf_r], writes=[idb_r])
        return idb, idb_r

    def phase_inproj(self):
        nc, kb = self.nc, self.kb
        kb.begin_phase()
        wv = self.w_in.rearrange("(k p) n -> p k n", p=128)
        wbdv = self.w_bd.rearrange("(k p) n -> p k n", p=128)
        own_groups = ([(0, 0, NCTX)] if self.need_ctx else []) + [(g, GROUPS[g][0], 512) for g in range(1, 9)]
        all_groups = [(g, GROUPS[g][0], GROUPS[g][1]) for g in range(len(GROUPS))]
        sets = [
            (C_AQ, 1024, PR_Q, own_groups),
            (C_AK, 256, PR_K, all_groups),
            (C_DQ, 1024, PR_DN, all_groups),
            (C_DV, 1024, PR_DN + 1024, all_groups),
            (C_LX, 1024, PR_LRU, all_groups),
        ]
        wss = [kb.sbuf("wset", [128, 16, 1024], BF16) for _ in range(2)]
        wvv, wvv_r = kb.sbuf("wv", [128, 16, 256], BF16)
        wbd, wbd_r = kb.sbuf("wbd", [128, 16, 16], BF16)
        kb.dma(kb.pool, wvv[:], wv[:, :, C_AV:C_AV + 256], writes=[wvv_r])
        kb.dma(kb.pool, wbd[:], wbdv, writes=[wbd_r])
        hTs = [kb.sbuf("hTl", [128, 16, 512], BF16) for _ in range(2)]
        evs = [kb.sbuf("ev", [128, 512], F32) for _ in range(4)]
        evb = [kb.sbuf("evb", [128, 256], BF16) for _ in range(2)]
        ei = 0
        hi = 0
        for si, (c0, ncols, r0, groups) in enumerate(sets):
            ws, ws_r = wss[si % 2]
            kb.dma(kb.pool, ws[:, :, 0:ncols], wv[:, :, c0:c0 + ncols], writes=[ws_r])
            for (g, t0, ntok) in groups:
                hT, hT_r = hTs[hi % 2]
                hi += 1
                kb.dma(kb.sp, hT[:, :, 0:ntok], self.hT_d[g, :, :, 0:ntok], reads=[self.r_hT[g]], writes=[hT_r])
                for ch in range(ncols // 128):
                    ps, ps_r = kb.bank()
                    for k in range(16):
                        kb.op(kb.pe, lambda: nc.tensor.matmul(ps[:, 0:ntok], lhsT=ws[:, k, ch * 128:(ch + 1) * 128],
                                                              rhs=hT[:, k, 0:ntok], start=(k == 0), stop=(k == 15)),
                              reads=[ws_r, hT_r], writes=[ps_r])
                    ev, ev_r = evs[ei % 4]
                    ei += 1
                    eng = self.evac_eng()
                    kb.op(eng, self.copy(eng, ev[:, 0:ntok], ps[:, 0:ntok]), reads=[ps_r], writes=[ev_r])
                    kb.dma(kb.sp, self.projT[r0 + ch * 128:r0 + (ch + 1) * 128, t0:t0 + ntok], ev[:, 0:ntok],
                           reads=[ev_r], mwrites=[self.r_projT])
                if c0 == C_AK:
                    ps, ps_r = kb.bank()
                    for k in range(16):
                        kb.op(kb.pe, lambda: nc.tensor.matmul(ps[0:16, 0:ntok], lhsT=wbd[:, k, :], rhs=hT[:, k, 0:ntok],
                                                              start=(k == 0), stop=(k == 15)),
                              reads=[wbd_r, hT_r], writes=[ps_r])
                    ev, ev_r = evs[ei % 4]
                    ei += 1
                    eng = self.evac_eng()
                    kb.op(eng, self.copy(eng, ev[0:16, 0:ntok], ps[0:16, 0:ntok]), reads=[ps_r], writes=[ev_r])
                    kb.dma(kb.sp, self.projT[PR_BD:PR_BD + 16, t0:t0 + ntok], ev[0:16, 0:ntok],
                           reads=[ev_r], mwrites=[self.r_projT])
                    for j in range(ntok // 128):
                        ps, ps_r = kb.bank()
                        for k in range(16):
                            kb.op(kb.pe, lambda: nc.tensor.matmul(ps[:, 0:256], lhsT=hT[:, k, j * 128:(j + 1) * 128],
                                                                  rhs=wvv[:, k, :], start=(k == 0), stop=(k == 15)),
                                  reads=[wvv_r, hT_r], writes=[ps_r])
                        eb, eb_r = evb[j % 2]
                        eng = self.evac_eng()
                        kb.op(eng, self.copy(eng, eb[:], ps[:, 0:256]), reads=[ps_r], writes=[eb_r])
                        kb.dma(kb.sp, self.vtok[t0 + j * 128:t0 + (j + 1) * 128, :], eb[:],
                               reads=[eb_r], mwrites=[self.r_vtok])
        kb.end_phase()


def present(a, half):
    return a if half == 0 else a[::-1]


def host_layer_inputs(inp, l, half=0):
    dsel = [0, 1] if half == 0 else [1, 0]
    m = {}
    m["w_mod"] = inp["w_mod"][l]
    m["b_mod"] = inp["b_mod"][l]
    m["gmix"] = inp["norm_mix_g"][l]
    m["gffn"] = inp["norm_ffn_g"][l]
    w_in = inp["w_in"][l]
    m["w_in"] = w_in
    pb = w_in[:, C_DB:C_DB + 8].reshape(D, 2, 4)[:, dsel, :].reshape(D, 8)
    pa = w_in[:, C_DA:C_DA + 8].reshape(D, 2, 4)[:, dsel, :].reshape(D, 8)
    m["w_bd"] = np.ascontiguousarray(np.concatenate([pb, pa], axis=1))
    m["sink"] = inp["attn_sink"][l]
    m["lru_cw"] = np.ascontiguousarray(inp["lru_conv_w"][l].T)
    m["lru_cb"] = np.ascontiguousarray(inp["lru_conv_b"][l].reshape(512, 1))
    wbd = np.zeros((2, 2, 4, 128, 128), np.float32)
    for d_ in range(2):
        for gt, nm in enumerate(("lru_w_rgate", "lru_w_igate")):
            w = inp[nm][l][dsel[d_]]
            for cc in range(4):
                wbd[d_, gt, cc, 0:64, 0:64] = w[2 * cc]
                wbd[d_, gt, cc, 64:128, 64:128] = w[2 * cc + 1]
    m["lru_wbd"] = wbd
    m["lru_bg"] = np.ascontiguousarray(np.stack([np.stack([inp["lru_b_rgate"][l][dsel[d_]], inp["lru_b_igate"][l][dsel[d_]]], 0)
                                                  for d_ in range(2)], 0).reshape(2, 2, 512, 1))
    m["lru_lam"] = np.ascontiguousarray(inp["lru_lambda"][l][dsel].reshape(2, 512, 1))
    m["dn_cw"] = np.ascontiguousarray(inp["dn_conv_w"][l].T)
    m["dn_ab"] = np.ascontiguousarray(np.concatenate([inp["dn_a_log"][l][dsel].reshape(-1), inp["dn_dt_bias"][l][dsel].reshape(-1)]))
    m["dn_ng"] = np.ascontiguousarray(inp["dn_norm_g"][l].reshape(128, 1))
    if "w_gu" in inp:
        m["w_out"] = inp["w_out"][l]
        m["w_router"] = inp["w_router"][l]
        m["b_router"] = inp["b_router"][l]
        m["w_gu"] = inp["w_gu"][l]
        m["b_guT"] = np.ascontiguousarray(inp["b_gu"][l].reshape(NE, 16, 128).transpose(0, 2, 1))
        m["w_down"] = inp["w_down"][l]
        m["b_down"] = inp["b_down"][l]
    return {f"l{l}_{k}": v for k, v in m.items()}


def host_shared_inputs(inp, b):
    m = {}
    m["xall"] = np.ascontiguousarray(np.concatenate([inp["ctx"][b], inp["x"][b]], 0), dtype=np.float32)
    m["c2"] = np.ascontiguousarray(np.stack([inp["c"][b], inp["c_ctx"]], 0))
    cos, sin, perm = rope_tables()
    m["ropec"], m["ropes"], m["permT"] = cos, sin, perm
    m["iota32"] = np.arange(NE, dtype=np.float32)
    m["iotap"] = np.arange(128, dtype=np.float32).reshape(128, 1)
    if "final_norm_g" in inp:
        m["final_g"] = inp["final_norm_g"]
    return m


def rope_tables():
    t = np.arange(NLAT)
    row = (t // 64).astype(np.float32)
    col = (t % 64).astype(np.float32)
    inv = (np.float32(10000.0) ** (-np.arange(0, 32, 2, dtype=np.float32) / np.float32(32))).astype(np.float32)
    cos = np.zeros((128, NLAT), np.float32)
    sin = np.zeros((128, NLAT), np.float32)
    perm = np.zeros((128, 128), np.float32)
    for m in range(128):
        dd = m % 64
        half, r = dd // 32, dd % 32
        j, second = r % 16, r // 16
        pos = row if half == 0 else col
        ang = (pos * inv[j]).astype(np.float32)
        cos[m] = np.cos(ang)
        sin[m] = np.sin(ang) if second else -np.sin(ang)
        partner = m - 16 if second else m + 16
        perm[partner, m] = 1.0
    return cos, sin, perm


def declare_attn(self):
    i = self.inp
    self.ropec = i("ropec", [128, NLAT])
    self.ropes = i("ropes", [128, NLAT])
    self.permT = i("permT", [128, 128])
    self.sink = i("sink", [16])
    kb = self.kb
    self.qr = kb.dram("qr", [1024, T], BF16)
    self.kr = kb.dram("kr", [256, T], BF16)
    self.mixT = kb.dram("mixT", [2048, T], BF16)
    self.r_qr, self.r_kr, self.r_mixT = kb.res("qr"), kb.res("kr"), kb.res("mixT")


def phase_rope(self):
    nc, kb = self.nc, self.kb
    kb.begin_phase()
    pm, pm_r = kb.sbuf("perm", [128, 128], BF16)
    kb.dma(kb.pool, pm[:], self.permT, writes=[pm_r])
    xs = [kb.sbuf("rx", [128, 512], F32) for _ in range(2)]
    xbs = [kb.sbuf("rxb", [128, 512], BF16) for _ in range(2)]
    cs = [kb.sbuf("rc", [128, 512], F32) for _ in range(2)]
    sn = [kb.sbuf("rs", [128, 512], F32) for _ in range(2)]
    t1s = [kb.sbuf("rt1", [128, 512], F32) for _ in range(2)]
    t2s = [kb.sbuf("rt2", [128, 512], F32) for _ in range(2)]
    obs = [kb.sbuf("rob", [128, 512], BF16) for _ in range(2)]
    it = 0
    for g in range(len(GROUPS)):
        t0, ntok = GROUPS[g]
        if g > 0:
            c_, c_r = cs[g % 2]
            s_, s_r = sn[g % 2]
            kb.dma(kb.sp, c_[:], self.ropec[:, t0 - NCTX:t0 - NCTX + 512], writes=[c_r])
            kb.dma(kb.sp, s_[:], self.ropes[:, t0 - NCTX:t0 - NCTX + 512], writes=[s_r])
        for (src0, dst, dst_r, nch) in ((PR_Q, self.qr, self.r_qr, 8), (PR_K, self.kr, self.r_kr, 2)):
            if g == 0 and src0 == PR_Q and not self.need_ctx:
                continue
            for ch in range(nch):
                x, x_r = xs[it % 2]
                xb, xb_r = xbs[it % 2]
                ob, ob_r = obs[it % 2]
                t1, t1_r = t1s[it % 2]
                t2, t2_r = t2s[it % 2]
                it += 1
                kb.dma(kb.act, x[:, 0:ntok], self.projT[src0 + ch * 128:src0 + (ch + 1) * 128, t0:t0 + ntok],
                       reads=[self.r_projT], writes=[x_r])
                if g == 0:
                    kb.op(kb.act, lambda: nc.scalar.copy(out=ob[:, 0:ntok], in_=x[:, 0:ntok]), reads=[x_r], writes=[ob_r])
                else:
                    kb.op(kb.act, lambda: nc.scalar.copy(out=xb[:], in_=x[:]), reads=[x_r], writes=[xb_r])
                    ps, ps_r = kb.bank()
                    kb.op(kb.pe, lambda: nc.tensor.matmul(ps[:], lhsT=pm[:], rhs=xb[:], start=True, stop=True),
                          reads=[pm_r, xb_r], writes=[ps_r])
                    kb.op(kb.pool, lambda: nc.gpsimd.tensor_tensor(out=t1[:], in0=x[:], in1=c_[:], op=ALU.mult),
                          reads=[x_r, c_r], writes=[t1_r])
                    kb.op(kb.dve, lambda: nc.vector.tensor_tensor(out=t2[:], in0=ps[:], in1=s_[:], op=ALU.mult),
                          reads=[ps_r, s_r], writes=[t2_r])
                    kb.op(kb.dve, lambda: nc.vector.tensor_tensor(out=ob[:], in0=t1[:], in1=t2[:], op=ALU.add),
                          reads=[t1_r, t2_r], writes=[ob_r])
                kb.dma(kb.sp, dst[ch * 128:(ch + 1) * 128, t0:t0 + ntok], ob[:, 0:ntok], reads=[ob_r], mwrites=[dst_r])
    kb.end_phase()


def phase_attn(self):
    nc, kb = self.nc, self.kb
    kb.begin_phase()
    ones, ones_r = kb.sbuf("ones", [128, 128], BF16)
    kb.op(kb.pool, lambda: nc.gpsimd.memset(ones[:], 1.0), writes=[ones_r])
    mf, mf_r = kb.sbuf("mf", [128, 128], F32)
    masks = []
    for (pat, cm) in (([[-1, 128]], 1), ([[1, 128]], -1)):
        mk, mk_r = kb.sbuf("mask", [128, 128], BF16)
        kb.op(kb.pool, lambda: nc.gpsimd.memset(mf[:], 1.0), writes=[mf_r])
        kb.op(kb.pool, lambda: nc.gpsimd.affine_select(out=mf[:], in_=mf[:], pattern=pat, compare_op=ALU.is_ge,
                                                       fill=0.0, base=0, channel_multiplier=cm),
              reads=[mf_r], writes=[mf_r])
        kb.op(kb.pool, lambda: nc.gpsimd.tensor_copy(out=mk[:], in_=mf[:]), reads=[mf_r], writes=[mk_r])
        masks.append((mk, mk_r))
    se, se_r = kb.sbuf("sinke", [128, 16], F32)
    kb.dma(kb.sp, se[:], self.sink.partition_broadcast(128), writes=[se_r])
    kb.op(kb.act, lambda: nc.scalar.activation(out=se[:], in_=se[:], func=AF.Exp), reads=[se_r], writes=[se_r])
    NT = T // 128
    V, V_r = kb.sbuf("V", [128, NT, 256], BF16)
    kb.dma(kb.sp, V[:], self.vtok.rearrange("(n p) c -> p n c", p=128), reads=[self.r_vtok], writes=[V_r])
    qT, qT_r = kb.sbuf("qT", [64, 4, T], BF16)
    kT, kT_r = kb.sbuf("kT", [64, T], BF16)
    Es = [kb.sbuf("E", [128, 512], BF16) for _ in range(10)]
    dens = [kb.sbuf("den", [64, 512], F32) for _ in range(2)]
    outs = [kb.sbuf("ao", [64, 512], BF16) for _ in range(2)]
    qv = self.qr.rearrange("(h d) t -> d h t", d=64)
    mv = self.mixT.rearrange("(h d) t -> d h t", d=64)
    ei = 0
    bi = 0
    qblocks = ([("c", 0), ("c", 1)] if self.need_ctx else []) + [("l", i) for i in range(NLAT // 128)]
    for g in range(4):
        kb.dma(kb.sp, qT[:], qv[:, 4 * g:4 * g + 4, :], reads=[self.r_qr], writes=[qT_r])
        kb.dma(kb.act, kT[:], self.kr[g * 64:(g + 1) * 64, :], reads=[self.r_kr], writes=[kT_r])
        for (kind, i) in qblocks:
            if kind == "c":
                tq = i * 128
                chunks = [(0, None), (128, None)]
            else:
                tq = NCTX + i * 128
                chunks = []
                if i > 0:
                    chunks.append((tq - 128, masks[0]))
                chunks.append((tq, None))
                if i < NLAT // 128 - 1:
                    chunks.append((tq + 128, masks[1]))
                chunks += [(0, None), (128, None)]
            Ec = []
            for (tk, mask) in chunks:
                ps, ps_r = kb.bank()
                kb.op(kb.pe, lambda: nc.tensor.matmul(ps[:].rearrange("p (h q) -> p h q", h=4),
                                                      lhsT=kT[:, tk:tk + 128], rhs=qT[:, :, tq:tq + 128],
                                                      start=True, stop=True),
                      reads=[kT_r, qT_r], writes=[ps_r])
                E, E_r = Es[ei % 10]
                ei += 1
                kb.op(kb.act, lambda: nc.scalar.activation(out=E[:], in_=ps[:], func=AF.Exp, scale=0.125),
                      reads=[ps_r], writes=[E_r])
                if mask is not None:
                    mk, mk_r = mask
                    kb.op(kb.pool, lambda: nc.gpsimd.tensor_tensor(
                        out=E[:].rearrange("p (h q) -> p h q", h=4), in0=E[:].rearrange("p (h q) -> p h q", h=4),
                        in1=mk[:].unsqueeze(1).to_broadcast([128, 4, 128]), op=ALU.mult),
                        reads=[E_r, mk_r], writes=[E_r])
                Ec.append((E, E_r, tk))
            psD, psD_r = kb.bank()
            for ci, (E, E_r, tk) in enumerate(Ec):
                kb.op(kb.pe, lambda: nc.tensor.matmul(psD[:], lhsT=ones[:], rhs=E[:], start=(ci == 0),
                                                      stop=(ci == len(Ec) - 1)),
                      reads=[ones_r, E_r], writes=[psD_r])
            den, den_r = dens[bi % 2]
            ao, ao_r = outs[bi % 2]
            bi += 1
            kb.op(kb.dve, lambda: nc.vector.tensor_tensor(
                out=den[:].rearrange("p (h q) -> p h q", h=4), in0=psD[0:64, :].rearrange("p (h q) -> p h q", h=4),
                in1=se[0:64, 4 * g:4 * g + 4].unsqueeze(2).to_broadcast([64, 4, 128]), op=ALU.add),
                reads=[psD_r, se_r], writes=[den_r])
            kb.op(kb.dve, lambda: nc.vector.reciprocal(out=den[:], in_=den[:]), reads=[den_r], writes=[den_r])
            psO, psO_r = kb.bank()
            for hh in range(4):
                for ci, (E, E_r, tk) in enumerate(Ec):
                    kb.op(kb.pe, lambda: nc.tensor.matmul(psO[0:64, hh * 128:(hh + 1) * 128],
                                                          lhsT=V[:, tk // 128, g * 64:(g + 1) * 64],
                                                          rhs=E[:, hh * 128:(hh + 1) * 128],
                                                          start=(ci == 0), stop=(ci == len(Ec) - 1)),
                          reads=[V_r, E_r], writes=[psO_r])
            kb.op(kb.dve, lambda: nc.vector.tensor_tensor(out=ao[:], in0=psO[0:64, :], in1=den[:], op=ALU.mult),
                  reads=[psO_r, den_r], writes=[ao_r])
            kb.dma(kb.sp, mv[:, 4 * g:4 * g + 4, tq:tq + 128], ao[:].rearrange("p (h q) -> p h q", h=4),
                   reads=[ao_r], mwrites=[self.r_mixT])
    kb.end_phase()


Layer.declare_attn = declare_attn
Layer.phase_rope = phase_rope
Layer.phase_attn = phase_attn


def declare_lru(self):
    i = self.inp
    self.lru_cw = i("lru_cw", [512, 4])
    self.lru_cb = i("lru_cb", [512, 1])
    self.lru_wbd = i("lru_wbd", [2, 2, 4, 128, 128])
    self.lru_bg = i("lru_bg", [2, 2, 512, 1])
    self.lru_lam = i("lru_lam", [2, 512, 1])


def gelu_tanh(self, out, x, tmp, x_r, tmp_r, out_r):
    nc, kb = self.nc, self.kb
    kb.op(kb.pool, lambda: nc.gpsimd.tensor_tensor(out=tmp, in0=x, in1=x, op=ALU.mult), reads=[x_r], writes=[tmp_r])
    kb.op(kb.dve, lambda: nc.vector.tensor_scalar(out=tmp, in0=tmp, scalar1=0.044715, scalar2=1.0, op0=ALU.mult,
                                                  op1=ALU.add), reads=[tmp_r], writes=[tmp_r])
    kb.op(kb.dve, lambda: nc.vector.tensor_tensor(out=tmp, in0=tmp, in1=x, op=ALU.mult), reads=[tmp_r, x_r],
          writes=[tmp_r])
    kb.op(kb.act, lambda: nc.scalar.activation(out=tmp, in_=tmp, func=AF.Sigmoid, scale=1.5957691216),
          reads=[tmp_r], writes=[tmp_r])
    kb.op(kb.dve, lambda: nc.vector.tensor_tensor(out=out, in0=tmp, in1=x, op=ALU.mult), reads=[tmp_r, x_r],
          writes=[out_r])


def phase_lru(self):
    nc, kb = self.nc, self.kb
    kb.begin_phase()
    W = T + 8
    CX, LX = 2, 2 + NCTX + 4
    Xp, Xp_r = kb.sbuf("lXp", [128, W], F32)
    U, U_r = kb.sbuf("lU", [128, T], F32)
    Rt, R_r = kb.sbuf("lR", [128, T], F32)
    It, I_r = kb.sbuf("lI", [128, T], F32)
    HF, HF_r = kb.sbuf("lHF", [128, T], F32)
    HB, HB_r = kb.sbuf("lHB", [128, T], F32)
    ob, ob_r = kb.sbuf("lob", [128, T], BF16)
    cw, cw_r = kb.sbuf("lcw", [128, 4], F32)
    cb, cb_r = kb.sbuf("lcb", [128, 1], F32)
    wbd, wbd_r = kb.sbuf("lwbd", [128, 4, 128], F32)
    bg, bg_r = kb.sbuf("lbg", [128, 4], F32)
    lam, lam_r = kb.sbuf("llam", [128, 4], F32)
    kb.op(kb.pool, lambda: nc.gpsimd.memset(Xp[:], 0.0), writes=[Xp_r])
    segs = [(0, NCTX, CX), (NCTX, NLAT, LX)]
    for cc in range(4):
        ch = slice(cc * 128, (cc + 1) * 128)
        kb.dma(kb.sp, cw[:], self.lru_cw[ch, :], writes=[cw_r])
        kb.dma(kb.sp, cb[:], self.lru_cb[ch, :], writes=[cb_r])
        for d in range(2):
            for gt in range(2):
                kb.dma(kb.sp, wbd[:, d * 2 + gt, :], self.lru_wbd[d, gt, cc], writes=[wbd_r])
                kb.dma(kb.sp, bg[:, d * 2 + gt:d * 2 + gt + 1], self.lru_bg[d, gt, ch, :], writes=[bg_r])
            kb.dma(kb.sp, lam[:, d:d + 1], self.lru_lam[d, ch, :], writes=[lam_r])
        kb.op(kb.act, lambda: nc.scalar.activation(out=lam[:, 2:4], in_=lam[:, 0:2], func=AF.Exp, scale=-1.0),
              reads=[lam_r], writes=[lam_r])
        kb.op(kb.act, lambda: nc.scalar.activation(out=lam[:, 2:4], in_=lam[:, 2:4], func=AF.Ln, bias=1.0),
              reads=[lam_r], writes=[lam_r])
        kb.op(kb.dve, lambda: nc.vector.tensor_scalar(out=lam[:, 2:4], in0=lam[:, 2:4], scalar1=-8.0, scalar2=None,
                                                      op0=ALU.mult), reads=[lam_r], writes=[lam_r])
        for (a0, a1) in ((0, CX), (CX + NCTX, LX), (LX + NLAT, W)):
            kb.op(kb.pool, lambda: nc.gpsimd.memset(Xp[:, a0:a1], 0.0), reads=[Xp_r], writes=[Xp_r])
        for (t0, n, p0) in segs:
            kb.dma(kb.sp, Xp[:, p0:p0 + n], self.projT[PR_LRU + cc * 128:PR_LRU + (cc + 1) * 128, t0:t0 + n],
                   reads=[self.r_projT], writes=[Xp_r])
        for (t0, n, p0) in segs:
            kb.op(kb.dve, lambda: nc.vector.tensor_scalar(out=U[:, t0:t0 + n], in0=Xp[:, p0 - 2:p0 - 2 + n],
                                                          scalar1=cw[:, 0:1], scalar2=cb[:, 0:1], op0=ALU.mult,
                                                          op1=ALU.add), reads=[Xp_r, cw_r, cb_r], writes=[U_r])
            for o in range(1, 4):
                kb.op(kb.dve, lambda: nc.vector.scalar_tensor_tensor(out=U[:, t0:t0 + n],
                                                                     in0=Xp[:, p0 - 2 + o:p0 - 2 + o + n],
                                                                     scalar=cw[:, o:o + 1], in1=U[:, t0:t0 + n],
                                                                     op0=ALU.mult, op1=ALU.add),
                      reads=[Xp_r, cw_r, U_r], writes=[U_r])
        Y = Xp[:, 0:T]
        kb.dma(kb.sp, Y, self.projT[PR_LRU + 512 + cc * 128:PR_LRU + 512 + (cc + 1) * 128, :],
               reads=[self.r_projT, U_r], writes=[Xp_r])
        for d in range(2):
            H, H_r = (HF, HF_r) if d == 0 else (HB, HB_r)
            for gt, (dst, dst_r) in enumerate(((Rt, R_r), (It, I_r))):
                for (t0, ntok) in GROUPS:
                    ps, ps_r = kb.bank()
                    kb.op(kb.pe, lambda: nc.tensor.matmul(ps[:, 0:ntok], lhsT=wbd[:, d * 2 + gt, :], rhs=U[:, t0:t0 + ntok],
                                                          start=True, stop=True), reads=[wbd_r, U_r], writes=[ps_r])
                    kb.op(kb.act, lambda: nc.scalar.activation(out=dst[:, t0:t0 + ntok], in_=ps[:, 0:ntok],
                                                               func=AF.Sigmoid, bias=bg[:, d * 2 + gt:d * 2 + gt + 1]),
                          reads=[ps_r, bg_r], writes=[dst_r])
            kb.op(kb.act, lambda: nc.scalar.activation(out=Rt[:], in_=Rt[:], func=AF.Exp, scale=lam[:, 2 + d:3 + d]),
                  reads=[R_r, lam_r], writes=[R_r])
            tmp, tmp_r = HB, HB_r
            kb.op(kb.pool, lambda: nc.gpsimd.tensor_tensor(out=tmp[:], in0=Rt[:], in1=Rt[:], op=ALU.mult),
                  reads=[R_r], writes=[tmp_r])
            kb.op(kb.dve, lambda: nc.vector.tensor_scalar(out=tmp[:], in0=tmp[:], scalar1=1.0, scalar2=None, op0=ALU.min),
                  reads=[tmp_r], writes=[tmp_r])
            kb.op(kb.act, lambda: nc.scalar.activation(out=tmp[:], in_=tmp[:], func=AF.Sqrt, scale=-1.0, bias=1.0),
                  reads=[tmp_r], writes=[tmp_r])
            kb.op(kb.dve, lambda: nc.vector.tensor_tensor(out=It[:], in0=It[:], in1=U[:], op=ALU.mult),
                  reads=[I_r, U_r], writes=[I_r])
            kb.op(kb.dve, lambda: nc.vector.tensor_tensor(out=It[:], in0=It[:], in1=tmp[:], op=ALU.mult),
                  reads=[I_r, tmp_r], writes=[I_r])
            if d == 0:
                kb.op(kb.dve, lambda: nc.vector.tensor_tensor_scan(out=H[:, 0:NCTX], data0=Rt[:, 0:NCTX], data1=It[:, 0:NCTX],
                                                                   initial=0.0, op0=ALU.mult, op1=ALU.add),
                      reads=[R_r, I_r], writes=[H_r])
                kb.op(kb.dve, lambda: nc.vector.tensor_tensor_scan(out=H[:, NCTX:T], data0=Rt[:, NCTX:T], data1=It[:, NCTX:T],
                                                                   initial=H[:, NCTX - 1:NCTX], op0=ALU.mult, op1=ALU.add),
                      reads=[R_r, I_r, H_r], writes=[H_r])
            else:
                kb.op(kb.dve, lambda: nc.vector.tensor_tensor_scan(out=H[:, 0:NCTX][:, ::-1],
                                                                   data0=Rt[:, 0:NCTX][:, ::-1], data1=It[:, 0:NCTX][:, ::-1],
                                                                   initial=0.0, op0=ALU.mult, op1=ALU.add),
                      reads=[R_r, I_r], writes=[H_r])
                kb.op(kb.dve, lambda: nc.vector.tensor_tensor_scan(out=H[:, NCTX:T][:, ::-1], data0=Rt[:, NCTX:T][:, ::-1],
                                                                   data1=It[:, NCTX:T][:, ::-1], initial=H[:, 0:1],
                                                                   op0=ALU.mult, op1=ALU.add),
                      reads=[R_r, I_r, H_r], writes=[H_r])
        kb.op(kb.pool, lambda: nc.gpsimd.tensor_tensor(out=HF[:], in0=HF[:], in1=HB[:], op=ALU.add),
              reads=[HF_r, HB_r], writes=[HF_r])
        gelu_tanh(self, HB[:], Y, Rt[:], Xp_r, R_r, HB_r)
        kb.op(kb.dve, lambda: nc.vector.tensor_tensor(out=ob[:], in0=HF[:], in1=HB[:], op=ALU.mult),
              reads=[HF_r, HB_r], writes=[ob_r])
        kb.dma(kb.sp, self.mixT[1536 + cc * 128:1536 + (cc + 1) * 128, :], ob[:], reads=[ob_r], mwrites=[self.r_mixT])
    kb.end_phase()


Layer.declare_lru = declare_lru
Layer.phase_lru = phase_lru


def declare_dn(self):
    i = self.inp
    self.dn_cw = i("dn_cw", [1536, 4])
    self.dn_ab = i("dn_ab", [16])
    self.dn_ng = i("dn_ng", [128, 1])


def phase_dn(self):
    nc, kb = self.nc, self.kb
    kb.begin_phase()
    NCH = T // 128
    W = T + 8
    CX, LX = 2, 2 + NCTX + 4
    segs = [(0, NCTX, CX), (NCTX, NLAT, LX)]
    identF, id_r = self.make_ident(F32)
    onesF, ones_r = kb.sbuf("dones", [128, 128], F32)
    kb.op(kb.pool, lambda: nc.gpsimd.memset(onesF[:], 1.0), writes=[ones_r])
    bd64, bd_r = kb.sbuf("bd64", [128, 128], F32)
    kb.op(kb.pool, lambda: nc.gpsimd.memset(bd64[:], 0.0), writes=[bd_r])
    kb.op(kb.pool, lambda: nc.gpsimd.memset(bd64[0:64, 0:64], 1.0), reads=[bd_r], writes=[bd_r])
    kb.op(kb.pool, lambda: nc.gpsimd.memset(bd64[64:128, 64:128], 1.0), reads=[bd_r], writes=[bd_r])
    MASKS = []
    for d in range(2):
        pat, cm = ([[1, 128]], -1) if d == 0 else ([[-1, 128]], 1)
        mi, mi_r = kb.sbuf("mincl", [128, 128], F32)
        msd, msd_r = kb.sbuf("msd", [128, 128], F32)
        mso, mso_r = kb.sbuf("mso", [128, 128], F32)
        kb.op(kb.pool, lambda: nc.gpsimd.memset(mi[:], 1.0), writes=[mi_r])
        kb.op(kb.pool, lambda: nc.gpsimd.affine_select(out=mi[:], in_=mi[:], pattern=pat, compare_op=ALU.is_ge, fill=0.0,
                                                       base=0, channel_multiplier=cm), reads=[mi_r], writes=[mi_r])
        kb.op(kb.pool, lambda: nc.gpsimd.memset(mso[:], 1.0), writes=[mso_r])
        kb.op(kb.pool, lambda: nc.gpsimd.affine_select(out=mso[:], in_=mso[:], pattern=pat, compare_op=ALU.is_gt, fill=0.0,
                                                       base=0, channel_multiplier=cm), reads=[mso_r], writes=[mso_r])
        kb.op(kb.pool, lambda: nc.gpsimd.tensor_tensor(out=msd[:], in0=mso[:], in1=bd64[:], op=ALU.mult),
              reads=[mso_r, bd_r], writes=[msd_r])
        kb.op(kb.pool, lambda: nc.gpsimd.tensor_tensor(out=mso[:], in0=mso[:], in1=msd[:], op=ALU.subtract),
              reads=[mso_r, msd_r], writes=[mso_r])
        MASKS.append(((mi, mi_r), (msd, msd_r), (mso, mso_r)))
    ab, ab_r = kb.sbuf("dnab", [128, 32], F32)
    kb.dma(kb.sp, ab[:, 0:16], self.dn_ab.partition_broadcast(128), writes=[ab_r])
    kb.op(kb.act, lambda: nc.scalar.activation(out=ab[:, 16:24], in_=ab[:, 0:8], func=AF.Exp), reads=[ab_r], writes=[ab_r])
    kb.op(kb.dve, lambda: nc.vector.tensor_scalar(out=ab[:, 16:24], in0=ab[:, 16:24], scalar1=-1.0, scalar2=None,
                                                  op0=ALU.mult), reads=[ab_r], writes=[ab_r])
    ng, ng_r = kb.sbuf("dnng", [128, 1], F32)
    kb.dma(kb.sp, ng[:], self.dn_ng, writes=[ng_r])
    cw, cw_r = kb.sbuf("dncw", [128, 4], F32)
    Xp, Xp_r = kb.sbuf("dXp", [128, W], F32)
    kb.op(kb.pool, lambda: nc.gpsimd.memset(Xp[:], 0.0), writes=[Xp_r])
    QKV = [kb.sbuf("dq", [128, T], F32), kb.sbuf("dk", [128, T], F32), kb.sbuf("dv", [128, T], F32)]
    G0, G0_r = kb.sbuf("dG0", [128, T], F32)
    GC, GC_r = kb.sbuf("dGC", [128, T], F32)
    OT, OT_r = kb.sbuf("dOT", [128, T], F32)
    cols, cols_r = kb.sbuf("dcols", [128, 6, NCH], F32)
    junk, junk_r = kb.sbuf("djunk", [128, 128], F32)
    NP = 56
    pool_t = [kb.sbuf("dt", [128, 128], F32) for _ in range(NP)]
    pi = [0]

    def tl():
        t = pool_t[pi[0] % NP]
        pi[0] += 1
        return t
    Ss = [kb.sbuf("dS", [128, 128], F32) for _ in range(2)]
    ev_i = [0]

    def evac(out, in_, reads, writes):
        ev_i[0] += 1
        if ev_i[0] % 2:
            kb.op(kb.act, lambda: nc.scalar.copy(out=out, in_=in_), reads=reads, writes=writes)
        else:
            kb.op(kb.dve, lambda: nc.vector.tensor_copy(out=out, in_=in_), reads=reads, writes=writes)

    for h in range(4):
        for comp in range(3):
            r0 = PR_DN + comp * 512 + h * 128
            X, X_r = QKV[comp]
            kb.dma(kb.sp, cw[:], self.dn_cw[comp * 512 + h * 128:comp * 512 + (h + 1) * 128, :], writes=[cw_r])
            for (t0, n, p0) in segs:
                kb.dma(kb.sp, Xp[:, p0:p0 + n], self.projT[r0:r0 + 128, t0:t0 + n], reads=[self.r_projT], writes=[Xp_r])
            for (t0, n, p0) in segs:
                kb.op(kb.dve, lambda: nc.vector.tensor_scalar(out=X[:, t0:t0 + n], in0=Xp[:, p0 - 2:p0 - 2 + n],
                                                              scalar1=cw[:, 0:1], scalar2=None, op0=ALU.mult),
                      reads=[Xp_r, cw_r], writes=[X_r])
                for o in range(1, 4):
                    kb.op(kb.dve, lambda: nc.vector.scalar_tensor_tensor(out=X[:, t0:t0 + n],
                                                                         in0=Xp[:, p0 - 2 + o:p0 - 2 + o + n],
                                                                         scalar=cw[:, o:o + 1], in1=X[:, t0:t0 + n],
                                                                         op0=ALU.mult, op1=ALU.add),
                          reads=[Xp_r, cw_r, X_r], writes=[X_r])
            kb.op(kb.act, lambda: nc.scalar.activation(out=X[:], in_=X[:], func=AF.Silu), reads=[X_r], writes=[X_r])
            if comp < 2:
                for (t0, ntok) in GROUPS:
                    kb.op(kb.act, lambda: nc.scalar.activation(out=G0[:, t0:t0 + ntok], in_=X[:, t0:t0 + ntok], func=AF.Square),
                          reads=[X_r], writes=[G0_r])
                    ps, ps_r = kb.bank()
                    kb.op(kb.pe, lambda: nc.tensor.matmul(ps[:, 0:ntok], lhsT=onesF[:], rhs=G0[:, t0:t0 + ntok], start=True,
                                                          stop=True), reads=[ones_r, G0_r], writes=[ps_r])
                    kb.op(kb.dve, lambda: nc.vector.tensor_scalar(out=GC[:, t0:t0 + ntok], in0=ps[:, 0:ntok], scalar1=EPS,
                                                                  scalar2=None, op0=ALU.add), reads=[ps_r], writes=[GC_r])
                kb.op(kb.act, lambda: nc.scalar.sqrt(out=GC[:], in_=GC[:]), reads=[GC_r], writes=[GC_r])
                kb.op(kb.dve, lambda: nc.vector.reciprocal(out=GC[:], in_=GC[:]), reads=[GC_r], writes=[GC_r])
                sc = 128.0 ** -0.5 if comp == 0 else 1.0
                kb.op(kb.dve, lambda: nc.vector.scalar_tensor_tensor(out=X[:], in0=X[:], scalar=sc, in1=GC[:], op0=ALU.mult,
                                                                     op1=ALU.mult), reads=[X_r, GC_r], writes=[X_r])
        (qT, q_r), (kT, k_r), (vT, v_r) = QKV
        kb.op(kb.pool, lambda: nc.gpsimd.memset(OT[:], 0.0), writes=[OT_r])
        BB, BB_r = Xp[:, 0:T], Xp_r
        for d in range(2):
            (mi, mi_r), (msd, msd_r), (mso, mso_r) = MASKS[d]
            kb.dma(kb.sp, G0[:], self.projT[PR_BD + 8 + d * 4 + h, :].partition_broadcast(128), reads=[self.r_projT],
                   writes=[G0_r])
            kb.dma(kb.sp, BB, self.projT[PR_BD + d * 4 + h, :].partition_broadcast(128), reads=[self.r_projT],
                   writes=[BB_r])
            kb.op(kb.act, lambda: nc.scalar.activation(out=BB, in_=BB, func=AF.Sigmoid), reads=[BB_r], writes=[BB_r])
            dtb = ab[:, 8 + d * 4 + h:8 + d * 4 + h + 1]
            negA = ab[:, 16 + d * 4 + h:16 + d * 4 + h + 1]
            kb.op(kb.dve, lambda: nc.vector.tensor_scalar(out=G0[:], in0=G0[:], scalar1=dtb, scalar2=None, op0=ALU.add),
                  reads=[G0_r, ab_r], writes=[G0_r])
            kb.op(kb.act, lambda: nc.scalar.activation(out=GC[:], in_=G0[:], func=AF.Abs), reads=[G0_r], writes=[GC_r])
            kb.op(kb.act, lambda: nc.scalar.activation(out=GC[:], in_=GC[:], func=AF.Exp, scale=-1.0), reads=[GC_r], writes=[GC_r])
            kb.op(kb.act, lambda: nc.scalar.activation(out=GC[:], in_=GC[:], func=AF.Ln, bias=1.0), reads=[GC_r], writes=[GC_r])
            kb.op(kb.dve, lambda: nc.vector.tensor_scalar(out=G0[:], in0=G0[:], scalar1=0.0, scalar2=None, op0=ALU.max),
                  reads=[G0_r], writes=[G0_r])
            kb.op(kb.dve, lambda: nc.vector.tensor_tensor(out=G0[:], in0=G0[:], in1=GC[:], op=ALU.add),
                  reads=[G0_r, GC_r], writes=[G0_r])
            kb.op(kb.dve, lambda: nc.vector.tensor_scalar(out=G0[:], in0=G0[:], scalar1=negA, scalar2=None, op0=ALU.mult),
                  reads=[G0_r, ab_r], writes=[G0_r])
            for c in range(NCH):
                sl = slice(c * 128, (c + 1) * 128)
                if d == 0:
                    kb.op(kb.dve, lambda: nc.vector.tensor_tensor_scan(out=GC[:, sl], data0=onesF[:], data1=G0[:, sl],
                                                                       initial=0.0, op0=ALU.mult, op1=ALU.add),
                          reads=[G0_r, ones_r], writes=[GC_r])
                else:
                    kb.op(kb.dve, lambda: nc.vector.tensor_tensor_scan(out=GC[:, sl][:, ::-1], data0=onesF[:],
                                                                       data1=G0[:, sl][:, ::-1], initial=0.0,
                                                                       op0=ALU.mult, op1=ALU.add),
                          reads=[G0_r, ones_r], writes=[GC_r])
                kb.op(kb.dve, lambda: nc.vector.scalar_tensor_tensor(out=junk[:], in0=GC[:, sl], scalar=1.0, in1=identF[:],
                                                                     op0=ALU.mult, op1=ALU.mult, accum_out=cols[:, 0, c:c + 1]),
                      reads=[GC_r, id_r], writes=[junk_r, cols_r])
                kb.op(kb.dve, lambda: nc.vector.scalar_tensor_tensor(out=junk[:], in0=BB[:, sl], scalar=1.0, in1=identF[:],
                                                                     op0=ALU.mult, op1=ALU.mult, accum_out=cols[:, 1, c:c + 1]),
                      reads=[BB_r, id_r], writes=[junk_r, cols_r])
            lastoff = 127 if d == 0 else 0
            kb.op(kb.dve, lambda: nc.vector.tensor_copy(out=cols[:, 2, :], in_=GC[:, lastoff::128]), reads=[GC_r], writes=[cols_r])
            kb.op(kb.act, lambda: nc.scalar.activation(out=cols[:, 3, :], in_=cols[:, 2, :], func=AF.Exp), reads=[cols_r],
                  writes=[cols_r])
            kb.op(kb.dve, lambda: nc.vector.tensor_tensor(out=cols[:, 4, :], in0=cols[:, 2, :], in1=cols[:, 0, :], op=ALU.subtract),
                  reads=[cols_r], writes=[cols_r])
            kb.op(kb.act, lambda: nc.scalar.activation(out=cols[:, 4, :], in_=cols[:, 4, :], func=AF.Exp), reads=[cols_r],
                  writes=[cols_r])
            kb.op(kb.dve, lambda: nc.vector.tensor_scalar(out=cols[:, 5, :], in0=cols[:, 1, :], scalar1=-1.0, scalar2=None,
                                                          op0=ALU.mult), reads=[cols_r], writes=[cols_r])
            S, S_r = Ss[0]
            kb.op(kb.pool, lambda: nc.gpsimd.memset(S[:], 0.0), writes=[S_r])
            si = 0
            order = list(range(NCH)) if d == 0 else [1, 0] + list(range(NCH - 1, 1, -1))
            for c in order:
                sl = slice(c * 128, (c + 1) * 128)
                gcol, bcol, egl, dcol, nbcol = (cols[:, 0, c:c + 1], cols[:, 1, c:c + 1], cols[:, 3, c:c + 1],
                                                cols[:, 4, c:c + 1], cols[:, 5, c:c + 1])
                eg, eg_r = tl()
                kb.op(kb.act, lambda: nc.scalar.activation(out=eg[:], in_=GC[:, sl], func=AF.Exp), reads=[GC_r], writes=[eg_r])
                kgT, kg_r = tl()
                qgT, qg_r = tl()
                kb.op(kb.dve, lambda: nc.vector.tensor_tensor(out=kgT[:], in0=kT[:, sl], in1=eg[:], op=ALU.mult),
                      reads=[k_r, eg_r], writes=[kg_r])
                kb.op(kb.pool, lambda: nc.gpsimd.tensor_tensor(out=qgT[:], in0=qT[:, sl], in1=eg[:], op=ALU.mult),
                      reads=[q_r, eg_r], writes=[qg_r])
                kd, kd_r = tl()
                vb, vb_r = tl()
                ps, ps_r = kb.bank()
                kb.op(kb.pe, lambda: nc.tensor.transpose(out=ps[:, 0:128], in_=kT[:, sl], identity=identF[:]),
                      reads=[k_r, id_r], writes=[ps_r])
                kb.op(kb.act, lambda: nc.scalar.mul(out=kd[:], in_=ps[:, 0:128], mul=dcol), reads=[ps_r, cols_r], writes=[kd_r])
                ps, ps_r = kb.bank()
                kb.op(kb.pe, lambda: nc.tensor.transpose(out=ps[:, 0:128], in_=vT[:, sl], identity=identF[:]),
                      reads=[v_r, id_r], writes=[ps_r])
                kb.op(kb.act, lambda: nc.scalar.mul(out=vb[:], in_=ps[:, 0:128], mul=bcol), reads=[ps_r, cols_r], writes=[vb_r])
                Dm, Dm_r = tl()
                kb.op(kb.dve, lambda: nc.vector.tensor_scalar(out=Dm[:], in0=GC[:, sl], scalar1=gcol, scalar2=0.0,
                                                              op0=ALU.subtract, op1=ALU.min), reads=[GC_r, cols_r], writes=[Dm_r])
                kb.op(kb.act, lambda: nc.scalar.activation(out=Dm[:], in_=Dm[:], func=AF.Exp), reads=[Dm_r], writes=[Dm_r])
                Dmi, Dmi_r = tl()
                kb.op(kb.pool, lambda: nc.gpsimd.tensor_tensor(out=Dmi[:], in0=Dm[:], in1=mi[:], op=ALU.mult),
                      reads=[Dm_r, mi_r], writes=[Dmi_r])
                bD, bD_r = tl()
                kb.op(kb.pool, lambda: nc.gpsimd.tensor_tensor(out=bD[:], in0=Dm[:], in1=BB[:, sl], op=ALU.mult),
                      reads=[Dm_r, BB_r], writes=[bD_r])
                wd, wd_r = tl()
                wo, wo_r = tl()
                kb.op(kb.pool, lambda: nc.gpsimd.tensor_tensor(out=wd[:], in0=bD[:], in1=msd[:], op=ALU.mult),
                      reads=[bD_r, msd_r], writes=[wd_r])
                kb.op(kb.pool, lambda: nc.gpsimd.tensor_tensor(out=wo[:], in0=bD[:], in1=mso[:], op=ALU.mult),
                      reads=[bD_r, mso_r], writes=[wo_r])
                psKK, psKK_r = kb.bank()
                kb.op(kb.pe, lambda: nc.tensor.matmul(psKK[:, 0:128], lhsT=kT[:, sl], rhs=kT[:, sl], start=True, stop=True),
                      reads=[k_r], writes=[psKK_r])
                psQK, psQK_r = kb.bank()
                kb.op(kb.pe, lambda: nc.tensor.matmul(psQK[:, 0:128], lhsT=kT[:, sl], rhs=qT[:, sl], start=True, stop=True),
                      reads=[k_r, q_r], writes=[psQK_r])
                MT, MT_r = tl()
                kb.op(kb.dve, lambda: nc.vector.tensor_tensor(out=MT[:], in0=psQK[:, 0:128], in1=Dmi[:], op=ALU.mult),
                      reads=[psQK_r, Dmi_r], writes=[MT_r])
                Bd, Bd_r = tl()
                Bo, Bo_r = tl()
                kb.op(kb.dve, lambda: nc.vector.tensor_tensor(out=Bd[:], in0=psKK[:, 0:128], in1=wd[:], op=ALU.mult),
                      reads=[psKK_r, wd_r], writes=[Bd_r])
                kb.op(kb.dve, lambda: nc.vector.tensor_tensor(out=Bo[:], in0=psKK[:, 0:128], in1=wo[:], op=ALU.mult),
                      reads=[psKK_r, wo_r], writes=[Bo_r])
                Ad, Ad_r = tl()
                Ao, Ao_r = tl()
                for (src, src_r, dst, dst_r) in ((Bd, Bd_r, Ad, Ad_r), (Bo, Bo_r, Ao, Ao_r)):
                    ps, ps_r = kb.bank()
                    kb.op(kb.pe, lambda: nc.tensor.transpose(out=ps[:, 0:128], in_=src[:], identity=identF[:]),
                          reads=[src_r, id_r], writes=[ps_r])
                    evac(dst[:], ps[:, 0:128], [ps_r], [dst_r])
                PT, PT_r = tl()
                kb.op(kb.dve, lambda: nc.vector.tensor_tensor(out=PT[:], in0=identF[:], in1=Bd[:], op=ALU.subtract),
                      reads=[id_r, Bd_r], writes=[PT_r])
                Ak, Ak_r, Bk, Bk_r = Ad, Ad_r, Bd, Bd_r
                for lvl in range(5):
                    A2, A2_r = tl()
                    ps, ps_r = kb.bank()
                    kb.op(kb.pe, lambda: nc.tensor.matmul(ps[:, 0:128], lhsT=Bk[:], rhs=Ak[:], start=True, stop=True),
                          reads=[Bk_r, Ak_r], writes=[ps_r])
                    evac(A2[:], ps[:, 0:128], [ps_r], [A2_r])
                    if lvl < 4:
                        B2, B2_r = tl()
                        ps, ps_r = kb.bank()
                        kb.op(kb.pe, lambda: nc.tensor.matmul(ps[:, 0:128], lhsT=Ak[:], rhs=Bk[:], start=True, stop=True),
                              reads=[Bk_r, Ak_r], writes=[ps_r])
                        evac(B2[:], ps[:, 0:128], [ps_r], [B2_r])
                    ps, ps_r = kb.bank()
                    kb.op(kb.pe, lambda: nc.tensor.matmul(ps[:, 0:128], lhsT=A2[:], rhs=PT[:], start=True, stop=True),
                          reads=[A2_r, PT_r], writes=[ps_r])
                    PT2, PT2_r = tl()
                    kb.op(kb.dve, lambda: nc.vector.tensor_tensor(out=PT2[:], in0=PT[:], in1=ps[:, 0:128], op=ALU.add),
                          reads=[PT_r, ps_r], writes=[PT2_r])
                    PT, PT_r = PT2, PT2_r
                    Ak, Ak_r = A2, A2_r
                    if lvl < 4:
                        Bk, Bk_r = B2, B2_r
                T64, T64_r = tl()
                ps, ps_r = kb.bank()
                kb.op(kb.pe, lambda: nc.tensor.transpose(out=ps[:, 0:128], in_=PT[:], identity=identF[:]),
                      reads=[PT_r, id_r], writes=[ps_r])
                evac(T64[:], ps[:, 0:128], [ps_r], [T64_r])
                Y, Y_r = tl()
                ps, ps_r = kb.bank()
                kb.op(kb.pe, lambda: nc.tensor.matmul(ps[:, 0:128], lhsT=Ao[:], rhs=PT[:], start=True, stop=True),
                      reads=[Ao_r, PT_r], writes=[ps_r])
                evac(Y[:], ps[:, 0:128], [ps_r], [Y_r])
                ps, ps_r = kb.bank()
                kb.op(kb.pe, lambda: nc.tensor.matmul(ps[:, 0:128], lhsT=T64[:], rhs=Y[:], start=True, stop=True),
                      reads=[T64_r, Y_r], writes=[ps_r])
                TT, TT_r = tl()
                kb.op(kb.dve, lambda: nc.vector.tensor_tensor(out=TT[:], in0=PT[:], in1=ps[:, 0:128], op=ALU.subtract),
                      reads=[PT_r, ps_r], writes=[TT_r])
                S, S_r = Ss[si % 2]
                Sn, Sn_r = Ss[(si + 1) % 2]
                si += 1
                ps, ps_r = kb.bank()
                kb.op(kb.pe, lambda: nc.tensor.matmul(ps[:, 0:128], lhsT=kgT[:], rhs=S[:], start=True, stop=True),
                      reads=[kg_r, S_r], writes=[ps_r])
                R, R_r = tl()
                kb.op(kb.dve, lambda: nc.vector.scalar_tensor_tensor(out=R[:], in0=ps[:, 0:128], scalar=nbcol, in1=vb[:],
                                                                     op0=ALU.mult, op1=ALU.add),
                      reads=[ps_r, cols_r, vb_r], writes=[R_r])
                ps, ps_r = kb.bank()
                kb.op(kb.pe, lambda: nc.tensor.matmul(ps[:, 0:128], lhsT=TT[:], rhs=R[:], start=True, stop=True),
                      reads=[TT_r, R_r], writes=[ps_r])
                vn, vn_r = tl()
                kb.op(kb.act, lambda: nc.scalar.copy(out=vn[:], in_=ps[:, 0:128]), reads=[ps_r], writes=[vn_r])
                ps, ps_r = kb.bank()
                kb.op(kb.pe, lambda: nc.tensor.matmul(ps[:, 0:128], lhsT=kd[:], rhs=vn[:], start=True, stop=True),
                      reads=[kd_r, vn_r], writes=[ps_r])
                kb.op(kb.dve, lambda: nc.vector.scalar_tensor_tensor(out=Sn[:], in0=S[:], scalar=egl, in1=ps[:, 0:128],
                                                                     op0=ALU.mult, op1=ALU.add),
                      reads=[S_r, cols_r, ps_r], writes=[Sn_r])
                psO, psO_r = kb.bank()
                kb.op(kb.pe, lambda: nc.tensor.matmul(psO[:, 0:128], lhsT=S[:], rhs=qgT[:], start=True, stop=False),
                      reads=[S_r, qg_r], writes=[psO_r])
                kb.op(kb.pe, lambda: nc.tensor.matmul(psO[:, 0:128], lhsT=vn[:], rhs=MT[:], start=False, stop=True),
                      reads=[vn_r, MT_r], writes=[psO_r])
                kb.op(kb.dve, lambda: nc.vector.tensor_tensor(out=OT[:, sl], in0=OT[:, sl], in1=psO[:, 0:128], op=ALU.add),
                      reads=[OT_r, psO_r], writes=[OT_r])
        kb.dma(kb.sp, G0[:], self.projT[PR_DN + 1536 + h * 128:PR_DN + 1536 + (h + 1) * 128, :], reads=[self.r_projT],
               writes=[G0_r])
        kb.op(kb.act, lambda: nc.scalar.activation(out=G0[:], in_=G0[:], func=AF.Silu), reads=[G0_r], writes=[G0_r])
        for (t0, ntok) in GROUPS:
            kb.op(kb.act, lambda: nc.scalar.activation(out=GC[:, t0:t0 + ntok], in_=OT[:, t0:t0 + ntok], func=AF.Square),
                  reads=[OT_r], writes=[GC_r])
            ps, ps_r = kb.bank()
            kb.op(kb.pe, lambda: nc.tensor.matmul(ps[:, 0:ntok], lhsT=onesF[:], rhs=GC[:, t0:t0 + ntok], start=True, stop=True),
                  reads=[ones_r, GC_r], writes=[ps_r])
            kb.op(kb.dve, lambda: nc.vector.tensor_scalar(out=Xp[:, t0:t0 + ntok], in0=ps[:, 0:ntok], scalar1=1.0 / 128,
                                                          scalar2=EPS, op0=ALU.mult, op1=ALU.add), reads=[ps_r], writes=[Xp_r])
        kb.op(kb.act, lambda: nc.scalar.sqrt(out=Xp[:, 0:T], in_=Xp[:, 0:T]), reads=[Xp_r], writes=[Xp_r])
        kb.op(kb.dve, lambda: nc.vector.reciprocal(out=Xp[:, 0:T], in_=Xp[:, 0:T]), reads=[Xp_r], writes=[Xp_r])
        kb.op(kb.dve, lambda: nc.vector.tensor_tensor(out=OT[:], in0=OT[:], in1=Xp[:, 0:T], op=ALU.mult),
              reads=[OT_r, Xp_r], writes=[OT_r])
        ob = GC[:].bitcast(BF16)[:, 0:T]
        kb.op(kb.dve, lambda: nc.vector.scalar_tensor_tensor(out=ob, in0=OT[:], scalar=ng[:, 0:1], in1=G0[:], op0=ALU.mult,
                                                             op1=ALU.mult), reads=[OT_r, ng_r, G0_r], writes=[GC_r])
        kb.dma(kb.sp, self.mixT[1024 + h * 128:1024 + (h + 1) * 128, :], ob, reads=[GC_r], mwrites=[self.r_mixT])
        kb.op(kb.pool, lambda: nc.gpsimd.memset(Xp[:], 0.0), reads=[Xp_r], writes=[Xp_r])
    kb.end_phase()


Layer.declare_dn = declare_dn
Layer.phase_dn = phase_dn


CAP = T
NSLOT = NE * CAP


def declare_ffn(self):
    i = self.inp
    self.w_out = i("w_out", [D, D])
    self.w_router = i("w_router", [D, NE])
    self.b_router = i("b_router", [NE])
    self.w_gu = i("w_gu", [NE, D, 2 * DE])
    self.b_guT = i("b_guT", [NE, 128, 16])
    self.w_down = i("w_down", [NE, DE, D])
    self.b_down = i("b_down", [NE, D])
    self.iota32 = i("iota32", [NE])
    self.iotap = i("iotap", [128, 1])
    kb = self.kb
    self.x1 = kb.dram("x1", [T, D], F32)
    self.r_x1 = kb.res("x1")
    self.gd = kb.dram("gd", [T, NE], F32)
    self.r_gd = kb.res("gd")
    self.yacc = kb.dram("yacc", [T, D], F32)
    self.r_yacc = [kb.res(f"yacc{j}") for j in range(T // 128)]
    self.hT2_d = kb.dram("hT2_d", [len(GROUPS), 128, 16, 512], BF16)
    self.r_hT2 = [kb.res(f"hT2_{g}") for g in range(len(GROUPS))]


def own_tiles(self):
    tiles = [(j * 128, 1) for j in range(NCTX // 128)] if self.need_ctx else []
    return tiles + [(NCTX + j * 128, 0) for j in range(NLAT // 128)]


def phase_outproj(self, xsrc, xsrc_r):
    nc, kb = self.nc, self.kb
    kb.begin_phase()
    wo, wo_r = kb.sbuf("wo", [128, 16, D], BF16)
    wv = self.w_out.rearrange("(k p) n -> p k n", p=128)
    for q in range(4):
        kb.dma(kb.pool, wo[:, :, q * 512:(q + 1) * 512], wv[:, :, q * 512:(q + 1) * 512], mwrites=[wo_r])
    gts = []
    for s in range(2):
        gt, gt_r = kb.sbuf("gate1", [128, D], F32)
        kb.dma(kb.sp, gt[:], self.modbc[s * 6 + 2], reads=[self.r_modbc], writes=[gt_r])
        gts.append((gt, gt_r))
    mts = [kb.sbuf("mt", [128, 16, 128], BF16) for _ in range(2)]
    xts = [kb.sbuf("xo", [128, D], F32) for _ in range(2)]
    tms = [kb.sbuf("tm", [128, D], F32) for _ in range(2)]
    mv = self.mixT.rearrange("(k p) t -> p k t", p=128)
    for ti, (t0, s) in enumerate(own_tiles(self)):
        mt, mt_r = mts[ti % 2]
        xt, xt_r = xts[ti % 2]
        tm, tm_r = tms[ti % 2]
        gt, gt_r = gts[s]
        kb.dma(kb.sp, mt[:], mv[:, :, t0:t0 + 128], reads=[self.r_mixT], writes=[mt_r])
        kb.dma(kb.act, xt[:], xsrc[t0:t0 + 128, :], reads=[xsrc_r], writes=[xt_r])
        for q in range(4):
            ps, ps_r = kb.bank()
            for k in range(16):
                kb.op(kb.pe, lambda: nc.tensor.matmul(ps[:], lhsT=mt[:, k, :], rhs=wo[:, k, q * 512:(q + 1) * 512],
                                                      start=(k == 0), stop=(k == 15)), reads=[mt_r, wo_r], writes=[ps_r])
            kb.op(kb.dve, lambda: nc.vector.tensor_tensor(out=tm[:, q * 512:(q + 1) * 512], in0=ps[:],
                                                          in1=gt[:, q * 512:(q + 1) * 512], op=ALU.mult),
                  reads=[ps_r, gt_r], writes=[tm_r])
        kb.op(kb.pool, lambda: nc.gpsimd.tensor_tensor(out=tm[:], in0=tm[:], in1=xt[:], op=ALU.add),
              reads=[tm_r, xt_r], writes=[tm_r])
        kb.dma(kb.sp, self.x1[t0:t0 + 128, :], tm[:], reads=[tm_r], mwrites=[self.r_x1])
    kb.end_phase()


def phase_route(self):
    nc, kb = self.nc, self.kb
    kb.begin_phase()
    identF, id_r = self.make_ident(F32)
    onesF, ones_r = kb.sbuf("rones", [128, 128], F32)
    kb.op(kb.pool, lambda: nc.gpsimd.memset(onesF[:], 1.0), writes=[ones_r])
    ustr, us_r = kb.sbuf("ustr", [128, 128], F32)
    kb.op(kb.pool, lambda: nc.gpsimd.memset(ustr[:], 1.0), writes=[us_r])
    kb.op(kb.pool, lambda: nc.gpsimd.affine_select(out=ustr[:], in_=ustr[:], pattern=[[1, 128]], compare_op=ALU.is_gt,
                                                   fill=0.0, base=0, channel_multiplier=-1), reads=[us_r], writes=[us_r])
    iot, iot_r = kb.sbuf("iota", [128, NE], F32)
    kb.dma(kb.sp, iot[:], self.iota32.partition_broadcast(128), writes=[iot_r])
    wr, wr_r = kb.sbuf("wr", [128, 16, NE], F32)
    kb.dma(kb.sp, wr[:], self.w_router.rearrange("(k p) n -> p k n", p=128), writes=[wr_r])
    br, br_r = kb.sbuf("br", [128, NE], F32)
    kb.dma(kb.sp, br[:], self.b_router.partition_broadcast(128), writes=[br_r])
    gb, gb_r = kb.sbuf("gb2", [128, D], F32)
    kb.dma(kb.sp, gb[:], self.gffn.partition_broadcast(128), writes=[gb_r])
    G, SH = [], []
    for s in range(2):
        g1, g1_r = kb.sbuf("g2", [128, D], F32)
        sh, sh_r = kb.sbuf("sh2", [128, D], F32)
        kb.dma(kb.sp, g1[:], self.modbc[s * 6 + 4], reads=[self.r_modbc], writes=[g1_r])
        kb.dma(kb.sp, sh[:], self.modbc[s * 6 + 3], reads=[self.r_modbc], writes=[sh_r])
        kb.op(kb.dve, lambda: nc.vector.scalar_tensor_tensor(out=g1[:], in0=g1[:], scalar=1.0, in1=gb[:], op0=ALU.add,
                                                             op1=ALU.mult), reads=[g1_r, gb_r], writes=[g1_r])
        G.append((g1, g1_r))
        SH.append((sh, sh_r))
    identB, idb_r = kb.sbuf("identB", [128, 128], BF16)
    kb.op(kb.dve, lambda: nc.vector.tensor_copy(out=identB[:], in_=identF[:]), reads=[id_r], writes=[idb_r])
    iop, iop_r = kb.sbuf("iop", [128, 1], F32)
    kb.dma(kb.sp, iop[:], self.iotap, writes=[iop_r])
    hTs = [kb.sbuf("hT2", [128, 16, 512], BF16) for _ in range(2)]

    def tile_group(t0):
        if t0 < NCTX:
            return 0, t0 // 128
        return 1 + (t0 - NCTX) // 512, ((t0 - NCTX) % 512) // 128
    xts = [kb.sbuf("xr", [128, D], F32) for _ in range(2)]
    junk, junk_r = kb.sbuf("junkr", [128, D], BF16)
    junkf, junkf_r = kb.sbuf("junkf", [128, NE], F32)
    hbs = [kb.sbuf("hbr", [128, D], BF16) for _ in range(2)]
    hTf, hTf_r = kb.sbuf("hTf", [128, 16, 128], F32)
    sts = [kb.sbuf("str", [128, 4], F32) for _ in range(2)]
    rts = [kb.sbuf("rt", [128, 96], F32) for _ in range(2)]
    dis = [kb.sbuf("di", [128, 4], I32) for _ in range(2)]
    dks = [[kb.sbuf("dk", [128, 1], I32) for _ in range(4)] for _ in range(2)]
    ius = [kb.sbuf("iu", [128, 8], U32) for _ in range(2)]
    poss = [kb.sbuf("pos", [128, NE], F32) for _ in range(2)]
    for ti, (t0, s) in enumerate(own_tiles(self)):
        xt, xt_r = xts[ti % 2]
        hb, hb_r = hbs[ti % 2]
        st, st_r = sts[ti % 2]
        rt, rt_r = rts[ti % 2]
        di, di_r = dis[ti % 2]
        iu, iu_r = ius[ti % 2]
        pos, pos_r = poss[ti % 2]
        g1, g1_r = G[s]
        sh, sh_r = SH[s]
        kb.dma(kb.sp, xt[:], self.x1[t0:t0 + 128, :], reads=[self.r_x1], writes=[xt_r])
        kb.op(kb.act, lambda: nc.scalar.activation(out=junk[:], in_=xt[:], func=AF.Square, accum_out=st[:, 0:1]),
              reads=[xt_r], writes=[junk_r, st_r])
        kb.op(kb.dve, lambda: nc.vector.tensor_scalar(out=st[:, 1:2], in0=st[:, 0:1], scalar1=1.0 / D, scalar2=EPS,
                                                      op0=ALU.mult, op1=ALU.add), reads=[st_r], writes=[st_r])
        kb.op(kb.act, lambda: nc.scalar.sqrt(out=st[:, 2:3], in_=st[:, 1:2]), reads=[st_r], writes=[st_r])
        kb.op(kb.dve, lambda: nc.vector.reciprocal(out=st[:, 3:4], in_=st[:, 2:3]), reads=[st_r], writes=[st_r])
        kb.op(kb.dve, lambda: nc.vector.scalar_tensor_tensor(out=xt[:], in0=xt[:], scalar=st[:, 3:4], in1=g1[:],
                                                             op0=ALU.mult, op1=ALU.mult), reads=[xt_r, st_r, g1_r], writes=[xt_r])
        kb.op(kb.pool, lambda: nc.gpsimd.tensor_tensor(out=xt[:], in0=xt[:], in1=sh[:], op=ALU.add),
              reads=[xt_r, sh_r], writes=[xt_r])
        kb.op(kb.act, lambda: nc.scalar.copy(out=hb[:], in_=xt[:]), reads=[xt_r], writes=[hb_r])
        for q in range(4):
            ps, ps_r = kb.bank()
            for kk in range(4):
                k = q * 4 + kk
                kb.op(kb.pe, lambda: nc.tensor.transpose(out=ps[:, kk * 128:(kk + 1) * 128], in_=xt[:, k * 128:(k + 1) * 128],
                                                         identity=identF[:]), reads=[xt_r, id_r], writes=[ps_r])
            eng = self.evac_eng()
            kb.op(eng, self.copy(eng, hTf[:, q * 4:q * 4 + 4, :], ps[:].rearrange("p (k t) -> p k t", t=128)),
                  reads=[ps_r], writes=[hTf_r])
        psl, psl_r = kb.bank()
        for k in range(16):
            kb.op(kb.pe, lambda: nc.tensor.matmul(psl[:, 0:NE], lhsT=hTf[:, k, :], rhs=wr[:, k, :], start=(k == 0), stop=(k == 15)),
                  reads=[hTf_r, wr_r], writes=[psl_r])
        kb.op(kb.dve, lambda: nc.vector.tensor_tensor(out=rt[:, 0:NE], in0=psl[:, 0:NE], in1=br[:], op=ALU.add),
              reads=[psl_r, br_r], writes=[rt_r])
        kb.op(kb.dve, lambda: nc.vector.max(out=rt[:, 32:40], in_=rt[:, 0:NE]), reads=[rt_r], writes=[rt_r])
        kb.op(kb.dve, lambda: nc.vector.max_index(out=iu[:], in_max=rt[:, 32:40], in_values=rt[:, 0:NE]),
              reads=[rt_r], writes=[iu_r])
        kb.op(kb.dve, lambda: nc.vector.tensor_copy(out=rt[:, 40:48], in_=iu[:]), reads=[iu_r], writes=[rt_r])
        kb.op(kb.dve, lambda: nc.vector.tensor_scalar(out=rt[:, 48:52], in0=rt[:, 32:36], scalar1=rt[:, 32:33], scalar2=None,
                                                      op0=ALU.subtract), reads=[rt_r], writes=[rt_r])
        kb.op(kb.act, lambda: nc.scalar.activation(out=rt[:, 48:52], in_=rt[:, 48:52], func=AF.Exp, accum_out=rt[:, 52:53]),
              reads=[rt_r], writes=[rt_r])
        kb.op(kb.dve, lambda: nc.vector.reciprocal(out=rt[:, 53:54], in_=rt[:, 52:53]), reads=[rt_r], writes=[rt_r])
        kb.op(kb.dve, lambda: nc.vector.tensor_scalar(out=rt[:, 48:52], in0=rt[:, 48:52], scalar1=rt[:, 53:54], scalar2=None,
                                                      op0=ALU.mult), reads=[rt_r], writes=[rt_r])
        kb.op(kb.dve, lambda: nc.vector.tensor_scalar(out=rt[:, 56:88], in0=iot[:], scalar1=rt[:, 40:41], scalar2=rt[:, 48:49],
                                                      op0=ALU.is_equal, op1=ALU.mult), reads=[rt_r, iot_r], writes=[rt_r])
        for k in range(1, 4):
            kb.op(kb.dve, lambda: nc.vector.tensor_scalar(out=junkf[:], in0=iot[:], scalar1=rt[:, 40 + k:41 + k],
                                                          scalar2=rt[:, 48 + k:49 + k], op0=ALU.is_equal, op1=ALU.mult),
                  reads=[rt_r, iot_r], writes=[junkf_r])
            kb.op(kb.dve, lambda: nc.vector.tensor_tensor(out=rt[:, 56:88], in0=rt[:, 56:88], in1=junkf[:], op=ALU.add),
                  reads=[rt_r, junkf_r], writes=[rt_r])
        kb.dma(kb.sp, self.gd[t0:t0 + 128, :], rt[:, 56:88], reads=[rt_r], mwrites=[self.r_gd])
        g, j = tile_group(t0)
        hT, hT_r = hTs[g % 2]
        for half in range(2):
            ps, ps_r = kb.bank()
            pb = ps[:].bitcast(BF16)
            for kk in range(8):
                k = half * 8 + kk
                kb.op(kb.pe, lambda: nc.tensor.transpose(out=pb[:, kk * 128:(kk + 1) * 128], in_=hb[:, k * 128:(k + 1) * 128],
                                                         identity=identB[:]), reads=[hb_r, idb_r], writes=[ps_r])
            eng = self.evac_eng()
            kb.op(eng, self.copy(eng, hT[:, half * 8:half * 8 + 8, j * 128:(j + 1) * 128],
                                 pb.rearrange("p (k t) -> p k t", t=128)), reads=[ps_r], writes=[hT_r])
        if (j + 1) * 128 == GROUPS[g][1]:
            ntok = GROUPS[g][1]
            kb.dma(kb.sp, self.hT2_d[g, :, :, 0:ntok], hT[:, :, 0:ntok], reads=[hT_r], writes=[self.r_hT2[g]])
    kb.end_phase()


def phase_experts(self):
    nc, kb = self.nc, self.kb
    kb.begin_phase()
    wbuf = [kb.sbuf("wb", [128, 16, 1024], BF16) for _ in range(3)]
    hTs = [kb.sbuf("xTe", [128, 16, 512], BF16) for _ in range(2)]
    aTs = [kb.sbuf("aTe", [128, 8, 512], BF16) for _ in range(2)]
    bgus = [kb.sbuf("bgu", [128, 16], F32) for _ in range(2)]
    bdfs = [kb.sbuf("bdf", [1, D], F32) for _ in range(2)]
    bdbs = [kb.sbuf("bdb", [1, D], BF16) for _ in range(2)]
    onesb, onesb_r = kb.sbuf("onesb", [1, 128], BF16)
    kb.op(kb.pool, lambda: nc.gpsimd.memset(onesb[:], 1.0), writes=[onesb_r])
    ost = [kb.sbuf("ost", [128, D], F32) for _ in range(2)]
    g1s = [kb.sbuf("g1s", [128, 512], F32) for _ in range(2)]
    g2s = [kb.sbuf("g2s", [128, 512], F32) for _ in range(2)]

    def load_bias(e):
        bgu_, bgu_r_ = bgus[e % 2]
        bdf_, bdf_r_ = bdfs[e % 2]
        bdb_, bdb_r_ = bdbs[e % 2]
        kb.dma(kb.sp, bgu_[:], self.b_guT[e], writes=[bgu_r_])
        kb.dma(kb.sp, bdf_[:], self.b_down[e:e + 1, :], writes=[bdf_r_])
        kb.op(kb.act, lambda: nc.scalar.copy(out=bdb_[:], in_=bdf_[:]), reads=[bdf_r_], writes=[bdb_r_])
    load_bias(0)
    groups = [(g, GROUPS[g][0], GROUPS[g][1]) for g in range(len(GROUPS)) if (g > 0 or self.need_ctx)]
    NT = T // 128
    gdt, gdt_r = kb.sbuf("gdt", [128, NT, NE], F32)
    kb.dma(kb.sp, gdt[:], self.gd.rearrange("(n p) e -> p n e", p=128), reads=[self.r_gd], writes=[gdt_r])
    gi = 0
    oi = 0
    hi = 0
    for e in range(NE):
        guv = self.w_gu[e].rearrange("(k p) n -> p k n", p=128)
        dnv = self.w_down[e].rearrange("(k p) n -> p k n", p=128)
        pieces = []
        for half in range(2):
            wb, wb_r = wbuf[half]
            kb.dma(kb.pool, wb[:, :, 0:512], guv[:, :, half * 512:(half + 1) * 512], mwrites=[wb_r])
            kb.dma(kb.pool, wb[:, :, 512:1024], guv[:, :, 1024 + half * 512:1024 + (half + 1) * 512], mwrites=[wb_r])
            pieces.append((wb, wb_r))
        wb, wb_r = wbuf[2]
        wdv = wb[:].rearrange("p (k two) n -> p k (two n)", two=2)
        wd_r = wb_r
        kb.dma(kb.pool, wdv[:, :, 0:1024], dnv[:, :, 0:1024], mwrites=[wd_r])
        kb.dma(kb.pool, wdv[:, :, 1024:2048], dnv[:, :, 1024:2048], mwrites=[wd_r])
        bgu, bgu_r = bgus[e % 2]
        bdb, bdb_r = bdbs[e % 2]
        if e + 1 < NE:
            load_bias(e + 1)
        for (g, t0, ntk) in groups:
            hT, hT_r = hTs[hi % 2]
            aT, aT_r = aTs[hi % 2]
            hi += 1
            kb.dma(kb.act, hT[:, :, 0:ntk], self.hT2_d[g, :, :, 0:ntk], reads=[self.r_hT2[g]], writes=[hT_r])
            for half in range(2):
                wb, wb_r = pieces[half]
                for jc in range(4):
                    psg, psg_r = kb.bank()
                    psl, psl_r = kb.bank()
                    for k in range(16):
                        kb.op(kb.pe, lambda: nc.tensor.matmul(psg[:, 0:ntk], lhsT=wb[:, k, jc * 128:(jc + 1) * 128],
                                                              rhs=hT[:, k, 0:ntk], start=(k == 0), stop=(k == 15)),
                              reads=[wb_r, hT_r], writes=[psg_r])
                    for k in range(16):
                        kb.op(kb.pe, lambda: nc.tensor.matmul(psl[:, 0:ntk], lhsT=wb[:, k, 512 + jc * 128:512 + (jc + 1) * 128],
                                                              rhs=hT[:, k, 0:ntk], start=(k == 0), stop=(k == 15)),
                              reads=[wb_r, hT_r], writes=[psl_r])
                    J = half * 4 + jc
                    g1, g1_r = g1s[gi % 2]
                    g2, g2_r = g2s[gi % 2]
                    gi += 1
                    kb.op(kb.dve, lambda: nc.vector.tensor_scalar(out=g1[:, 0:ntk], in0=psg[:, 0:ntk], scalar1=bgu[:, J:J + 1],
                                                                  scalar2=7.0, op0=ALU.add, op1=ALU.min),
                          reads=[psg_r, bgu_r], writes=[g1_r])
                    kb.op(kb.act, lambda: nc.scalar.activation(out=g2[:, 0:ntk], in_=g1[:, 0:ntk], func=AF.Sigmoid, scale=1.702),
                          reads=[g1_r], writes=[g2_r])
                    kb.op(kb.pool, lambda: nc.gpsimd.tensor_tensor(out=g1[:, 0:ntk], in0=g1[:, 0:ntk], in1=g2[:, 0:ntk],
                                                                   op=ALU.mult), reads=[g1_r, g2_r], writes=[g1_r])
                    kb.op(kb.dve, lambda: nc.vector.tensor_scalar(out=g2[:, 0:ntk], in0=psl[:, 0:ntk], scalar1=bgu[:, 8 + J:9 + J],
                                                                  scalar2=7.0, op0=ALU.add, op1=ALU.min),
                          reads=[psl_r, bgu_r, g2_r], writes=[g2_r])
                    kb.op(kb.dve, lambda: nc.vector.tensor_scalar(out=g2[:, 0:ntk], in0=g2[:, 0:ntk], scalar1=-7.0, scalar2=1.0,
                                                                  op0=ALU.max, op1=ALU.add), reads=[g2_r], writes=[g2_r])
                    kb.op(kb.dve, lambda: nc.vector.tensor_tensor(out=aT[:, J, 0:ntk], in0=g1[:, 0:ntk], in1=g2[:, 0:ntk],
                                                                  op=ALU.mult), reads=[g1_r, g2_r], writes=[aT_r])
            for j in range(ntk // 128):
                o, o_r = ost[oi % 2]
                oi += 1
                tix = (t0 + j * 128) // 128
                gcol = gdt[:, tix, e:e + 1]
                yp, yp_r = o, o_r
                if e > 0:
                    kb.dma(kb.act, yp[:], self.yacc[t0 + j * 128:t0 + (j + 1) * 128, :], reads=[self.r_yacc[tix]], writes=[yp_r])
                for q in range(4):
                    ps, ps_r = kb.bank()
                    for k in range(8):
                        kb.op(kb.pe, lambda: nc.tensor.matmul(ps[:], lhsT=aT[:, k, j * 128:(j + 1) * 128],
                                                              rhs=wdv[:, k, q * 512:(q + 1) * 512], start=(k == 0), stop=False),
                              reads=[aT_r, wd_r], writes=[ps_r])
                    kb.op(kb.pe, lambda: nc.tensor.matmul(ps[:], lhsT=onesb[:], rhs=bdb[:, q * 512:(q + 1) * 512], start=False,
                                                          stop=True), reads=[onesb_r, bdb_r], writes=[ps_r])
                    if e == 0:
                        kb.op(kb.dve, lambda: nc.vector.tensor_scalar(out=o[:, q * 512:(q + 1) * 512], in0=ps[:], scalar1=gcol,
                                                                      scalar2=None, op0=ALU.mult), reads=[ps_r, gdt_r], writes=[o_r])
                    else:
                        kb.op(kb.dve, lambda: nc.vector.scalar_tensor_tensor(out=o[:, q * 512:(q + 1) * 512], in0=ps[:], scalar=gcol,
                                                                             in1=yp[:, q * 512:(q + 1) * 512], op0=ALU.mult,
                                                                             op1=ALU.add), reads=[ps_r, gdt_r, yp_r], writes=[o_r])
                r0 = t0 + j * 128
                kb.dma(kb.sp, self.yacc[r0:r0 + 128, :], o[:], reads=[o_r], writes=[self.r_yacc[tix]])
    kb.end_phase()


def phase_combine(self, xdst, xdst_r, final_out=None, final_g=None):
    nc, kb = self.nc, self.kb
    kb.begin_phase()
    gts = []
    for s in range(2):
        gt, gt_r = kb.sbuf("gate2", [128, D], F32)
        kb.dma(kb.sp, gt[:], self.modbc[s * 6 + 5], reads=[self.r_modbc], writes=[gt_r])
        gts.append((gt, gt_r))
    if final_out is not None:
        fg, fg_r = kb.sbuf("fg", [128, D], F32)
        kb.dma(kb.sp, fg[:], final_g.partition_broadcast(128), writes=[fg_r])
    xts = [kb.sbuf("xc", [128, D], F32) for _ in range(2)]
    accs = [kb.sbuf("acc", [128, D], F32) for _ in range(2)]
    sts = [kb.sbuf("stc", [128, 4], F32) for _ in range(2)]
    junk, junk_r = kb.sbuf("junkc", [128, D], BF16)
    for ti, (t0, s) in enumerate(own_tiles(self)):
        xt, xt_r = xts[ti % 2]
        acc, acc_r = accs[ti % 2]
        gt, gt_r = gts[s]
        kb.dma(kb.sp, acc[:], self.yacc[t0:t0 + 128, :], reads=[self.r_yacc[t0 // 128]], writes=[acc_r])
        kb.dma(kb.act, xt[:], self.x1[t0:t0 + 128, :], reads=[self.r_x1], writes=[xt_r])
        kb.op(kb.pool, lambda: nc.gpsimd.tensor_tensor(out=acc[:], in0=acc[:], in1=gt[:], op=ALU.mult),
              reads=[acc_r, gt_r], writes=[acc_r])
        kb.op(kb.dve, lambda: nc.vector.tensor_tensor(out=acc[:], in0=acc[:], in1=xt[:], op=ALU.add),
              reads=[acc_r, xt_r], writes=[acc_r])
        if final_out is None:
            kb.dma(kb.sp, xdst[t0:t0 + 128, :], acc[:], reads=[acc_r], mwrites=[xdst_r])
        else:
            st, st_r = sts[ti % 2]
            kb.op(kb.act, lambda: nc.scalar.activation(out=junk[:], in_=acc[:], func=AF.Square, accum_out=st[:, 0:1]),
                  reads=[acc_r], writes=[junk_r, st_r])
            kb.op(kb.dve, lambda: nc.vector.tensor_scalar(out=st[:, 1:2], in0=st[:, 0:1], scalar1=1.0 / D, scalar2=EPS,
                                                          op0=ALU.mult, op1=ALU.add), reads=[st_r], writes=[st_r])
            kb.op(kb.act, lambda: nc.scalar.sqrt(out=st[:, 2:3], in_=st[:, 1:2]), reads=[st_r], writes=[st_r])
            kb.op(kb.dve, lambda: nc.vector.reciprocal(out=st[:, 3:4], in_=st[:, 2:3]), reads=[st_r], writes=[st_r])
            kb.op(kb.dve, lambda: nc.vector.scalar_tensor_tensor(out=acc[:], in0=acc[:], scalar=st[:, 3:4], in1=fg[:],
                                                                 op0=ALU.mult, op1=ALU.mult), reads=[acc_r, st_r, fg_r],
                  writes=[acc_r])
            kb.dma(kb.sp, final_out[t0 - NCTX:t0 - NCTX + 128, :], acc[:], reads=[acc_r], mwrites=[xdst_r])
    kb.end_phase()


Layer.declare_ffn = declare_ffn
Layer.phase_outproj = phase_outproj
Layer.phase_route = phase_route
Layer.phase_experts = phase_experts
Layer.phase_combine = phase_combine


def build_model(layers=(0, 1), stop_after=None, debug=()):
    nc = bass.Bass("TRN2", target_bir_lowering=False)
    kb = KB(nc)
    kb.debug = set(debug) | {"out"}
    Ls = []
    for l in layers:
        last = (l == 1)
        L = Layer(nc, kb, need_ctx=not last, stop_after=stop_after, lidx=l, last=last)
        L.declare_io()
        L.run()
        Ls.append(L)
    kb.finish()
    return nc, kb


_NC_CACHE = {}


def kernel(**inputs):
    inp = {k: np.asarray(v) for k, v in inputs.items()}
    if "nc" not in _NC_CACHE:
        _NC_CACHE["nc"] = build_model()
    nc, kb = _NC_CACHE["nc"]
    names = set(kb._inp.keys())
    lay = {}
    for l in (0, 1):
        lay.update(host_layer_inputs(inp, l))
    maps = []
    for b in range(4):
        m = dict(lay)
        m.update(host_shared_inputs(inp, b))
        maps.append({k: np.ascontiguousarray(v, dtype=np.float32) for k, v in m.items() if k in names})
    res = run_bass_kernel_spmd(nc, maps, core_ids=list(range(4)))
    return np.stack([res.results[b]["out"] for b in range(4)], 0).astype(np.float32)
```

```python
import numpy as np
from contextlib import ExitStack
import concourse.bass as bass
import concourse.mybir as mybir
from concourse.bass_utils import run_bass_kernel_spmd

F32 = mybir.dt.float32
BF16 = mybir.dt.bfloat16
I32 = mybir.dt.int32
U32 = mybir.dt.uint32
AF = mybir.ActivationFunctionType
ALU = mybir.AluOpType
AX = mybir.AxisListType

SEM_LIMIT = 30000

D = 2048
NCTX = 256
NLAT = 4096
T = NCTX + NLAT
OWN_CTX = 256
OWN_LAT = 4096
NE = 32
DE = 1024
EPS = 1e-6


class Res:
    __slots__ = ("w", "r", "name")

    def __init__(self, name=""):
        self.w = []
        self.r = []
        self.name = name


class Eng:
    def __init__(self, kb, name, h, same_sync):
        self.kb = kb
        self.name = name
        self.h = h
        self.same_sync = same_sync
        self.sem = kb.new_sem(name)
        self.count = 0
        self.seen = {}
        self.dma_sems = []
        self.dma_cnt = []
        self.dma_rr = 0
        self.prev = None

    def _wait(self, dep):
        sem, val = dep
        if self.seen.get(sem.num, 0) >= val:
            return
        self.h.wait_ge(sem, val)
        self.seen[sem.num] = val
        if (sem.num, val) in self.kb.settle:
            self.h.nop(cycle_cnt=2400)

    def _roll(self):
        if self.count >= SEM_LIMIT:
            self.prev = (self.sem, self.count)
            self.sem = self.kb.new_sem(self.name)
            self.count = 0


class KB:
    def __init__(self, nc):
        self.nc = nc
        self.es = ExitStack()
        self.stacks = [self.es]
        self.nsem = 0
        self.pe = Eng(self, "pe", nc.tensor, False)
        self.act = Eng(self, "act", nc.scalar, True)
        self.dve = Eng(self, "dve", nc.vector, True)
        self.pool = Eng(self, "pool", nc.gpsimd, True)
        self.sp = Eng(self, "sp", nc.sync, True)
        self.engs = [self.pe, self.act, self.dve, self.pool, self.sp]
        self.n_ops = 0
        self.uid = 0
        self.banks = []
        for i in range(8):
            t = self.es.enter_context(nc.psum_tensor(f"bank{i}", [128, 512], F32))
            self.banks.append((t, Res(f"bank{i}")))
        self.bank_rr = 0
        self.debug = set()
        self._dram, self._res, self._inp = {}, {}, {}
        self._bregs = {}
        self.settle = set()

    def new_sem(self, name):
        self.nsem += 1
        return self.es.enter_context(self.nc.semaphore(f"s{self.nsem}_{name}"))

    def bank(self):
        b = self.banks[self.bank_rr % 8]
        self.bank_rr += 1
        return b

    def sbuf(self, name, shape, dtype):
        self.uid += 1
        t = self.stacks[-1].enter_context(self.nc.sbuf_tensor(f"{name}_{self.uid}", list(shape), dtype))
        return t, Res(name)

    def dram(self, name, shape, dtype):
        if name in self._dram:
            return self._dram[name]
        kind = "ExternalOutput" if name in self.debug else "Internal"
        ap = self.nc.dram_tensor(name, list(shape), dtype, kind=kind).ap()
        self._dram[name] = ap
        return ap

    def res(self, name):
        if name not in self._res:
            self._res[name] = Res(name)
        return self._res[name]

    def inp(self, name, shape, dtype=F32):
        if name not in self._inp:
            self._inp[name] = self.nc.dram_tensor(name, list(shape), dtype, kind="ExternalInput").ap()
        return self._inp[name]

    def begin_phase(self):
        self.stacks.append(ExitStack())

    def end_phase(self):
        self.barrier()
        self.stacks.pop().close()

    def barrier(self):
        toks = []
        for e in self.engs:
            if e.count > 0:
                toks.append((e.sem, e.count))
            elif e.prev is not None:
                toks.append(e.prev)
            for s, c in zip(e.dma_sems, e.dma_cnt):
                if c > 0:
                    toks.append((s, c * 16))
        for e in self.engs:
            for tk in toks:
                if tk[0] is e.sem and not e.same_sync:
                    continue
                e._wait(tk)

    def _deps(self, eng, reads, writes, mwrites=()):
        def wt(d):
            if not (d[0] is eng.sem and not eng.same_sync):
                eng._wait(d)
        for r in reads:
            for d in r.w:
                wt(d)
        for w in writes:
            for d in w.w:
                wt(d)
            for d in w.r:
                wt(d)
        for w in mwrites:
            for d in w.r:
                wt(d)

    @staticmethod
    def _prune(lst):
        last = {}
        for s_, v in lst:
            if s_.num not in last or last[s_.num][1] < v:
                last[s_.num] = (s_, v)
        return list(last.values())

    def _commit(self, tok, reads, writes, mwrites=()):
        for w in writes:
            w.w = [tok]
            w.r = []
        for w in mwrites:
            w.w.append(tok)
            if len(w.w) > 32:
                w.w = self._prune(w.w)
        for r in reads:
            if r not in writes:
                r.r.append(tok)
                if len(r.r) > 32:
                    r.r = self._prune(r.r)

    def op(self, eng, fn, reads=(), writes=(), mwrites=()):
        self._deps(eng, reads, writes, mwrites)
        eng._roll()
        ins = fn()
        eng.count += 1
        ins.then_inc(eng.sem, 1)
        tok = (eng.sem, eng.count)
        self._commit(tok, reads, writes, mwrites)
        self.n_ops += 1
        return tok

    def _dma_sem(self, eng, nsems):
        if len(eng.dma_sems) < nsems:
            eng.dma_sems.append(self.new_sem(eng.name + "dma"))
            eng.dma_cnt.append(0)
            i = len(eng.dma_sems) - 1
        else:
            i = eng.dma_rr % len(eng.dma_sems)
            if eng.dma_cnt[i] * 16 >= SEM_LIMIT:
                eng._wait((eng.dma_sems[i], eng.dma_cnt[i] * 16))
                eng.dma_sems[i] = self.new_sem(eng.name + "dma")
                eng.dma_cnt[i] = 0
        eng.dma_rr += 1
        if eng.dma_cnt[i] > 0:
            eng._wait((eng.dma_sems[i], eng.dma_cnt[i] * 16))
        return i

    def dma(self, eng, out, in_, reads=(), writes=(), mwrites=(), nsems=8, settle=False, **kw):
        self._deps(eng, reads, writes, mwrites)
        i = self._dma_sem(eng, nsems)
        sem = eng.dma_sems[i]
        ins = eng.h.dma_start(out=out, in_=in_, **kw)
        eng.dma_cnt[i] += 1
        ins.then_inc(sem, 16)
        tok = (sem, eng.dma_cnt[i] * 16)
        try:
            bcast = any(int(st_) == 0 and int(n_) > 1 for st_, n_ in in_.ap)
        except Exception:
            bcast = False
        if eng is self.pool or settle or bcast:
            self.settle.add((sem.num, tok[1]))
        self._commit(tok, reads, writes, mwrites)
        self.n_ops += 1
        return tok

    def idma(self, out=None, out_offset=None, in_=None, in_offset=None, reads=(), writes=(), mwrites=(), nsems=8, **kw):
        eng = self.pool
        self._deps(eng, reads, writes, mwrites)
        i = self._dma_sem(eng, nsems)
        sem = eng.dma_sems[i]
        if "bounds_check" in kw and isinstance(kw["bounds_check"], int):
            bv = kw["bounds_check"]
            if bv not in self._bregs:
                reg = self.nc.gpsimd.alloc_register(f"bnd{bv}")
                self.nc.gpsimd.reg_mov(reg, bv)
                self._bregs[bv] = reg
            kw["bounds_check"] = self._bregs[bv]
        ins = eng.h.indirect_dma_start(out=out, out_offset=out_offset, in_=in_, in_offset=in_offset, **kw)
        eng.dma_cnt[i] += 1
        ins.then_inc(sem, 16)
        tok = (sem, eng.dma_cnt[i] * 16)
        self._commit(tok, reads, writes, mwrites)
        self.n_ops += 1
        return tok

    def finish(self):
        self.barrier()
        while self.stacks:
            self.stacks.pop().close()


GROUPS = [(0, NCTX)] + [(NCTX + 512 * i, 512) for i in range(NLAT // 512)]
C_AQ, C_AK, C_AV, C_DQ, C_DK, C_DV, C_DG, C_DB, C_DA, C_LX, C_LY = (
    0, 1024, 1280, 1536, 2048, 2560, 3072, 3584, 3592, 3600, 4112)
IN_W = 4624
PR_Q, PR_K, PR_DN, PR_LRU, PR_BD, PR_ROWS = 0, 1024, 1280, 3328, 4352, 4368


class Layer:
    SHARED = ("xall", "c2", "ropec", "ropes", "permT", "final_g", "iota32", "iotap")

    def __init__(self, nc, kb, need_ctx, stop_after=None, lidx=0, last=False):
        self.nc = nc
        self.kb = kb
        self.need_ctx = need_ctx
        self.stop_after = stop_after
        self.lidx = lidx
        self.last = last
        self.ev = 0

    def inp(self, name, shape, dtype=F32):
        if name not in self.SHARED:
            name = f"l{self.lidx}_{name}"
        return self.kb.inp(name, shape, dtype)

    def declare_io(self):
        i = self.inp
        self.xall = i("xall", [T, D])
        self.c2 = i("c2", [2, D])
        self.w_mod = i("w_mod", [D, 6 * D])
        self.b_mod = i("b_mod", [6 * D])
        self.gmix = i("gmix", [D])
        self.gffn = i("gffn", [D])
        self.w_in = i("w_in", [D, IN_W])
        self.w_bd = i("w_bd", [D, 16])
        kb = self.kb
        self.modbc = kb.dram("modbc", [12, 128, D], F32)
        self.hT_d = kb.dram("hT_d", [len(GROUPS), 128, 16, 512], BF16)
        self.projT = kb.dram("projT", [PR_ROWS, T], F32)
        self.vtok = kb.dram("vtok", [T, 256], BF16)
        self.r_modbc = kb.res("modbc")
        self.r_hT = [kb.res(f"hT_{g}") for g in range(len(GROUPS))]
        self.r_projT = kb.res("projT")
        self.r_vtok = kb.res("vtok")
        self.final_g = i("final_g", [D])
        self.xbuf = kb.dram("xbuf", [T, D], F32)
        self.r_xbuf = kb.res("xbuf")
        self.out = kb.dram("out", [NLAT, D], F32)
        self.r_out = kb.res("out")
        self.declare_attn()
        self.declare_lru()
        self.declare_dn()
        self.declare_ffn()

    def evac_eng(self):
        self.ev += 1
        return self.kb.act if self.ev % 2 else self.kb.dve

    def copy(self, eng, out, in_):
        nc = self.nc
        if eng is self.kb.act:
            return lambda: nc.scalar.copy(out=out, in_=in_)
        if eng is self.kb.dve:
            return lambda: nc.vector.tensor_copy(out=out, in_=in_)
        return lambda: nc.gpsimd.tensor_copy(out=out, in_=in_)

    def run(self):
        stop = self.stop_after
        if self.lidx == 0:
            xsrc, xsrc_r = self.xall, Res("xall")
        else:
            xsrc, xsrc_r = self.xbuf, self.r_xbuf
        self.phase_mod()
        if stop == "mod":
            return
        all_groups = [(g, GROUPS[g][0], GROUPS[g][1], 1 if g == 0 else 0) for g in range(len(GROUPS))]
        self.phase_norm(xsrc, self.gmix, 0, 1, self.hT_d, self.r_hT, all_groups, src_res=xsrc_r)
        self.phase_inproj()
        if stop == "inproj":
            return
        self.phase_rope()
        self.phase_attn()
        if stop == "attn":
            return
        self.phase_lru()
        if stop == "lru":
            return
        self.phase_dn()
        if stop == "dn":
            return
        self.phase_outproj(xsrc, xsrc_r)
        if stop == "outproj":
            return
        self.phase_route()
        if stop == "route":
            return
        self.phase_experts()
        if self.last:
            self.phase_combine(self.out, self.r_out, final_out=self.out, final_g=self.final_g)
        else:
            self.phase_combine(self.xbuf, self.r_xbuf)

    def phase_mod(self):
        nc, kb = self.nc, self.kb
        kb.begin_phase()
        sc, sc_r = kb.sbuf("sc", [128, 2, 16], F32)
        kb.dma(kb.sp, sc[:], self.c2.rearrange("s (p k) -> p s k", k=16), writes=[sc_r])
        kb.op(kb.act, lambda: nc.scalar.activation(out=sc[:], in_=sc[:], func=AF.Silu), reads=[sc_r], writes=[sc_r])
        rep, rep_r = kb.sbuf("rep", [128, 16, 2, 128], F32)
        kb.op(kb.dve, lambda: nc.vector.tensor_copy(
            out=rep[:], in_=sc[:].rearrange("p s k -> p k s").unsqueeze(3).to_broadcast([128, 16, 2, 128])),
            reads=[sc_r], writes=[rep_r])
        wts = [kb.sbuf("wmod", [128, 16, 512], F32) for _ in range(2)]
        bbs = [kb.sbuf("bmod", [128, 512], F32) for _ in range(2)]
        outs = [kb.sbuf("modo", [128, 512], F32) for _ in range(4)]
        wv = self.w_mod.rearrange("(p k) n -> p k n", k=16)
        oi = 0
        for piece in range(24):
            n0 = piece * 512
            wt, wt_r = wts[piece % 2]
            bb, bb_r = bbs[piece % 2]
            kb.dma(kb.sp if piece % 2 else kb.act, wt[:], wv[:, :, n0:n0 + 512], writes=[wt_r])
            kb.dma(kb.sp, bb[:], self.b_mod[n0:n0 + 512].partition_broadcast(128), writes=[bb_r])
            j, c0 = n0 // D, n0 % D
            for s in range(2):
                ps, ps_r = kb.bank()
                for k in range(16):
                    kb.op(kb.pe, lambda: nc.tensor.matmul(ps[:], lhsT=rep[:, k, s, :], rhs=wt[:, k, :],
                                                          start=(k == 0), stop=(k == 15)),
                          reads=[rep_r, wt_r], writes=[ps_r])
                o, o_r = outs[oi % 4]
                oi += 1
                kb.op(kb.dve, lambda: nc.vector.tensor_tensor(out=o[:], in0=ps[:], in1=bb[:], op=ALU.add),
                      reads=[ps_r, bb_r], writes=[o_r])
                kb.dma(kb.sp, self.modbc[s * 6 + j, :, c0:c0 + 512], o[:], reads=[o_r], mwrites=[self.r_modbc])
        kb.end_phase()

    def phase_norm(self, src, gvec, mod_shift, mod_scale, dst, dst_res, groups, src_res=None):
        nc, kb = self.nc, self.kb
        kb.begin_phase()
        ident, id_r = self.make_ident(BF16)
        gb, gb_r = kb.sbuf("gb", [128, D], F32)
        kb.dma(kb.sp, gb[:], gvec.partition_broadcast(128), writes=[gb_r])
        G, SH = [], []
        for s in range(2):
            g1, g1_r = kb.sbuf("g1", [128, D], F32)
            sh, sh_r = kb.sbuf("sh", [128, D], F32)
            kb.dma(kb.sp, g1[:], self.modbc[s * 6 + mod_scale], reads=[self.r_modbc], writes=[g1_r])
            kb.dma(kb.sp, sh[:], self.modbc[s * 6 + mod_shift], reads=[self.r_modbc], writes=[sh_r])
            kb.op(kb.dve, lambda: nc.vector.scalar_tensor_tensor(out=g1[:], in0=g1[:], scalar=1.0, in1=gb[:],
                                                                 op0=ALU.add, op1=ALU.mult),
                  reads=[g1_r, gb_r], writes=[g1_r])
            G.append((g1, g1_r))
            SH.append((sh, sh_r))
        xts = [kb.sbuf("xt", [128, D], F32) for _ in range(2)]
        junk, junk_r = kb.sbuf("junk", [128, D], BF16)
        hbs = [kb.sbuf("hb", [128, D], BF16) for _ in range(2)]
        hTs = [kb.sbuf("hT", [128, 16, 512], BF16) for _ in range(2)]
        sts = [kb.sbuf("st", [128, 4], F32) for _ in range(2)]
        ti = 0
        for gi, (gidx, t0, ntok, s) in enumerate(groups):
            hT, hT_r = hTs[gi % 2]
            for j in range(ntok // 128):
                xt, xt_r = xts[ti % 2]
                hb, hb_r = hbs[ti % 2]
                st, st_r = sts[ti % 2]
                ti += 1
                rd = [src_res] if src_res is not None else []
                kb.dma(kb.sp, xt[:], src[t0 + j * 128:t0 + (j + 1) * 128, :], reads=rd, writes=[xt_r])
                kb.op(kb.act, lambda: nc.scalar.activation(out=junk[:], in_=xt[:], func=AF.Square,
                                                           accum_out=st[:, 0:1]),
                      reads=[xt_r], writes=[junk_r, st_r])
                kb.op(kb.dve, lambda: nc.vector.tensor_scalar(out=st[:, 1:2], in0=st[:, 0:1], scalar1=1.0 / D,
                                                              scalar2=EPS, op0=ALU.mult, op1=ALU.add),
                      reads=[st_r], writes=[st_r])
                kb.op(kb.act, lambda: nc.scalar.sqrt(out=st[:, 2:3], in_=st[:, 1:2]), reads=[st_r], writes=[st_r])
                kb.op(kb.dve, lambda: nc.vector.reciprocal(out=st[:, 3:4], in_=st[:, 2:3]), reads=[st_r], writes=[st_r])
                g1, g1_r = G[s]
                sh, sh_r = SH[s]
                kb.op(kb.dve, lambda: nc.vector.scalar_tensor_tensor(out=xt[:], in0=xt[:], scalar=st[:, 3:4], in1=g1[:],
                                                                     op0=ALU.mult, op1=ALU.mult),
                      reads=[xt_r, st_r, g1_r], writes=[xt_r])
                kb.op(kb.pool, lambda: nc.gpsimd.tensor_tensor(out=hb[:], in0=xt[:], in1=sh[:], op=ALU.add),
                      reads=[xt_r, sh_r], writes=[hb_r])
                for half in range(2):
                    ps, ps_r = kb.bank()
                    pb = ps[:].bitcast(BF16)
                    for kk in range(8):
                        k = half * 8 + kk
                        kb.op(kb.pe, lambda: nc.tensor.transpose(out=pb[:, kk * 128:(kk + 1) * 128],
                                                                 in_=hb[:, k * 128:(k + 1) * 128], identity=ident[:]),
                              reads=[hb_r, id_r], writes=[ps_r])
                    eng = self.evac_eng()
                    kb.op(eng, self.copy(eng, hT[:, half * 8:half * 8 + 8, j * 128:(j + 1) * 128],
                                         pb.rearrange("p (k t) -> p k t", t=128)),
                          reads=[ps_r], writes=[hT_r])
            kb.dma(kb.sp, dst[gidx, :, :, 0:ntok], hT[:, :, 0:ntok], reads=[hT_r], writes=[dst_res[gidx]])
        kb.end_phase()

    def make_ident(self, dtype):
        nc, kb = self.nc, self.kb
        idf, idf_r = kb.sbuf("idf", [128, 128], F32)
        kb.op(kb.pool, lambda: nc.gpsimd.memset(idf[:], 1.0), writes=[idf_r])
        kb.op(kb.pool, lambda: nc.gpsimd.affine_select(out=idf[:], in_=idf[:], pattern=[[-1, 128]],
                                                       compare_op=ALU.is_equal, fill=0.0, base=0,
                                                       channel_multiplier=1),
              reads=[idf_r], writes=[idf_r])
        if dtype == F32:
            return idf, idf_r
        idb, idb_r = kb.sbuf("idb", [128, 128], dtype)
        kb.op(kb.dve, lambda: nc.vector.tensor_copy(out=idb[:], in_=idf[:]), reads=[idf_r], writes=[idb_r])
        return idb, idb_r

    def phase_inproj(self):
        nc, kb = self.nc, self.kb
        kb.begin_phase()
        wv = self.w_in.rearrange("(k p) n -> p k n", p=128)
        wbdv = self.w_bd.rearrange("(k p) n -> p k n", p=128)
        own_groups = ([(0, 0, NCTX)] if self.need_ctx else []) + [(g, GROUPS[g][0], 512) for g in range(1, 9)]
        all_groups = [(g, GROUPS[g][0], GROUPS[g][1]) for g in range(len(GROUPS))]
        sets = [
            (C_AQ, 1024, PR_Q, own_groups),
            (C_AK, 256, PR_K, all_groups),
            (C_DQ, 1024, PR_DN, all_groups),
            (C_DV, 1024, PR_DN + 1024, all_groups),
            (C_LX, 1024, PR_LRU, all_groups),
        ]
        wss = [kb.sbuf("wset", [128, 16, 1024], BF16) for _ in range(2)]
        wvv, wvv_r = kb.sbuf("wv", [128, 16, 256], BF16)
        wbd, wbd_r = kb.sbuf("wbd", [128, 16, 16], BF16)
        kb.dma(kb.pool, wvv[:], wv[:, :, C_AV:C_AV + 256], writes=[wvv_r])
        kb.dma(kb.pool, wbd[:], wbdv, writes=[wbd_r])
        hTs = [kb.sbuf("hTl", [128, 16, 512], BF16) for _ in range(2)]
        evs = [kb.sbuf("ev", [128, 512], F32) for _ in range(4)]
        evb = [kb.sbuf("evb", [128, 256], BF16) for _ in range(2)]
        ei = 0
        hi = 0
        for si, (c0, ncols, r0, groups) in enumerate(sets):
            ws, ws_r = wss[si % 2]
            kb.dma(kb.pool, ws[:, :, 0:ncols], wv[:, :, c0:c0 + ncols], writes=[ws_r])
            for (g, t0, ntok) in groups:
                hT, hT_r = hTs[hi % 2]
                hi += 1
                kb.dma(kb.sp, hT[:, :, 0:ntok], self.hT_d[g, :, :, 0:ntok], reads=[self.r_hT[g]], writes=[hT_r])
                for ch in range(ncols // 128):
                    ps, ps_r = kb.bank()
                    for k in range(16):
                        kb.op(kb.pe, lambda: nc.tensor.matmul(ps[:, 0:ntok], lhsT=ws[:, k, ch * 128:(ch + 1) * 128],
                                                              rhs=hT[:, k, 0:ntok], start=(k == 0), stop=(k == 15)),
                              reads=[ws_r, hT_r], writes=[ps_r])
                    ev, ev_r = evs[ei % 4]
                    ei += 1
                    eng = self.evac_eng()
                    kb.op(eng, self.copy(eng, ev[:, 0:ntok], ps[:, 0:ntok]), reads=[ps_r], writes=[ev_r])
                    kb.dma(kb.sp, self.projT[r0 + ch * 128:r0 + (ch + 1) * 128, t0:t0 + ntok], ev[:, 0:ntok],
                           reads=[ev_r], mwrites=[self.r_projT])
                if c0 == C_AK:
                    ps, ps_r = kb.bank()
                    for k in range(16):
                        kb.op(kb.pe, lambda: nc.tensor.matmul(ps[0:16, 0:ntok], lhsT=wbd[:, k, :], rhs=hT[:, k, 0:ntok],
                                                              start=(k == 0), stop=(k == 15)),
                              reads=[wbd_r, hT_r], writes=[ps_r])
                    ev, ev_r = evs[ei % 4]
                    ei += 1
                    eng = self.evac_eng()
                    kb.op(eng, self.copy(eng, ev[0:16, 0:ntok], ps[0:16, 0:ntok]), reads=[ps_r], writes=[ev_r])
                    kb.dma(kb.sp, self.projT[PR_BD:PR_BD + 16, t0:t0 + ntok], ev[0:16, 0:ntok],
                           reads=[ev_r], mwrites=[self.r_projT])
                    for j in range(ntok // 128):
                        ps, ps_r = kb.bank()
                        for k in range(16):
                            kb.op(kb.pe, lambda: nc.tensor.matmul(ps[:, 0:256], lhsT=hT[:, k, j * 128:(j + 1) * 128],
                                                                  rhs=wvv[:, k, :], start=(k == 0), stop=(k == 15)),
                                  reads=[wvv_r, hT_r], writes=[ps_r])
                        eb, eb_r = evb[j % 2]
                        eng = self.evac_eng()
                        kb.op(eng, self.copy(eng, eb[:], ps[:, 0:256]), reads=[ps_r], writes=[eb_r])
                        kb.dma(kb.sp, self.vtok[t0 + j * 128:t0 + (j + 1) * 128, :], eb[:],
                               reads=[eb_r], mwrites=[self.r_vtok])
        kb.end_phase()


def present(a, half):
    return a if half == 0 else a[::-1]


def host_layer_inputs(inp, l, half=0):
    dsel = [0, 1] if half == 0 else [1, 0]
    m = {}
    m["w_mod"] = inp["w_mod"][l]
    m["b_mod"] = inp["b_mod"][l]
    m["gmix"] = inp["norm_mix_g"][l]
    m["gffn"] = inp["norm_ffn_g"][l]
    w_in = inp["w_in"][l]
    m["w_in"] = w_in
    pb = w_in[:, C_DB:C_DB + 8].reshape(D, 2, 4)[:, dsel, :].reshape(D, 8)
    pa = w_in[:, C_DA:C_DA + 8].reshape(D, 2, 4)[:, dsel, :].reshape(D, 8)
    m["w_bd"] = np.ascontiguousarray(np.concatenate([pb, pa], axis=1))
    m["sink"] = inp["attn_sink"][l]
    m["lru_cw"] = np.ascontiguousarray(inp["lru_conv_w"][l].T)
    m["lru_cb"] = np.ascontiguousarray(inp["lru_conv_b"][l].reshape(512, 1))
    wbd = np.zeros((2, 2, 4, 128, 128), np.float32)
    for d_ in range(2):
        for gt, nm in enumerate(("lru_w_rgate", "lru_w_igate")):
            w = inp[nm][l][dsel[d_]]
            for cc in range(4):
                wbd[d_, gt, cc, 0:64, 0:64] = w[2 * cc]
                wbd[d_, gt, cc, 64:128, 64:128] = w[2 * cc + 1]
    m["lru_wbd"] = wbd
    m["lru_bg"] = np.ascontiguousarray(np.stack([np.stack([inp["lru_b_rgate"][l][dsel[d_]], inp["lru_b_igate"][l][dsel[d_]]], 0)
                                                  for d_ in range(2)], 0).reshape(2, 2, 512, 1))
    m["lru_lam"] = np.ascontiguousarray(inp["lru_lambda"][l][dsel].reshape(2, 512, 1))
    m["dn_cw"] = np.ascontiguousarray(inp["dn_conv_w"][l].T)
    m["dn_ab"] = np.ascontiguousarray(np.concatenate([inp["dn_a_log"][l][dsel].reshape(-1), inp["dn_dt_bias"][l][dsel].reshape(-1)]))
    m["dn_ng"] = np.ascontiguousarray(inp["dn_norm_g"][l].reshape(128, 1))
    if "w_gu" in inp:
        m["w_out"] = inp["w_out"][l]
        m["w_router"] = inp["w_router"][l]
        m["b_router"] = inp["b_router"][l]
        m["w_gu"] = inp["w_gu"][l]
        m["b_guT"] = np.ascontiguousarray(inp["b_gu"][l].reshape(NE, 16, 128).transpose(0, 2, 1))
        m["w_down"] = inp["w_down"][l]
        m["b_down"] = inp["b_down"][l]
    return {f"l{l}_{k}": v for k, v in m.items()}


def host_shared_inputs(inp, b):
    m = {}
    m["xall"] = np.ascontiguousarray(np.concatenate([inp["ctx"][b], inp["x"][b]], 0), dtype=np.float32)
    m["c2"] = np.ascontiguousarray(np.stack([inp["c"][b], inp["c_ctx"]], 0))
    cos, sin, perm = rope_tables()
    m["ropec"], m["ropes"], m["permT"] = cos, sin, perm
    m["iota32"] = np.arange(NE, dtype=np.float32)
    m["iotap"] = np.arange(128, dtype=np.float32).reshape(128, 1)
    if "final_norm_g" in inp:
        m["final_g"] = inp["final_norm_g"]
    return m


def rope_tables():
    t = np.arange(NLAT)
    row = (t // 64).astype(np.float32)
    col = (t % 64).astype(np.float32)
    inv = (np.float32(10000.0) ** (-np.arange(0, 32, 2, dtype=np.float32) / np.float32(32))).astype(np.float32)
    cos = np.zeros((128, NLAT), np.float32)
    sin = np.zeros((128, NLAT), np.float32)
    perm = np.zeros((128, 128), np.float32)
    for m in range(128):
        dd = m % 64
        half, r = dd // 32, dd % 32
        j, second = r % 16, r // 16
        pos = row if half == 0 else col
        ang = (pos * inv[j]).astype(np.float32)
        cos[m] = np.cos(ang)
        sin[m] = np.sin(ang) if second else -np.sin(ang)
        partner = m - 16 if second else m + 16
        perm[partner, m] = 1.0
    return cos, sin, perm


def declare_attn(self):
    i = self.inp
    self.ropec = i("ropec", [128, NLAT])
    self.ropes = i("ropes", [128, NLAT])
    self.permT = i("permT", [128, 128])
    self.sink = i("sink", [16])
    kb = self.kb
    self.qr = kb.dram("qr", [1024, T], BF16)
    self.kr = kb.dram("kr", [256, T], BF16)
    self.mixT = kb.dram("mixT", [2048, T], BF16)
    self.r_qr, self.r_kr, self.r_mixT = kb.res("qr"), kb.res("kr"), kb.res("mixT")


def phase_rope(self):
    nc, kb = self.nc, self.kb
    kb.begin_phase()
    pm, pm_r = kb.sbuf("perm", [128, 128], BF16)
    kb.dma(kb.pool, pm[:], self.permT, writes=[pm_r])
    xs = [kb.sbuf("rx", [128, 512], F32) for _ in range(2)]
    xbs = [kb.sbuf("rxb", [128, 512], BF16) for _ in range(2)]
    cs = [kb.sbuf("rc", [128, 512], F32) for _ in range(2)]
    sn = [kb.sbuf("rs", [128, 512], F32) for _ in range(2)]
    t1s = [kb.sbuf("rt1", [128, 512], F32) for _ in range(2)]
    t2s = [kb.sbuf("rt2", [128, 512], F32) for _ in range(2)]
    obs = [kb.sbuf("rob", [128, 512], BF16) for _ in range(2)]
    it = 0
    for g in range(len(GROUPS)):
        t0, ntok = GROUPS[g]
        if g > 0:
            c_, c_r = cs[g % 2]
            s_, s_r = sn[g % 2]
            kb.dma(kb.sp, c_[:], self.ropec[:, t0 - NCTX:t0 - NCTX + 512], writes=[c_r])
            kb.dma(kb.sp, s_[:], self.ropes[:, t0 - NCTX:t0 - NCTX + 512], writes=[s_r])
        for (src0, dst, dst_r, nch) in ((PR_Q, self.qr, self.r_qr, 8), (PR_K, self.kr, self.r_kr, 2)):
            if g == 0 and src0 == PR_Q and not self.need_ctx:
                continue
            for ch in range(nch):
                x, x_r = xs[it % 2]
                xb, xb_r = xbs[it % 2]
                ob, ob_r = obs[it % 2]
                t1, t1_r = t1s[it % 2]
                t2, t2_r = t2s[it % 2]
                it += 1
                kb.dma(kb.act, x[:, 0:ntok], self.projT[src0 + ch * 128:src0 + (ch + 1) * 128, t0:t0 + ntok],
                       reads=[self.r_projT], writes=[x_r])
                if g == 0:
                    kb.op(kb.act, lambda: nc.scalar.copy(out=ob[:, 0:ntok], in_=x[:, 0:ntok]), reads=[x_r], writes=[ob_r])
                else:
                    kb.op(kb.act, lambda: nc.scalar.copy(out=xb[:], in_=x[:]), reads=[x_r], writes=[xb_r])
                    ps, ps_r = kb.bank()
                    kb.op(kb.pe, lambda: nc.tensor.matmul(ps[:], lhsT=pm[:], rhs=xb[:], start=True, stop=True),
                          reads=[pm_r, xb_r], writes=[ps_r])
                    kb.op(kb.pool, lambda: nc.gpsimd.tensor_tensor(out=t1[:], in0=x[:], in1=c_[:], op=ALU.mult),
                          reads=[x_r, c_r], writes=[t1_r])
                    kb.op(kb.dve, lambda: nc.vector.tensor_tensor(out=t2[:], in0=ps[:], in1=s_[:], op=ALU.mult),
                          reads=[ps_r, s_r], writes=[t2_r])
                    kb.op(kb.dve, lambda: nc.vector.tensor_tensor(out=ob[:], in0=t1[:], in1=t2[:], op=ALU.add),
                          reads=[t1_r, t2_r], writes=[ob_r])
                kb.dma(kb.sp, dst[ch * 128:(ch + 1) * 128, t0:t0 + ntok], ob[:, 0:ntok], reads=[ob_r], mwrites=[dst_r])
    kb.end_phase()


def phase_attn(self):
    nc, kb = self.nc, self.kb
    kb.begin_phase()
    ones, ones_r = kb.sbuf("ones", [128, 128], BF16)
    kb.op(kb.pool, lambda: nc.gpsimd.memset(ones[:], 1.0), writes=[ones_r])
    mf, mf_r = kb.sbuf("mf", [128, 128], F32)
    masks = []
    for (pat, cm) in (([[-1, 128]], 1), ([[1, 128]], -1)):
        mk, mk_r = kb.sbuf("mask", [128, 128], BF16)
        kb.op(kb.pool, lambda: nc.gpsimd.memset(mf[:], 1.0), writes=[mf_r])
        kb.op(kb.pool, lambda: nc.gpsimd.affine_select(out=mf[:], in_=mf[:], pattern=pat, compare_op=ALU.is_ge,
                                                       fill=0.0, base=0, channel_multiplier=cm),
              reads=[mf_r], writes=[mf_r])
        kb.op(kb.pool, lambda: nc.gpsimd.tensor_copy(out=mk[:], in_=mf[:]), reads=[mf_r], writes=[mk_r])
        masks.append((mk, mk_r))
    se, se_r = kb.sbuf("sinke", [128, 16], F32)
    kb.dma(kb.sp, se[:], self.sink.partition_broadcast(128), writes=[se_r])
    kb.op(kb.act, lambda: nc.scalar.activation(out=se[:], in_=se[:], func=AF.Exp), reads=[se_r], writes=[se_r])
    NT = T // 128
    V, V_r = kb.sbuf("V", [128, NT, 256], BF16)
    kb.dma(kb.sp, V[:], self.vtok.rearrange("(n p) c -> p n c", p=128), reads=[self.r_vtok], writes=[V_r])
    qT, qT_r = kb.sbuf("qT", [64, 4, T], BF16)
    kT, kT_r = kb.sbuf("kT", [64, T], BF16)
    Es = [kb.sbuf("E", [128, 512], BF16) for _ in range(10)]
    dens = [kb.sbuf("den", [64, 512], F32) for _ in range(2)]
    outs = [kb.sbuf("ao", [64, 512], BF16) for _ in range(2)]
    qv = self.qr.rearrange("(h d) t -> d h t", d=64)
    mv = self.mixT.rearrange("(h d) t -> d h t", d=64)
    ei = 0
    bi = 0
    qblocks = ([("c", 0), ("c", 1)] if self.need_ctx else []) + [("l", i) for i in range(NLAT // 128)]
    for g in range(4):
        kb.dma(kb.sp, qT[:], qv[:, 4 * g:4 * g + 4, :], reads=[self.r_qr], writes=[qT_r])
        kb.dma(kb.act, kT[:], self.kr[g * 64:(g + 1) * 64, :], reads=[self.r_kr], writes=[kT_r])
        for (kind, i) in qblocks:
            if kind == "c":
                tq = i * 128
                chunks = [(0, None), (128, None)]
            else:
                tq = NCTX + i * 128
                chunks = []
                if i > 0:
                    chunks.append((tq - 128, masks[0]))
                chunks.append((tq, None))
                if i < NLAT // 128 - 1:
                    chunks.append((tq + 128, masks[1]))
                chunks += [(0, None), (128, None)]
            Ec = []
            for (tk, mask) in chunks:
                ps, ps_r = kb.bank()
                kb.op(kb.pe, lambda: nc.tensor.matmul(ps[:].rearrange("p (h q) -> p h q", h=4),
                                                      lhsT=kT[:, tk:tk + 128], rhs=qT[:, :, tq:tq + 128],
                                                      start=True, stop=True),
                      reads=[kT_r, qT_r], writes=[ps_r])
                E, E_r = Es[ei % 10]
                ei += 1
                kb.op(kb.act, lambda: nc.scalar.activation(out=E[:], in_=ps[:], func=AF.Exp, scale=0.125),
                      reads=[ps_r], writes=[E_r])
                if mask is not None:
                    mk, mk_r = mask
                    kb.op(kb.pool, lambda: nc.gpsimd.tensor_tensor(
                        out=E[:].rearrange("p (h q) -> p h q", h=4), in0=E[:].rearrange("p (h q) -> p h q", h=4),
                        in1=mk[:].unsqueeze(1).to_broadcast([128, 4, 128]), op=ALU.mult),
                        reads=[E_r, mk_r], writes=[E_r])
                Ec.append((E, E_r, tk))
            psD, psD_r = kb.bank()
            for ci, (E, E_r, tk) in enumerate(Ec):
                kb.op(kb.pe, lambda: nc.tensor.matmul(psD[:], lhsT=ones[:], rhs=E[:], start=(ci == 0),
                                                      stop=(ci == len(Ec) - 1)),
                      reads=[ones_r, E_r], writes=[psD_r])
            den, den_r = dens[bi % 2]
            ao, ao_r = outs[bi % 2]
            bi += 1
            kb.op(kb.dve, lambda: nc.vector.tensor_tensor(
                out=den[:].rearrange("p (h q) -> p h q", h=4), in0=psD[0:64, :].rearrange("p (h q) -> p h q", h=4),
                in1=se[0:64, 4 * g:4 * g + 4].unsqueeze(2).to_broadcast([64, 4, 128]), op=ALU.add),
                reads=[psD_r, se_r], writes=[den_r])
            kb.op(kb.dve, lambda: nc.vector.reciprocal(out=den[:], in_=den[:]), reads=[den_r], writes=[den_r])
            psO, psO_r = kb.bank()
            for hh in range(4):
                for ci, (E, E_r, tk) in enumerate(Ec):
                    kb.op(kb.pe, lambda: nc.tensor.matmul(psO[0:64, hh * 128:(hh + 1) * 128],
                                                          lhsT=V[:, tk // 128, g * 64:(g + 1) * 64],
                                                          rhs=E[:, hh * 128:(hh + 1) * 128],
                                                          start=(ci == 0), stop=(ci == len(Ec) - 1)),
                          reads=[V_r, E_r], writes=[psO_r])
            kb.op(kb.dve, lambda: nc.vector.tensor_tensor(out=ao[:], in0=psO[0:64, :], in1=den[:], op=ALU.mult),
                  reads=[psO_r, den_r], writes=[ao_r])
            kb.dma(kb.sp, mv[:, 4 * g:4 * g + 4, tq:tq + 128], ao[:].rearrange("p (h q) -> p h q", h=4),
                   reads=[ao_r], mwrites=[self.r_mixT])
    kb.end_phase()


Layer.declare_attn = declare_attn
Layer.phase_rope = phase_rope
Layer.phase_attn = phase_attn


def declare_lru(self):
    i = self.inp
    self.lru_cw = i("lru_cw", [512, 4])
    self.lru_cb = i("lru_cb", [512, 1])
    self.lru_wbd = i("lru_wbd", [2, 2, 4, 128, 128])
    self.lru_bg = i("lru_bg", [2, 2, 512, 1])
    self.lru_lam = i("lru_lam", [2, 512, 1])


def gelu_tanh(self, out, x, tmp, x_r, tmp_r, out_r):
    nc, kb = self.nc, self.kb
    kb.op(kb.pool, lambda: nc.gpsimd.tensor_tensor(out=tmp, in0=x, in1=x, op=ALU.mult), reads=[x_r], writes=[tmp_r])
    kb.op(kb.dve, lambda: nc.vector.tensor_scalar(out=tmp, in0=tmp, scalar1=0.044715, scalar2=1.0, op0=ALU.mult,
                                                  op1=ALU.add), reads=[tmp_r], writes=[tmp_r])
    kb.op(kb.dve, lambda: nc.vector.tensor_tensor(out=tmp, in0=tmp, in1=x, op=ALU.mult), reads=[tmp_r, x_r],
          writes=[tmp_r])
    kb.op(kb.act, lambda: nc.scalar.activation(out=tmp, in_=tmp, func=AF.Sigmoid, scale=1.5957691216),
          reads=[tmp_r], writes=[tmp_r])
    kb.op(kb.dve, lambda: nc.vector.tensor_tensor(out=out, in0=tmp, in1=x, op=ALU.mult), reads=[tmp_r, x_r],
          writes=[out_r])


def phase_lru(self):
    nc, kb = self.nc, self.kb
    kb.begin_phase()
    W = T + 8
    CX, LX = 2, 2 + NCTX + 4
    Xp, Xp_r = kb.sbuf("lXp", [128, W], F32)
    U, U_r = kb.sbuf("lU", [128, T], F32)
    Rt, R_r = kb.sbuf("lR", [128, T], F32)
    It, I_r = kb.sbuf("lI", [128, T], F32)
    HF, HF_r = kb.sbuf("lHF", [128, T], F32)
    HB, HB_r = kb.sbuf("lHB", [128, T], F32)
    ob, ob_r = kb.sbuf("lob", [128, T], BF16)
    cw, cw_r = kb.sbuf("lcw", [128, 4], F32)
    cb, cb_r = kb.sbuf("lcb", [128, 1], F32)
    wbd, wbd_r = kb.sbuf("lwbd", [128, 4, 128], F32)
    bg, bg_r = kb.sbuf("lbg", [128, 4], F32)
    lam, lam_r = kb.sbuf("llam", [128, 4], F32)
    kb.op(kb.pool, lambda: nc.gpsimd.memset(Xp[:], 0.0), writes=[Xp_r])
    segs = [(0, NCTX, CX), (NCTX, NLAT, LX)]
    for cc in range(4):
        ch = slice(cc * 128, (cc + 1) * 128)
        kb.dma(kb.sp, cw[:], self.lru_cw[ch, :], writes=[cw_r])
        kb.dma(kb.sp, cb[:], self.lru_cb[ch, :], writes=[cb_r])
        for d in range(2):
            for gt in range(2):
                kb.dma(kb.sp, wbd[:, d * 2 + gt, :], self.lru_wbd[d, gt, cc], writes=[wbd_r])
                kb.dma(kb.sp, bg[:, d * 2 + gt:d * 2 + gt + 1], self.lru_bg[d, gt, ch, :], writes=[bg_r])
            kb.dma(kb.sp, lam[:, d:d + 1], self.lru_lam[d, ch, :], writes=[lam_r])
        kb.op(kb.act, lambda: nc.scalar.activation(out=lam[:, 2:4], in_=lam[:, 0:2], func=AF.Exp, scale=-1.0),
              reads=[lam_r], writes=[lam_r])
        kb.op(kb.act, lambda: nc.scalar.activation(out=lam[:, 2:4], in_=lam[:, 2:4], func=AF.Ln, bias=1.0),
              reads=[lam_r], writes=[lam_r])
        kb.op(kb.dve, lambda: nc.vector.tensor_scalar(out=lam[:, 2:4], in0=lam[:, 2:4], scalar1=-8.0, scalar2=None,
                                                      op0=ALU.mult), reads=[lam_r], writes=[lam_r])
        for (a0, a1) in ((0, CX), (CX + NCTX, LX), (LX + NLAT, W)):
            kb.op(kb.pool, lambda: nc.gpsimd.memset(Xp[:, a0:a1], 0.0), reads=[Xp_r], writes=[Xp_r])
        for (t0, n, p0) in segs:
            kb.dma(kb.sp, Xp[:, p0:p0 + n], self.projT[PR_LRU + cc * 128:PR_LRU + (cc + 1) * 128, t0:t0 + n],
                   reads=[self.r_projT], writes=[Xp_r])
        for (t0, n, p0) in segs:
            kb.op(kb.dve, lambda: nc.vector.tensor_scalar(out=U[:, t0:t0 + n], in0=Xp[:, p0 - 2:p0 - 2 + n],
                                                          scalar1=cw[:, 0:1], scalar2=cb[:, 0:1], op0=ALU.mult,
                                                          op1=ALU.add), reads=[Xp_r, cw_r, cb_r], writes=[U_r])
            for o in range(1, 4):
                kb.op(kb.dve, lambda: nc.vector.scalar_tensor_tensor(out=U[:, t0:t0 + n],
                                                                     in0=Xp[:, p0 - 2 + o:p0 - 2 + o + n],
                                                                     scalar=cw[:, o:o + 1], in1=U[:, t0:t0 + n],
                                                                     op0=ALU.mult, op1=ALU.add),
                      reads=[Xp_r, cw_r, U_r], writes=[U_r])
        Y = Xp[:, 0:T]
        kb.dma(kb.sp, Y, self.projT[PR_LRU + 512 + cc * 128:PR_LRU + 512 + (cc + 1) * 128, :],
               reads=[self.r_projT, U_r], writes=[Xp_r])
        for d in range(2):
            H, H_r = (HF, HF_r) if d == 0 else (HB, HB_r)
            for gt, (dst, dst_r) in enumerate(((Rt, R_r), (It, I_r))):
                for (t0, ntok) in GROUPS:
                    ps, ps_r = kb.bank()
                    kb.op(kb.pe, lambda: nc.tensor.matmul(ps[:, 0:ntok], lhsT=wbd[:, d * 2 + gt, :], rhs=U[:, t0:t0 + ntok],
                                                          start=True, stop=True), reads=[wbd_r, U_r], writes=[ps_r])
                    kb.op(kb.act, lambda: nc.scalar.activation(out=dst[:, t0:t0 + ntok], in_=ps[:, 0:ntok],
                                                               func=AF.Sigmoid, bias=bg[:, d * 2 + gt:d * 2 + gt + 1]),
                          reads=[ps_r, bg_r], writes=[dst_r])
            kb.op(kb.act, lambda: nc.scalar.activation(out=Rt[:], in_=Rt[:], func=AF.Exp, scale=lam[:, 2 + d:3 + d]),
                  reads=[R_r, lam_r], writes=[R_r])
            tmp, tmp_r = HB, HB_r
            kb.op(kb.pool, lambda: nc.gpsimd.tensor_tensor(out=tmp[:], in0=Rt[:], in1=Rt[:], op=ALU.mult),
                  reads=[R_r], writes=[tmp_r])
            kb.op(kb.dve, lambda: nc.vector.tensor_scalar(out=tmp[:], in0=tmp[:], scalar1=1.0, scalar2=None, op0=ALU.min),
                  reads=[tmp_r], writes=[tmp_r])
            kb.op(kb.act, lambda: nc.scalar.activation(out=tmp[:], in_=tmp[:], func=AF.Sqrt, scale=-1.0, bias=1.0),
                  reads=[tmp_r], writes=[tmp_r])
            kb.op(kb.dve, lambda: nc.vector.tensor_tensor(out=It[:], in0=It[:], in1=U[:], op=ALU.mult),
                  reads=[I_r, U_r], writes=[I_r])
            kb.op(kb.dve, lambda: nc.vector.tensor_tensor(out=It[:], in0=It[:], in1=tmp[:], op=ALU.mult),
                  reads=[I_r, tmp_r], writes=[I_r])
            if d == 0:
                kb.op(kb.dve, lambda: nc.vector.tensor_tensor_scan(out=H[:, 0:NCTX], data0=Rt[:, 0:NCTX], data1=It[:, 0:NCTX],
                                                                   initial=0.0, op0=ALU.mult, op1=ALU.add),
                      reads=[R_r, I_r], writes=[H_r])
                kb.op(kb.dve, lambda: nc.vector.tensor_tensor_scan(out=H[:, NCTX:T], data0=Rt[:, NCTX:T], data1=It[:, NCTX:T],
                                                                   initial=H[:, NCTX - 1:NCTX], op0=ALU.mult, op1=ALU.add),
                      reads=[R_r, I_r, H_r], writes=[H_r])
            else:
                kb.op(kb.dve, lambda: nc.vector.tensor_tensor_scan(out=H[:, 0:NCTX][:, ::-1],
                                                                   data0=Rt[:, 0:NCTX][:, ::-1], data1=It[:, 0:NCTX][:, ::-1],
                                                                   initial=0.0, op0=ALU.mult, op1=ALU.add),
                      reads=[R_r, I_r], writes=[H_r])
                kb.op(kb.dve, lambda: nc.vector.tensor_tensor_scan(out=H[:, NCTX:T][:, ::-1], data0=Rt[:, NCTX:T][:, ::-1],
                                                                   data1=It[:, NCTX:T][:, ::-1], initial=H[:, 0:1],
                                                                   op0=ALU.mult, op1=ALU.add),
                      reads=[R_r, I_r, H_r], writes=[H_r])
        kb.op(kb.pool, lambda: nc.gpsimd.tensor_tensor(out=HF[:], in0=HF[:], in1=HB[:], op=ALU.add),
              reads=[HF_r, HB_r], writes=[HF_r])
        gelu_tanh(self, HB[:], Y, Rt[:], Xp_r, R_r, HB_r)
        kb.op(kb.dve, lambda: nc.vector.tensor_tensor(out=ob[:], in0=HF[:], in1=HB[:], op=ALU.mult),
              reads=[HF_r, HB_r], writes=[ob_r])
        kb.dma(kb.sp, self.mixT[1536 + cc * 128:1536 + (cc + 1) * 128, :], ob[:], reads=[ob_r], mwrites=[self.r_mixT])
    kb.end_phase()


Layer.declare_lru = declare_lru
Layer.phase_lru = phase_lru


def declare_dn(self):
    i = self.inp
    self.dn_cw = i("dn_cw", [1536, 4])
    self.dn_ab = i("dn_ab", [16])
    self.dn_ng = i("dn_ng", [128, 1])


def phase_dn(self):
    nc, kb = self.nc, self.kb
    kb.begin_phase()
    NCH = T // 128
    W = T + 8
    CX, LX = 2, 2 + NCTX + 4
    segs = [(0, NCTX, CX), (NCTX, NLAT, LX)]
    identF, id_r = self.make_ident(F32)
    onesF, ones_r = kb.sbuf("dones", [128, 128], F32)
    kb.op(kb.pool, lambda: nc.gpsimd.memset(onesF[:], 1.0), writes=[ones_r])
    bd64, bd_r = kb.sbuf("bd64", [128, 128], F32)
    kb.op(kb.pool, lambda: nc.gpsimd.memset(bd64[:], 0.0), writes=[bd_r])
    kb.op(kb.pool, lambda: nc.gpsimd.memset(bd64[0:64, 0:64], 1.0), reads=[bd_r], writes=[bd_r])
    kb.op(kb.pool, lambda: nc.gpsimd.memset(bd64[64:128, 64:128], 1.0), reads=[bd_r], writes=[bd_r])
    MASKS = []
    for d in range(2):
        pat, cm = ([[1, 128]], -1) if d == 0 else ([[-1, 128]], 1)
        mi, mi_r = kb.sbuf("mincl", [128, 128], F32)
        msd, msd_r = kb.sbuf("msd", [128, 128], F32)
        mso, mso_r = kb.sbuf("mso", [128, 128], F32)
        kb.op(kb.pool, lambda: nc.gpsimd.memset(mi[:], 1.0), writes=[mi_r])
        kb.op(kb.pool, lambda: nc.gpsimd.affine_select(out=mi[:], in_=mi[:], pattern=pat, compare_op=ALU.is_ge, fill=0.0,
                                                       base=0, channel_multiplier=cm), reads=[mi_r], writes=[mi_r])
        kb.op(kb.pool, lambda: nc.gpsimd.memset(mso[:], 1.0), writes=[mso_r])
        kb.op(kb.pool, lambda: nc.gpsimd.affine_select(out=mso[:], in_=mso[:], pattern=pat, compare_op=ALU.is_gt, fill=0.0,
                                                       base=0, channel_multiplier=cm), reads=[mso_r], writes=[mso_r])
        kb.op(kb.pool, lambda: nc.gpsimd.tensor_tensor(out=msd[:], in0=mso[:], in1=bd64[:], op=ALU.mult),
              reads=[mso_r, bd_r], writes=[msd_r])
        kb.op(kb.pool, lambda: nc.gpsimd.tensor_tensor(out=mso[:], in0=mso[:], in1=msd[:], op=ALU.subtract),
              reads=[mso_r, msd_r], writes=[mso_r])
        MASKS.append(((mi, mi_r), (msd, msd_r), (mso, mso_r)))
    ab, ab_r = kb.sbuf("dnab", [128, 32], F32)
    kb.dma(kb.sp, ab[:, 0:16], self.dn_ab.partition_broadcast(128), writes=[ab_r])
    kb.op(kb.act, lambda: nc.scalar.activation(out=ab[:, 16:24], in_=ab[:, 0:8], func=AF.Exp), reads=[ab_r], writes=[ab_r])
    kb.op(kb.dve, lambda: nc.vector.tensor_scalar(out=ab[:, 16:24], in0=ab[:, 16:24], scalar1=-1.0, scalar2=None,
                                                  op0=ALU.mult), reads=[ab_r], writes=[ab_r])
    ng, ng_r = kb.sbuf("dnng", [128, 1], F32)
    kb.dma(kb.sp, ng[:], self.dn_ng, writes=[ng_r])
    cw, cw_r = kb.sbuf("dncw", [128, 4], F32)
    Xp, Xp_r = kb.sbuf("dXp", [128, W], F32)
    kb.op(kb.pool, lambda: nc.gpsimd.memset(Xp[:], 0.0), writes=[Xp_r])
    QKV = [kb.sbuf("dq", [128, T], F32), kb.sbuf("dk", [128, T], F32), kb.sbuf("dv", [128, T], F32)]
    G0, G0_r = kb.sbuf("dG0", [128, T], F32)
    GC, GC_r = kb.sbuf("dGC", [128, T], F32)
    OT, OT_r = kb.sbuf("dOT", [128, T], F32)
    cols, cols_r = kb.sbuf("dcols", [128, 6, NCH], F32)
    junk, junk_r = kb.sbuf("djunk", [128, 128], F32)
    NP = 56
    pool_t = [kb.sbuf("dt", [128, 128], F32) for _ in range(NP)]
    pi = [0]

    def tl():
        t = pool_t[pi[0] % NP]
        pi[0] += 1
        return t
    Ss = [kb.sbuf("dS", [128, 128], F32) for _ in range(2)]
    ev_i = [0]

    def evac(out, in_, reads, writes):
        ev_i[0] += 1
        if ev_i[0] % 2:
            kb.op(kb.act, lambda: nc.scalar.copy(out=out, in_=in_), reads=reads, writes=writes)
        else:
            kb.op(kb.dve, lambda: nc.vector.tensor_copy(out=out, in_=in_), reads=reads, writes=writes)

    for h in range(4):
        for comp in range(3):
            r0 = PR_DN + comp * 512 + h * 128
            X, X_r = QKV[comp]
            kb.dma(kb.sp, cw[:], self.dn_cw[comp * 512 + h * 128:comp * 512 + (h + 1) * 128, :], writes=[cw_r])
            for (t0, n, p0) in segs:
                kb.dma(kb.sp, Xp[:, p0:p0 + n], self.projT[r0:r0 + 128, t0:t0 + n], reads=[self.r_projT], writes=[Xp_r])
            for (t0, n, p0) in segs:
                kb.op(kb.dve, lambda: nc.vector.tensor_scalar(out=X[:, t0:t0 + n], in0=Xp[:, p0 - 2:p0 - 2 + n],
                                                              scalar1=cw[:, 0:1], scalar2=None, op0=ALU.mult),
                      reads=[Xp_r, cw_r], writes=[X_r])
                for o in range(1, 4):
                    kb.op(kb.dve, lambda: nc.vector.scalar_tensor_tensor(out=X[:, t0:t0 + n],
                                                                         in0=Xp[:, p0 - 2 + o:p0 - 2 + o + n],
                                                                         scalar=cw[:, o:o + 1], in1=X[:, t0:t0 + n],
                                                                         op0=ALU.mult, op1=ALU.add),
                          reads=[Xp_r, cw_r, X_r], writes=[X_r])
            kb.op(kb.act, lambda: nc.scalar.activation(out=X[:], in_=X[:], func=AF.Silu), reads=[X_r], writes=[X_r])
            if comp < 2:
                for (t0, ntok) in GROUPS:
                    kb.op(kb.act, lambda: nc.scalar.activation(out=G0[:, t0:t0 + ntok], in_=X[:, t0:t0 + ntok], func=AF.Square),
                          reads=[X_r], writes=[G0_r])
                    ps, ps_r = kb.bank()
                    kb.op(kb.pe, lambda: nc.tensor.matmul(ps[:, 0:ntok], lhsT=onesF[:], rhs=G0[:, t0:t0 + ntok], start=True,
                                                          stop=True), reads=[ones_r, G0_r], writes=[ps_r])
                    kb.op(kb.dve, lambda: nc.vector.tensor_scalar(out=GC[:, t0:t0 + ntok], in0=ps[:, 0:ntok], scalar1=EPS,
                                                                  scalar2=None, op0=ALU.add), reads=[ps_r], writes=[GC_r])
                kb.op(kb.act, lambda: nc.scalar.sqrt(out=GC[:], in_=GC[:]), reads=[GC_r], writes=[GC_r])
                kb.op(kb.dve, lambda: nc.vector.reciprocal(out=GC[:], in_=GC[:]), reads=[GC_r], writes=[GC_r])
                sc = 128.0 ** -0.5 if comp == 0 else 1.0
                kb.op(kb.dve, lambda: nc.vector.scalar_tensor_tensor(out=X[:], in0=X[:], scalar=sc, in1=GC[:], op0=ALU.mult,
                                                                     op1=ALU.mult), reads=[X_r, GC_r], writes=[X_r])
        (qT, q_r), (kT, k_r), (vT, v_r) = QKV
        kb.op(kb.pool, lambda: nc.gpsimd.memset(OT[:], 0.0), writes=[OT_r])
        BB, BB_r = Xp[:, 0:T], Xp_r
        for d in range(2):
            (mi, mi_r), (msd, msd_r), (mso, mso_r) = MASKS[d]
            kb.dma(kb.sp, G0[:], self.projT[PR_BD + 8 + d * 4 + h, :].partition_broadcast(128), reads=[self.r_projT],
                   writes=[G0_r])
            kb.dma(kb.sp, BB, self.projT[PR_BD + d * 4 + h, :].partition_broadcast(128), reads=[self.r_projT],
                   writes=[BB_r])
            kb.op(kb.act, lambda: nc.scalar.activation(out=BB, in_=BB, func=AF.Sigmoid), reads=[BB_r], writes=[BB_r])
            dtb = ab[:, 8 + d * 4 + h:8 + d * 4 + h + 1]
            negA = ab[:, 16 + d * 4 + h:16 + d * 4 + h + 1]
            kb.op(kb.dve, lambda: nc.vector.tensor_scalar(out=G0[:], in0=G0[:], scalar1=dtb, scalar2=None, op0=ALU.add),
                  reads=[G0_r, ab_r], writes=[G0_r])
            kb.op(kb.act, lambda: nc.scalar.activation(out=GC[:], in_=G0[:], func=AF.Abs), reads=[G0_r], writes=[GC_r])
            kb.op(kb.act, lambda: nc.scalar.activation(out=GC[:], in_=GC[:], func=AF.Exp, scale=-1.0), reads=[GC_r], writes=[GC_r])
            kb.op(kb.act, lambda: nc.scalar.activation(out=GC[:], in_=GC[:], func=AF.Ln, bias=1.0), reads=[GC_r], writes=[GC_r])
            kb.op(kb.dve, lambda: nc.vector.tensor_scalar(out=G0[:], in0=G0[:], scalar1=0.0, scalar2=None, op0=ALU.max),
                  reads=[G0_r], writes=[G0_r])
            kb.op(kb.dve, lambda: nc.vector.tensor_tensor(out=G0[:], in0=G0[:], in1=GC[:], op=ALU.add),
                  reads=[G0_r, GC_r], writes=[G0_r])
            kb.op(kb.dve, lambda: nc.vector.tensor_scalar(out=G0[:], in0=G0[:], scalar1=negA, scalar2=None, op0=ALU.mult),
                  reads=[G0_r, ab_r], writes=[G0_r])
            for c in range(NCH):
                sl = slice(c * 128, (c + 1) * 128)
                if d == 0:
                    kb.op(kb.dve, lambda: nc.vector.tensor_tensor_scan(out=GC[:, sl], data0=onesF[:], data1=G0[:, sl],
                                                                       initial=0.0, op0=ALU.mult, op1=ALU.add),
                          reads=[G0_r, ones_r], writes=[GC_r])
                else:
                    kb.op(kb.dve, lambda: nc.vector.tensor_tensor_scan(out=GC[:, sl][:, ::-1], data0=onesF[:],
                                                                       data1=G0[:, sl][:, ::-1], initial=0.0,
                                                                       op0=ALU.mult, op1=ALU.add),
                          reads=[G0_r, ones_r], writes=[GC_r])
                kb.op(kb.dve, lambda: nc.vector.scalar_tensor_tensor(out=junk[:], in0=GC[:, sl], scalar=1.0, in1=identF[:],
                                                                     op0=ALU.mult, op1=ALU.mult, accum_out=cols[:, 0, c:c + 1]),
                      reads=[GC_r, id_r], writes=[junk_r, cols_r])
                kb.op(kb.dve, lambda: nc.vector.scalar_tensor_tensor(out=junk[:], in0=BB[:, sl], scalar=1.0, in1=identF[:],
                                                                     op0=ALU.mult, op1=ALU.mult, accum_out=cols[:, 1, c:c + 1]),
                      reads=[BB_r, id_r], writes=[junk_r, cols_r])
            lastoff = 127 if d == 0 else 0
            kb.op(kb.dve, lambda: nc.vector.tensor_copy(out=cols[:, 2, :], in_=GC[:, lastoff::128]), reads=[GC_r], writes=[cols_r])
            kb.op(kb.act, lambda: nc.scalar.activation(out=cols[:, 3, :], in_=cols[:, 2, :], func=AF.Exp), reads=[cols_r],
                  writes=[cols_r])
            kb.op(kb.dve, lambda: nc.vector.tensor_tensor(out=cols[:, 4, :], in0=cols[:, 2, :], in1=cols[:, 0, :], op=ALU.subtract),
                  reads=[cols_r], writes=[cols_r])
            kb.op(kb.act, lambda: nc.scalar.activation(out=cols[:, 4, :], in_=cols[:, 4, :], func=AF.Exp), reads=[cols_r],
                  writes=[cols_r])
            kb.op(kb.dve, lambda: nc.vector.tensor_scalar(out=cols[:, 5, :], in0=cols[:, 1, :], scalar1=-1.0, scalar2=None,
                                                          op0=ALU.mult), reads=[cols_r], writes=[cols_r])
            S, S_r = Ss[0]
            kb.op(kb.pool, lambda: nc.gpsimd.memset(S[:], 0.0), writes=[S_r])
            si = 0
            order = list(range(NCH)) if d == 0 else [1, 0] + list(range(NCH - 1, 1, -1))
            for c in order:
                sl = slice(c * 128, (c + 1) * 128)
                gcol, bcol, egl, dcol, nbcol = (cols[:, 0, c:c + 1], cols[:, 1, c:c + 1], cols[:, 3, c:c + 1],
                                                cols[:, 4, c:c + 1], cols[:, 5, c:c + 1])
                eg, eg_r = tl()
                kb.op(kb.act, lambda: nc.scalar.activation(out=eg[:], in_=GC[:, sl], func=AF.Exp), reads=[GC_r], writes=[eg_r])
                kgT, kg_r = tl()
                qgT, qg_r = tl()
                kb.op(kb.dve, lambda: nc.vector.tensor_tensor(out=kgT[:], in0=kT[:, sl], in1=eg[:], op=ALU.mult),
                      reads=[k_r, eg_r], writes=[kg_r])
                kb.op(kb.pool, lambda: nc.gpsimd.tensor_tensor(out=qgT[:], in0=qT[:, sl], in1=eg[:], op=ALU.mult),
                      reads=[q_r, eg_r], writes=[qg_r])
                kd, kd_r = tl()
                vb, vb_r = tl()
                ps, ps_r = kb.bank()
                kb.op(kb.pe, lambda: nc.tensor.transpose(out=ps[:, 0:128], in_=kT[:, sl], identity=identF[:]),
                      reads=[k_r, id_r], writes=[ps_r])
                kb.op(kb.act, lambda: nc.scalar.mul(out=kd[:], in_=ps[:, 0:128], mul=dcol), reads=[ps_r, cols_r], writes=[kd_r])
                ps, ps_r = kb.bank()
                kb.op(kb.pe, lambda: nc.tensor.transpose(out=ps[:, 0:128], in_=vT[:, sl], identity=identF[:]),
                      reads=[v_r, id_r], writes=[ps_r])
                kb.op(kb.act, lambda: nc.scalar.mul(out=vb[:], in_=ps[:, 0:128], mul=bcol), reads=[ps_r, cols_r], writes=[vb_r])
                Dm, Dm_r = tl()
                kb.op(kb.dve, lambda: nc.vector.tensor_scalar(out=Dm[:], in0=GC[:, sl], scalar1=gcol, scalar2=0.0,
                                                              op0=ALU.subtract, op1=ALU.min), reads=[GC_r, cols_r], writes=[Dm_r])
                kb.op(kb.act, lambda: nc.scalar.activation(out=Dm[:], in_=Dm[:], func=AF.Exp), reads=[Dm_r], writes=[Dm_r])
                Dmi, Dmi_r = tl()
                kb.op(kb.pool, lambda: nc.gpsimd.tensor_tensor(out=Dmi[:], in0=Dm[:], in1=mi[:], op=ALU.mult),
                      reads=[Dm_r, mi_r], writes=[Dmi_r])
                bD, bD_r = tl()
                kb.op(kb.pool, lambda: nc.gpsimd.tensor_tensor(out=bD[:], in0=Dm[:], in1=BB[:, sl], op=ALU.mult),
                      reads=[Dm_r, BB_r], writes=[bD_r])
                wd, wd_r = tl()
                wo, wo_r = tl()
                kb.op(kb.pool, lambda: nc.gpsimd.tensor_tensor(out=wd[:], in0=bD[:], in1=msd[:], op=ALU.mult),
                      reads=[bD_r, msd_r], writes=[wd_r])
                kb.op(kb.pool, lambda: nc.gpsimd.tensor_tensor(out=wo[:], in0=bD[:], in1=mso[:], op=ALU.mult),
                      reads=[bD_r, mso_r], writes=[wo_r])
                psKK, psKK_r = kb.bank()
                kb.op(kb.pe, lambda: nc.tensor.matmul(psKK[:, 0:128], lhsT=kT[:, sl], rhs=kT[:, sl], start=True, stop=True),
                      reads=[k_r], writes=[psKK_r])
                psQK, psQK_r = kb.bank()
                kb.op(kb.pe, lambda: nc.tensor.matmul(psQK[:, 0:128], lhsT=kT[:, sl], rhs=qT[:, sl], start=True, stop=True),
                      reads=[k_r, q_r], writes=[psQK_r])
                MT, MT_r = tl()
                kb.op(kb.dve, lambda: nc.vector.tensor_tensor(out=MT[:], in0=psQK[:, 0:128], in1=Dmi[:], op=ALU.mult),
                      reads=[psQK_r, Dmi_r], writes=[MT_r])
                Bd, Bd_r = tl()
                Bo, Bo_r = tl()
                kb.op(kb.dve, lambda: nc.vector.tensor_tensor(out=Bd[:], in0=psKK[:, 0:128], in1=wd[:], op=ALU.mult),
                      reads=[psKK_r, wd_r], writes=[Bd_r])
                kb.op(kb.dve, lambda: nc.vector.tensor_tensor(out=Bo[:], in0=psKK[:, 0:128], in1=wo[:], op=ALU.mult),
                      reads=[psKK_r, wo_r], writes=[Bo_r])
                Ad, Ad_r = tl()
                Ao, Ao_r = tl()
                for (src, src_r, dst, dst_r) in ((Bd, Bd_r, Ad, Ad_r), (Bo, Bo_r, Ao, Ao_r)):
                    ps, ps_r = kb.bank()
                    kb.op(kb.pe, lambda: nc.tensor.transpose(out=ps[:, 0:128], in_=src[:], identity=identF[:]),
                          reads=[src_r, id_r], writes=[ps_r])
                    evac(dst[:], ps[:, 0:128], [ps_r], [dst_r])
                PT, PT_r = tl()
                kb.op(kb.dve, lambda: nc.vector.tensor_tensor(out=PT[:], in0=identF[:], in1=Bd[:], op=ALU.subtract),
                      reads=[id_r, Bd_r], writes=[PT_r])
                Ak, Ak_r, Bk, Bk_r = Ad, Ad_r, Bd, Bd_r
                for lvl in range(5):
                    A2, A2_r = tl()
                    ps, ps_r = kb.bank()
                    kb.op(kb.pe, lambda: nc.tensor.matmul(ps[:, 0:128], lhsT=Bk[:], rhs=Ak[:], start=True, stop=True),
                          reads=[Bk_r, Ak_r], writes=[ps_r])
                    evac(A2[:], ps[:, 0:128], [ps_r], [A2_r])
                    if lvl < 4:
                        B2, B2_r = tl()
                        ps, ps_r = kb.bank()
                        kb.op(kb.pe, lambda: nc.tensor.matmul(ps[:, 0:128], lhsT=Ak[:], rhs=Bk[:], start=True, stop=True),
                              reads=[Bk_r, Ak_r], writes=[ps_r])
                        evac(B2[:], ps[:, 0:128], [ps_r], [B2_r])
                    ps, ps_r = kb.bank()
                    kb.op(kb.pe, lambda: nc.tensor.matmul(ps[:, 0:128], lhsT=A2[:], rhs=PT[:], start=True, stop=True),
                          reads=[A2_r, PT_r], writes=[ps_r])
                    PT2, PT2_r = tl()
                    kb.op(kb.dve, lambda: nc.vector.tensor_tensor(out=PT2[:], in0=PT[:], in1=ps[:, 0:128], op=ALU.add),
                          reads=[PT_r, ps_r], writes=[PT2_r])
                    PT, PT_r = PT2, PT2_r
                    Ak, Ak_r = A2, A2_r
                    if lvl < 4:
                        Bk, Bk_r = B2, B2_r
                T64, T64_r = tl()
                ps, ps_r = kb.bank()
                kb.op(kb.pe, lambda: nc.tensor.transpose(out=ps[:, 0:128], in_=PT[:], identity=identF[:]),
                      reads=[PT_r, id_r], writes=[ps_r])
                evac(T64[:], ps[:, 0:128], [ps_r], [T64_r])
                Y, Y_r = tl()
                ps, ps_r = kb.bank()
                kb.op(kb.pe, lambda: nc.tensor.matmul(ps[:, 0:128], lhsT=Ao[:], rhs=PT[:], start=True, stop=True),
                      reads=[Ao_r, PT_r], writes=[ps_r])
                evac(Y[:], ps[:, 0:128], [ps_r], [Y_r])
                ps, ps_r = kb.bank()
                kb.op(kb.pe, lambda: nc.tensor.matmul(ps[:, 0:128], lhsT=T64[:], rhs=Y[:], start=True, stop=True),
                      reads=[T64_r, Y_r], writes=[ps_r])
                TT, TT_r = tl()
                kb.op(kb.dve, lambda: nc.vector.tensor_tensor(out=TT[:], in0=PT[:], in1=ps[:, 0:128], op=ALU.subtract),
                      reads=[PT_r, ps_r], writes=[TT_r])
                S, S_r = Ss[si % 2]
                Sn, Sn_r = Ss[(si + 1) % 2]
                si += 1
                ps, ps_r = kb.bank()
                kb.op(kb.pe, lambda: nc.tensor.matmul(ps[:, 0:128], lhsT=kgT[:], rhs=S[:], start=True, stop=True),
                      reads=[kg_r, S_r], writes=[ps_r])
                R, R_r = tl()
                kb.op(kb.dve, lambda: nc.vector.scalar_tensor_tensor(out=R[:], in0=ps[:, 0:128], scalar=nbcol, in1=vb[:],
                                                                     op0=ALU.mult, op1=ALU.add),
                      reads=[ps_r, cols_r, vb_r], writes=[R_r])
                ps, ps_r = kb.bank()
                kb.op(kb.pe, lambda: nc.tensor.matmul(ps[:, 0:128], lhsT=TT[:], rhs=R[:], start=True, stop=True),
                      reads=[TT_r, R_r], writes=[ps_r])
                vn, vn_r = tl()
                kb.op(kb.act, lambda: nc.scalar.copy(out=vn[:], in_=ps[:, 0:128]), reads=[ps_r], writes=[vn_r])
                ps, ps_r = kb.bank()
                kb.op(kb.pe, lambda: nc.tensor.matmul(ps[:, 0:128], lhsT=kd[:], rhs=vn[:], start=True, stop=True),
                      reads=[kd_r, vn_r], writes=[ps_r])
                kb.op(kb.dve, lambda: nc.vector.scalar_tensor_tensor(out=Sn[:], in0=S[:], scalar=egl, in1=ps[:, 0:128],
                                                                     op0=ALU.mult, op1=ALU.add),
                      reads=[S_r, cols_r, ps_r], writes=[Sn_r])
                psO, psO_r = kb.bank()
                kb.op(kb.pe, lambda: nc.tensor.matmul(psO[:, 0:128], lhsT=S[:], rhs=qgT[:], start=True, stop=False),
                      reads=[S_r, qg_r], writes=[psO_r])
                kb.op(kb.pe, lambda: nc.tensor.matmul(psO[:, 0:128], lhsT=vn[:], rhs=MT[:], start=False, stop=True),
                      reads=[vn_r, MT_r], writes=[psO_r])
                kb.op(kb.dve, lambda: nc.vector.tensor_tensor(out=OT[:, sl], in0=OT[:, sl], in1=psO[:, 0:128], op=ALU.add),
                      reads=[OT_r, psO_r], writes=[OT_r])
        kb.dma(kb.sp, G0[:], self.projT[PR_DN + 1536 + h * 128:PR_DN + 1536 + (h + 1) * 128, :], reads=[self.r_projT],
               writes=[G0_r])
        kb.op(kb.act, lambda: nc.scalar.activation(out=G0[:], in_=G0[:], func=AF.Silu), reads=[G0_r], writes=[G0_r])
        for (t0, ntok) in GROUPS:
            kb.op(kb.act, lambda: nc.scalar.activation(out=GC[:, t0:t0 + ntok], in_=OT[:, t0:t0 + ntok], func=AF.Square),
                  reads=[OT_r], writes=[GC_r])
            ps, ps_r = kb.bank()
            kb.op(kb.pe, lambda: nc.tensor.matmul(ps[:, 0:ntok], lhsT=onesF[:], rhs=GC[:, t0:t0 + ntok], start=True, stop=True),
                  reads=[ones_r, GC_r], writes=[ps_r])
            kb.op(kb.dve, lambda: nc.vector.tensor_scalar(out=Xp[:, t0:t0 + ntok], in0=ps[:, 0:ntok], scalar1=1.0 / 128,
                                                          scalar2=EPS, op0=ALU.mult, op1=ALU.add), reads=[ps_r], writes=[Xp_r])
        kb.op(kb.act, lambda: nc.scalar.sqrt(out=Xp[:, 0:T], in_=Xp[:, 0:T]), reads=[Xp_r], writes=[Xp_r])
        kb.op(kb.dve, lambda: nc.vector.reciprocal(out=Xp[:, 0:T], in_=Xp[:, 0:T]), reads=[Xp_r], writes=[Xp_r])
        kb.op(kb.dve, lambda: nc.vector.tensor_tensor(out=OT[:], in0=OT[:], in1=Xp[:, 0:T], op=ALU.mult),
              reads=[OT_r, Xp_r], writes=[OT_r])
        ob = GC[:].bitcast(BF16)[:, 0:T]
        kb.op(kb.dve, lambda: nc.vector.scalar_tensor_tensor(out=ob, in0=OT[:], scalar=ng[:, 0:1], in1=G0[:], op0=ALU.mult,
                                                             op1=ALU.mult), reads=[OT_r, ng_r, G0_r], writes=[GC_r])
        kb.dma(kb.sp, self.mixT[1024 + h * 128:1024 + (h + 1) * 128, :], ob, reads=[GC_r], mwrites=[self.r_mixT])
        kb.op(kb.pool, lambda: nc.gpsimd.memset(Xp[:], 0.0), reads=[Xp_r], writes=[Xp_r])
    kb.end_phase()


Layer.declare_dn = declare_dn
Layer.phase_dn = phase_dn


CAP = T
NSLOT = NE * CAP


def declare_ffn(self):
    i = self.inp
    self.w_out = i("w_out", [D, D])
    self.w_router = i("w_router", [D, NE])
    self.b_router = i("b_router", [NE])
    self.w_gu = i("w_gu", [NE, D, 2 * DE])
    self.b_guT = i("b_guT", [NE, 128, 16])
    self.w_down = i("w_down", [NE, DE, D])
    self.b_down = i("b_down", [NE, D])
    self.iota32 = i("iota32", [NE])
    self.iotap = i("iotap", [128, 1])
    kb = self.kb
    self.x1 = kb.dram("x1", [T, D], F32)
    self.r_x1 = kb.res("x1")
    self.gd = kb.dram("gd", [T, NE], F32)
    self.r_gd = kb.res("gd")
    self.yacc = kb.dram("yacc", [T, D], F32)
    self.r_yacc = [kb.res(f"yacc{j}") for j in range(T // 128)]
    self.hT2_d = kb.dram("hT2_d", [len(GROUPS), 128, 16, 512], BF16)
    self.r_hT2 = [kb.res(f"hT2_{g}") for g in range(len(GROUPS))]


def own_tiles(self):
    tiles = [(j * 128, 1) for j in range(NCTX // 128)] if self.need_ctx else []
    return tiles + [(NCTX + j * 128, 0) for j in range(NLAT // 128)]


def phase_outproj(self, xsrc, xsrc_r):
    nc, kb = self.nc, self.kb
    kb.begin_phase()
    wo, wo_r = kb.sbuf("wo", [128, 16, D], BF16)
    wv = self.w_out.rearrange("(k p) n -> p k n", p=128)
    for q in range(4):
        kb.dma(kb.pool, wo[:, :, q * 512:(q + 1) * 512], wv[:, :, q * 512:(q + 1) * 512], mwrites=[wo_r])
    gts = []
    for s in range(2):
        gt, gt_r = kb.sbuf("gate1", [128, D], F32)
        kb.dma(kb.sp, gt[:], self.modbc[s * 6 + 2], reads=[self.r_modbc], writes=[gt_r])
        gts.append((gt, gt_r))
    mts = [kb.sbuf("mt", [128, 16, 128], BF16) for _ in range(2)]
    xts = [kb.sbuf("xo", [128, D], F32) for _ in range(2)]
    tms = [kb.sbuf("tm", [128, D], F32) for _ in range(2)]
    mv = self.mixT.rearrange("(k p) t -> p k t", p=128)
    for ti, (t0, s) in enumerate(own_tiles(self)):
        mt, mt_r = mts[ti % 2]
        xt, xt_r = xts[ti % 2]
        tm, tm_r = tms[ti % 2]
        gt, gt_r = gts[s]
        kb.dma(kb.sp, mt[:], mv[:, :, t0:t0 + 128], reads=[self.r_mixT], writes=[mt_r])
        kb.dma(kb.act, xt[:], xsrc[t0:t0 + 128, :], reads=[xsrc_r], writes=[xt_r])
        for q in range(4):
            ps, ps_r = kb.bank()
            for k in range(16):
                kb.op(kb.pe, lambda: nc.tensor.matmul(ps[:], lhsT=mt[:, k, :], rhs=wo[:, k, q * 512:(q + 1) * 512],
                                                      start=(k == 0), stop=(k == 15)), reads=[mt_r, wo_r], writes=[ps_r])
            kb.op(kb.dve, lambda: nc.vector.tensor_tensor(out=tm[:, q * 512:(q + 1) * 512], in0=ps[:],
                                                          in1=gt[:, q * 512:(q + 1) * 512], op=ALU.mult),
                  reads=[ps_r, gt_r], writes=[tm_r])
        kb.op(kb.pool, lambda: nc.gpsimd.tensor_tensor(out=tm[:], in0=tm[:], in1=xt[:], op=ALU.add),
              reads=[tm_r, xt_r], writes=[tm_r])
        kb.dma(kb.sp, self.x1[t0:t0 + 128, :], tm[:], reads=[tm_r], mwrites=[self.r_x1])
    kb.end_phase()


def phase_route(self):
    nc, kb = self.nc, self.kb
    kb.begin_phase()
    identF, id_r = self.make_ident(F32)
    onesF, ones_r = kb.sbuf("rones", [128, 128], F32)
    kb.op(kb.pool, lambda: nc.gpsimd.memset(onesF[:], 1.0), writes=[ones_r])
    ustr, us_r = kb.sbuf("ustr", [128, 128], F32)
    kb.op(kb.pool, lambda: nc.gpsimd.memset(ustr[:], 1.0), writes=[us_r])
    kb.op(kb.pool, lambda: nc.gpsimd.affine_select(out=ustr[:], in_=ustr[:], pattern=[[1, 128]], compare_op=ALU.is_gt,
                                                   fill=0.0, base=0, channel_multiplier=-1), reads=[us_r], writes=[us_r])
    iot, iot_r = kb.sbuf("iota", [128, NE], F32)
    kb.dma(kb.sp, iot[:], self.iota32.partition_broadcast(128), writes=[iot_r])
    wr, wr_r = kb.sbuf("wr", [128, 16, NE], F32)
    kb.dma(kb.sp, wr[:], self.w_router.rearrange("(k p) n -> p k n", p=128), writes=[wr_r])
    br, br_r = kb.sbuf("br", [128, NE], F32)
    kb.dma(kb.sp, br[:], self.b_router.partition_broadcast(128), writes=[br_r])
    gb, gb_r = kb.sbuf("gb2", [128, D], F32)
    kb.dma(kb.sp, gb[:], self.gffn.partition_broadcast(128), writes=[gb_r])
    G, SH = [], []
    for s in range(2):
        g1, g1_r = kb.sbuf("g2", [128, D], F32)
        sh, sh_r = kb.sbuf("sh2", [128, D], F32)
        kb.dma(kb.sp, g1[:], self.modbc[s * 6 + 4], reads=[self.r_modbc], writes=[g1_r])
        kb.dma(kb.sp, sh[:], self.modbc[s * 6 + 3], reads=[self.r_modbc], writes=[sh_r])
        kb.op(kb.dve, lambda: nc.vector.scalar_tensor_tensor(out=g1[:], in0=g1[:], scalar=1.0, in1=gb[:], op0=ALU.add,
                                                             op1=ALU.mult), reads=[g1_r, gb_r], writes=[g1_r])
        G.append((g1, g1_r))
        SH.append((sh, sh_r))
    identB, idb_r = kb.sbuf("identB", [128, 128], BF16)
    kb.op(kb.dve, lambda: nc.vector.tensor_copy(out=identB[:], in_=identF[:]), reads=[id_r], writes=[idb_r])
    iop, iop_r = kb.sbuf("iop", [128, 1], F32)
    kb.dma(kb.sp, iop[:], self.iotap, writes=[iop_r])
    hTs = [kb.sbuf("hT2", [128, 16, 512], BF16) for _ in range(2)]

    def tile_group(t0):
        if t0 < NCTX:
            return 0, t0 // 128
        return 1 + (t0 - NCTX) // 512, ((t0 - NCTX) % 512) // 128
    xts = [kb.sbuf("xr", [128, D], F32) for _ in range(2)]
    junk, junk_r = kb.sbuf("junkr", [128, D], BF16)
    junkf, junkf_r = kb.sbuf("junkf", [128, NE], F32)
    hbs = [kb.sbuf("hbr", [128, D], BF16) for _ in range(2)]
    hTf, hTf_r = kb.sbuf("hTf", [128, 16, 128], F32)
    sts = [kb.sbuf("str", [128, 4], F32) for _ in range(2)]
    rts = [kb.sbuf("rt", [128, 96], F32) for _ in range(2)]
    dis = [kb.sbuf("di", [128, 4], I32) for _ in range(2)]
    dks = [[kb.sbuf("dk", [128, 1], I32) for _ in range(4)] for _ in range(2)]
    ius = [kb.sbuf("iu", [128, 8], U32) for _ in range(2)]
    poss = [kb.sbuf("pos", [128, NE], F32) for _ in range(2)]
    for ti, (t0, s) in enumerate(own_tiles(self)):
        xt, xt_r = xts[ti % 2]
        hb, hb_r = hbs[ti % 2]
        st, st_r = sts[ti % 2]
        rt, rt_r = rts[ti % 2]
        di, di_r = dis[ti % 2]
        iu, iu_r = ius[ti % 2]
        pos, pos_r = poss[ti % 2]
        g1, g1_r = G[s]
        sh, sh_r = SH[s]
        kb.dma(kb.sp, xt[:], self.x1[t0:t0 + 128, :], reads=[self.r_x1], writes=[xt_r])
        kb.op(kb.act, lambda: nc.scalar.activation(out=junk[:], in_=xt[:], func=AF.Square, accum_out=st[:, 0:1]),
              reads=[xt_r], writes=[junk_r, st_r])
        kb.op(kb.dve, lambda: nc.vector.tensor_scalar(out=st[:, 1:2], in0=st[:, 0:1], scalar1=1.0 / D, scalar2=EPS,
                                                      op0=ALU.mult, op1=ALU.add), reads=[st_r], writes=[st_r])
        kb.op(kb.act, lambda: nc.scalar.sqrt(out=st[:, 2:3], in_=st[:, 1:2]), reads=[st_r], writes=[st_r])
        kb.op(kb.dve, lambda: nc.vector.reciprocal(out=st[:, 3:4], in_=st[:, 2:3]), reads=[st_r], writes=[st_r])
        kb.op(kb.dve, lambda: nc.vector.scalar_tensor_tensor(out=xt[:], in0=xt[:], scalar=st[:, 3:4], in1=g1[:],
                                                             op0=ALU.mult, op1=ALU.mult), reads=[xt_r, st_r, g1_r], writes=[xt_r])
        kb.op(kb.pool, lambda: nc.gpsimd.tensor_tensor(out=xt[:], in0=xt[:], in1=sh[:], op=ALU.add),
              reads=[xt_r, sh_r], writes=[xt_r])
        kb.op(kb.act, lambda: nc.scalar.copy(out=hb[:], in_=xt[:]), reads=[xt_r], writes=[hb_r])
        for q in range(4):
            ps, ps_r = kb.bank()
            for kk in range(4):
                k = q * 4 + kk
                kb.op(kb.pe, lambda: nc.tensor.transpose(out=ps[:, kk * 128:(kk + 1) * 128], in_=xt[:, k * 128:(k + 1) * 128],
                                                         identity=identF[:]), reads=[xt_r, id_r], writes=[ps_r])
            eng = self.evac_eng()
            kb.op(eng, self.copy(eng, hTf[:, q * 4:q * 4 + 4, :], ps[:].rearrange("p (k t) -> p k t", t=128)),
                  reads=[ps_r], writes=[hTf_r])
        psl, psl_r = kb.bank()
        for k in range(16):
            kb.op(kb.pe, lambda: nc.tensor.matmul(psl[:, 0:NE], lhsT=hTf[:, k, :], rhs=wr[:, k, :], start=(k == 0), stop=(k == 15)),
                  reads=[hTf_r, wr_r], writes=[psl_r])
        kb.op(kb.dve, lambda: nc.vector.tensor_tensor(out=rt[:, 0:NE], in0=psl[:, 0:NE], in1=br[:], op=ALU.add),
              reads=[psl_r, br_r], writes=[rt_r])
        kb.op(kb.dve, lambda: nc.vector.max(out=rt[:, 32:40], in_=rt[:, 0:NE]), reads=[rt_r], writes=[rt_r])
        kb.op(kb.dve, lambda: nc.vector.max_index(out=iu[:], in_max=rt[:, 32:40], in_values=rt[:, 0:NE]),
              reads=[rt_r], writes=[iu_r])
        kb.op(kb.dve, lambda: nc.vector.tensor_copy(out=rt[:, 40:48], in_=iu[:]), reads=[iu_r], writes=[rt_r])
        kb.op(kb.dve, lambda: nc.vector.tensor_scalar(out=rt[:, 48:52], in0=rt[:, 32:36], scalar1=rt[:, 32:33], scalar2=None,
                                                      op0=ALU.subtract), reads=[rt_r], writes=[rt_r])
        kb.op(kb.act, lambda: nc.scalar.activation(out=rt[:, 48:52], in_=rt[:, 48:52], func=AF.Exp, accum_out=rt[:, 52:53]),
              reads=[rt_r], writes=[rt_r])
        kb.op(kb.dve, lambda: nc.vector.reciprocal(out=rt[:, 53:54], in_=rt[:, 52:53]), reads=[rt_r], writes=[rt_r])
        kb.op(kb.dve, lambda: nc.vector.tensor_scalar(out=rt[:, 48:52], in0=rt[:, 48:52], scalar1=rt[:, 53:54], scalar2=None,
                                                      op0=ALU.mult), reads=[rt_r], writes=[rt_r])
        kb.op(kb.dve, lambda: nc.vector.tensor_scalar(out=rt[:, 56:88], in0=iot[:], scalar1=rt[:, 40:41], scalar2=rt[:, 48:49],
                                                      op0=ALU.is_equal, op1=ALU.mult), reads=[rt_r, iot_r], writes=[rt_r])
        for k in range(1, 4):
            kb.op(kb.dve, lambda: nc.vector.tensor_scalar(out=junkf[:], in0=iot[:], scalar1=rt[:, 40 + k:41 + k],
                                                          scalar2=rt[:, 48 + k:49 + k], op0=ALU.is_equal, op1=ALU.mult),
                  reads=[rt_r, iot_r], writes=[junkf_r])
            kb.op(kb.dve, lambda: nc.vector.tensor_tensor(out=rt[:, 56:88], in0=rt[:, 56:88], in1=junkf[:], op=ALU.add),
                  reads=[rt_r, junkf_r], writes=[rt_r])
        kb.dma(kb.sp, self.gd[t0:t0 + 128, :], rt[:, 56:88], reads=[rt_r], mwrites=[self.r_gd])
        g, j = tile_group(t0)
        hT, hT_r = hTs[g % 2]
        for half in range(2):
            ps, ps_r = kb.bank()
            pb = ps[:].bitcast(BF16)
            for kk in range(8):
                k = half * 8 + kk
                kb.op(kb.pe, lambda: nc.tensor.transpose(out=pb[:, kk * 128:(kk + 1) * 128], in_=hb[:, k * 128:(k + 1) * 128],
                                                         identity=identB[:]), reads=[hb_r, idb_r], writes=[ps_r])
            eng = self.evac_eng()
            kb.op(eng, self.copy(eng, hT[:, half * 8:half * 8 + 8, j * 128:(j + 1) * 128],
                                 pb.rearrange("p (k t) -> p k t", t=128)), reads=[ps_r], writes=[hT_r])
        if (j + 1) * 128 == GROUPS[g][1]:
            ntok = GROUPS[g][1]
            kb.dma(kb.sp, self.hT2_d[g, :, :, 0:ntok], hT[:, :, 0:ntok], reads=[hT_r], writes=[self.r_hT2[g]])
    kb.end_phase()


def phase_experts(self):
    nc, kb = self.nc, self.kb
    kb.begin_phase()
    wbuf = [kb.sbuf("wb", [128, 16, 1024], BF16) for _ in range(3)]
    hTs = [kb.sbuf("xTe", [128, 16, 512], BF16) for _ in range(2)]
    aTs = [kb.sbuf("aTe", [128, 8, 512], BF16) for _ in range(2)]
    bgus = [kb.sbuf("bgu", [128, 16], F32) for _ in range(2)]
    bdfs = [kb.sbuf("bdf", [1, D], F32) for _ in range(2)]
    bdbs = [kb.sbuf("bdb", [1, D], BF16) for _ in range(2)]
    onesb, onesb_r = kb.sbuf("onesb", [1, 128], BF16)
    kb.op(kb.pool, lambda: nc.gpsimd.memset(onesb[:], 1.0), writes=[onesb_r])
    ost = [kb.sbuf("ost", [128, D], F32) for _ in range(2)]
    g1s = [kb.sbuf("g1s", [128, 512], F32) for _ in range(2)]
    g2s = [kb.sbuf("g2s", [128, 512], F32) for _ in range(2)]

    def load_bias(e):
        bgu_, bgu_r_ = bgus[e % 2]
        bdf_, bdf_r_ = bdfs[e % 2]
        bdb_, bdb_r_ = bdbs[e % 2]
        kb.dma(kb.sp, bgu_[:], self.b_guT[e], writes=[bgu_r_])
        kb.dma(kb.sp, bdf_[:], self.b_down[e:e + 1, :], writes=[bdf_r_])
        kb.op(kb.act, lambda: nc.scalar.copy(out=bdb_[:], in_=bdf_[:]), reads=[bdf_r_], writes=[bdb_r_])
    load_bias(0)
    groups = [(g, GROUPS[g][0], GROUPS[g][1]) for g in range(len(GROUPS)) if (g > 0 or self.need_ctx)]
    NT = T // 128
    gdt, gdt_r = kb.sbuf("gdt", [128, NT, NE], F32)
    kb.dma(kb.sp, gdt[:], self.gd.rearrange("(n p) e -> p n e", p=128), reads=[self.r_gd], writes=[gdt_r])
    gi = 0
    oi = 0
    hi = 0
    def load_w(e, piece):
        wb, wb_r = wbuf[piece]
        if piece < 2:
            guv = self.w_gu[e].rearrange("(k p) n -> p k n", p=128)
            kb.dma(kb.pool, wb[:, :, 0:512], guv[:, :, piece * 512:(piece + 1) * 512], mwrites=[wb_r])
            kb.dma(kb.pool, wb[:, :, 512:1024], guv[:, :, 1024 + piece * 512:1024 + (piece + 1) * 512], mwrites=[wb_r])
        else:
            dnv = self.w_down[e].rearrange("(k p) n -> p k n", p=128)
            wv_ = wb[:].rearrange("p (k two) n -> p k (two n)", two=2)
            kb.dma(kb.pool, wv_[:, :, 0:1024], dnv[:, :, 0:1024], mwrites=[wb_r])
            kb.dma(kb.pool, wv_[:, :, 1024:2048], dnv[:, :, 1024:2048], mwrites=[wb_r])

    for p_ in range(3):
        load_w(0, p_)
    for e in range(NE):
        pieces = [wbuf[0], wbuf[1]]
        wdv = wbuf[2][0][:].rearrange("p (k two) n -> p k (two n)", two=2)
        wd_r = wbuf[2][1]
        bgu, bgu_r = bgus[e % 2]
        bdb, bdb_r = bdbs[e % 2]
        if e + 1 < NE:
            load_bias(e + 1)
        for gidx, (g, t0, ntk) in enumerate(groups):
            pre = (gidx == len(groups) - 1) and (e + 1 < NE)
            hT, hT_r = hTs[hi % 2]
            aT, aT_r = aTs[hi % 2]
            hi += 1
            kb.dma(kb.act, hT[:, :, 0:ntk], self.hT2_d[g, :, :, 0:ntk], reads=[self.r_hT2[g]], writes=[hT_r])
            for half in range(2):
                wb, wb_r = pieces[half]
                for jc in range(4):
                    psg, psg_r = kb.bank()
                    psl, psl_r = kb.bank()
                    for k in range(16):
                        kb.op(kb.pe, lambda: nc.tensor.matmul(psg[:, 0:ntk], lhsT=wb[:, k, jc * 128:(jc + 1) * 128],
                                                              rhs=hT[:, k, 0:ntk], start=(k == 0), stop=(k == 15)),
                              reads=[wb_r, hT_r], writes=[psg_r])
                    for k in range(16):
                        kb.op(kb.pe, lambda: nc.tensor.matmul(psl[:, 0:ntk], lhsT=wb[:, k, 512 + jc * 128:512 + (jc + 1) * 128],
                                                              rhs=hT[:, k, 0:ntk], start=(k == 0), stop=(k == 15)),
                              reads=[wb_r, hT_r], writes=[psl_r])
                    J = half * 4 + jc
                    g1, g1_r = g1s[gi % 2]
                    g2, g2_r = g2s[gi % 2]
                    gi += 1
                    kb.op(kb.dve, lambda: nc.vector.tensor_scalar(out=g1[:, 0:ntk], in0=psg[:, 0:ntk], scalar1=bgu[:, J:J + 1],
                                                                  scalar2=7.0, op0=ALU.add, op1=ALU.min),
                          reads=[psg_r, bgu_r], writes=[g1_r])
                    kb.op(kb.act, lambda: nc.scalar.activation(out=g2[:, 0:ntk], in_=g1[:, 0:ntk], func=AF.Sigmoid, scale=1.702),
                          reads=[g1_r], writes=[g2_r])
                    kb.op(kb.pool, lambda: nc.gpsimd.tensor_tensor(out=g1[:, 0:ntk], in0=g1[:, 0:ntk], in1=g2[:, 0:ntk],
                                                                   op=ALU.mult), reads=[g1_r, g2_r], writes=[g1_r])
                    kb.op(kb.dve, lambda: nc.vector.tensor_scalar(out=g2[:, 0:ntk], in0=psl[:, 0:ntk], scalar1=bgu[:, 8 + J:9 + J],
                                                                  scalar2=7.0, op0=ALU.add, op1=ALU.min),
                          reads=[psl_r, bgu_r, g2_r], writes=[g2_r])
                    kb.op(kb.dve, lambda: nc.vector.tensor_scalar(out=g2[:, 0:ntk], in0=g2[:, 0:ntk], scalar1=-7.0, scalar2=1.0,
                                                                  op0=ALU.max, op1=ALU.add), reads=[g2_r], writes=[g2_r])
                    kb.op(kb.dve, lambda: nc.vector.tensor_tensor(out=aT[:, J, 0:ntk], in0=g1[:, 0:ntk], in1=g2[:, 0:ntk],
                                                                  op=ALU.mult), reads=[g1_r, g2_r], writes=[aT_r])
                if pre:
                    load_w(e + 1, half)
            for j in range(ntk // 128):
                o, o_r = ost[oi % 2]
                oi += 1
                tix = (t0 + j * 128) // 128
                gcol = gdt[:, tix, e:e + 1]
                yp, yp_r = o, o_r
                if e > 0:
                    kb.dma(kb.act, yp[:], self.yacc[t0 + j * 128:t0 + (j + 1) * 128, :], reads=[self.r_yacc[tix]], writes=[yp_r])
                for q in range(4):
                    ps, ps_r = kb.bank()
                    for k in range(8):
                        kb.op(kb.pe, lambda: nc.tensor.matmul(ps[:], lhsT=aT[:, k, j * 128:(j + 1) * 128],
                                                              rhs=wdv[:, k, q * 512:(q + 1) * 512], start=(k == 0), stop=False),
                              reads=[aT_r, wd_r], writes=[ps_r])
                    kb.op(kb.pe, lambda: nc.tensor.matmul(ps[:], lhsT=onesb[:], rhs=bdb[:, q * 512:(q + 1) * 512], start=False,
                                                          stop=True), reads=[onesb_r, bdb_r], writes=[ps_r])
                    if e == 0:
                        kb.op(kb.dve, lambda: nc.vector.tensor_scalar(out=o[:, q * 512:(q + 1) * 512], in0=ps[:], scalar1=gcol,
                                                                      scalar2=None, op0=ALU.mult), reads=[ps_r, gdt_r], writes=[o_r])
                    else:
                        kb.op(kb.dve, lambda: nc.vector.scalar_tensor_tensor(out=o[:, q * 512:(q + 1) * 512], in0=ps[:], scalar=gcol,
                                                                             in1=yp[:, q * 512:(q + 1) * 512], op0=ALU.mult,
                                                                             op1=ALU.add), reads=[ps_r, gdt_r, yp_r], writes=[o_r])
                r0 = t0 + j * 128
                kb.dma(kb.sp, self.yacc[r0:r0 + 128, :], o[:], reads=[o_r], writes=[self.r_yacc[tix]])
            if pre:
                load_w(e + 1, 2)
    kb.end_phase()


def phase_combine(self, xdst, xdst_r, final_out=None, final_g=None):
    nc, kb = self.nc, self.kb
    kb.begin_phase()
    gts = []
    for s in range(2):
        gt, gt_r = kb.sbuf("gate2", [128, D], F32)
        kb.dma(kb.sp, gt[:], self.modbc[s * 6 + 5], reads=[self.r_modbc], writes=[gt_r])
        gts.append((gt, gt_r))
    if final_out is not None:
        fg, fg_r = kb.sbuf("fg", [128, D], F32)
        kb.dma(kb.sp, fg[:], final_g.partition_broadcast(128), writes=[fg_r])
    xts = [kb.sbuf("xc", [128, D], F32) for _ in range(2)]
    accs = [kb.sbuf("acc", [128, D], F32) for _ in range(2)]
    sts = [kb.sbuf("stc", [128, 4], F32) for _ in range(2)]
    junk, junk_r = kb.sbuf("junkc", [128, D], BF16)
    for ti, (t0, s) in enumerate(own_tiles(self)):
        xt, xt_r = xts[ti % 2]
        acc, acc_r = accs[ti % 2]
        gt, gt_r = gts[s]
        kb.dma(kb.sp, acc[:], self.yacc[t0:t0 + 128, :], reads=[self.r_yacc[t0 // 128]], writes=[acc_r])
        kb.dma(kb.act, xt[:], self.x1[t0:t0 + 128, :], reads=[self.r_x1], writes=[xt_r])
        kb.op(kb.pool, lambda: nc.gpsimd.tensor_tensor(out=acc[:], in0=acc[:], in1=gt[:], op=ALU.mult),
              reads=[acc_r, gt_r], writes=[acc_r])
        kb.op(kb.dve, lambda: nc.vector.tensor_tensor(out=acc[:], in0=acc[:], in1=xt[:], op=ALU.add),
              reads=[acc_r, xt_r], writes=[acc_r])
        if final_out is None:
            kb.dma(kb.sp, xdst[t0:t0 + 128, :], acc[:], reads=[acc_r], mwrites=[xdst_r])
        else:
            st, st_r = sts[ti % 2]
            kb.op(kb.act, lambda: nc.scalar.activation(out=junk[:], in_=acc[:], func=AF.Square, accum_out=st[:, 0:1]),
                  reads=[acc_r], writes=[junk_r, st_r])
            kb.op(kb.dve, lambda: nc.vector.tensor_scalar(out=st[:, 1:2], in0=st[:, 0:1], scalar1=1.0 / D, scalar2=EPS,
                                                          op0=ALU.mult, op1=ALU.add), reads=[st_r], writes=[st_r])
            kb.op(kb.act, lambda: nc.scalar.sqrt(out=st[:, 2:3], in_=st[:, 1:2]), reads=[st_r], writes=[st_r])
            kb.op(kb.dve, lambda: nc.vector.reciprocal(out=st[:, 3:4], in_=st[:, 2:3]), reads=[st_r], writes=[st_r])
            kb.op(kb.dve, lambda: nc.vector.scalar_tensor_tensor(out=acc[:], in0=acc[:], scalar=st[:, 3:4], in1=fg[:],
                                                                 op0=ALU.mult, op1=ALU.mult), reads=[acc_r, st_r, fg_r],
                  writes=[acc_r])
            kb.dma(kb.sp, final_out[t0 - NCTX:t0 - NCTX + 128, :], acc[:], reads=[acc_r], mwrites=[xdst_r])
    kb.end_phase()


Layer.declare_ffn = declare_ffn
Layer.phase_outproj = phase_outproj
Layer.phase_route = phase_route
Layer.phase_experts = phase_experts
Layer.phase_combine = phase_combine


def build_model(layers=(0, 1), stop_after=None, debug=()):
    nc = bass.Bass("TRN2", target_bir_lowering=False)
    kb = KB(nc)
    kb.debug = set(debug) | {"out"}
    Ls = []
    for l in layers:
        last = (l == 1)
        L = Layer(nc, kb, need_ctx=not last, stop_after=stop_after, lidx=l, last=last)
        L.declare_io()
        L.run()
        Ls.append(L)
    kb.finish()
    return nc, kb


_NC_CACHE = {}


def kernel(**inputs):
    inp = {k: np.asarray(v) for k, v in inputs.items()}
    if "nc" not in _NC_CACHE:
        _NC_CACHE["nc"] = build_model()
    nc, kb = _NC_CACHE["nc"]
    names = set(kb._inp.keys())
    lay = {}
    for l in (0, 1):
        lay.update(host_layer_inputs(inp, l))
    maps = []
    for b in range(4):
        m = dict(lay)
        m.update(host_shared_inputs(inp, b))
        maps.append({k: np.ascontiguousarray(v, dtype=np.float32) for k, v in m.items() if k in names})
    res = run_bass_kernel_spmd(nc, maps, core_ids=list(range(4)))
    return np.stack([res.results[b]["out"] for b in range(4)], 0).astype(np.float32)
```
